# Optimizing a Trainium2 kernel written in Bass

```python
import math
import jax, jax.numpy as jnp
from jax import lax
import numpy as np

D_MODEL = 1024
BATCH = 4
SEQ = 8192
DEPTH = 1

CHUNK = 64
EPS = 1e-6
DA_HEADS = 4
DA_HEAD_DIM = 64
DA_V_DIM = 2 * DA_HEAD_DIM
DA_QK_COLS = DA_HEADS * 2 * DA_HEAD_DIM
DA_WIDTH = DA_HEADS * DA_V_DIM
Q_BLOCK = 128
SG_GROUPS = 4
SG_BLOCK = 128
SG_GROUP_DIM = 128
SG_WIDTH = SG_GROUPS * SG_GROUP_DIM
N_BRANCHES = 2
IN_COLS = 3 * DA_QK_COLS // 1 - DA_QK_COLS + DA_WIDTH + 2 * SG_WIDTH + N_BRANCHES * D_MODEL
PEER_HEADS = 8
PEER_KEYS = 128
PEER_EXPERTS = PEER_KEYS * PEER_KEYS
PEER_KEY_DIM = 256
PEER_HALF = PEER_KEY_DIM // 2
PEER_TOPK = 16
PEER_TOK_BLOCK = 64

kernel_name = 'hybrid_diffattn_sgu_peer_adaln'


def rmsnorm(x, g):
    xf = x.astype(jnp.float32)
    y = xf * lax.rsqrt(jnp.mean(xf * xf, axis=-1, keepdims=True) + EPS)
    return (y * g.astype(jnp.float32)).astype(x.dtype)


def layernorm(x, g, b):
    xf = x.astype(jnp.float32)
    mu = jnp.mean(xf, axis=-1, keepdims=True)
    xc = xf - mu
    y = xc * lax.rsqrt(jnp.mean(xc * xc, axis=-1, keepdims=True) + EPS)
    return (y * g.astype(jnp.float32) + b.astype(jnp.float32)).astype(x.dtype)


def diff_attention(q, k, v, lam, lam_init, head_g):
    B, S = q.shape[0], q.shape[1]
    nblk = S // Q_BLOCK
    scale = DA_HEAD_DIM ** -0.5
    kt = jnp.transpose(k, (0, 2, 3, 1, 4))
    vt = jnp.transpose(v, (0, 2, 1, 3))
    qb = q.reshape(B, nblk, Q_BLOCK, DA_HEADS, 2, DA_HEAD_DIM).transpose(1, 0, 3, 4, 2, 5)
    key_chunk = jnp.arange(S) // CHUNK

    def block(args):
        qi, bi = args
        s = jnp.einsum('bhiqd,bhikd->bhiqk', qi, kt).astype(jnp.float32) * scale
        q_chunk = (bi * Q_BLOCK + jnp.arange(Q_BLOCK)) // CHUNK
        mask = key_chunk[None, :] <= q_chunk[:, None]
        p = jax.nn.softmax(jnp.where(mask, s, -jnp.inf), axis=-1)
        a = p[:, :, 0] - lam * p[:, :, 1]
        return jnp.einsum('bhqk,bhkv->bqhv', a.astype(vt.dtype), vt)

    o = lax.map(block, (qb, jnp.arange(nblk)))
    o = o.transpose(1, 0, 2, 3, 4).reshape(B, S, DA_HEADS, DA_V_DIM)
    o = rmsnorm(o, head_g) * (1.0 - lam_init)
    return o.reshape(B, S, DA_WIDTH)


def spatial_gating(u, sv, ln_g, ln_b, w_s, b_s):
    B, S = u.shape[0], u.shape[1]
    nb = S // SG_BLOCK
    sv = layernorm(sv, ln_g, ln_b)
    pos_chunk = jnp.arange(SG_BLOCK) // CHUNK
    mask = pos_chunk[:, None] >= pos_chunk[None, :]
    w = jnp.where(mask[None], w_s, 0.0)
    svb = sv.reshape(B, nb, SG_BLOCK, SG_GROUPS, SG_GROUP_DIM)
    mixed = jnp.einsum('gpq,bnqgc->bnpgc', w, svb) + b_s.T[None, None, :, :, None]
    return u * mixed.reshape(B, S, SG_WIDTH)


def peer(h, w_query, sub_keys, down, up):
    B, S, D = h.shape
    q = jnp.einsum('bsd,dk->bsk', h, w_query).reshape(B, S, PEER_HEADS, 2, PEER_HALF)
    sc = jnp.einsum('bshpd,hpnd->bshpn', q, sub_keys).astype(jnp.float32)
    s1, i1 = lax.top_k(sc[:, :, :, 0, :], PEER_TOPK)
    s2, i2 = lax.top_k(sc[:, :, :, 1, :], PEER_TOPK)
    cand = (s1[..., :, None] + s2[..., None, :]).reshape(B, S, PEER_HEADS, PEER_TOPK * PEER_TOPK)
    cidx = (i1[..., :, None] * PEER_KEYS + i2[..., None, :]).reshape(B, S, PEER_HEADS, PEER_TOPK * PEER_TOPK)
    top_s, pos = lax.top_k(cand, PEER_TOPK)
    eidx = jnp.take_along_axis(cidx, pos, axis=-1)
    g = jax.nn.softmax(top_s, axis=-1)
    nb = S // PEER_TOK_BLOCK
    hb = h.reshape(B, nb, PEER_TOK_BLOCK, D).transpose(1, 0, 2, 3)
    eb = eidx.reshape(B, nb, PEER_TOK_BLOCK, PEER_HEADS, PEER_TOPK).transpose(1, 0, 2, 3, 4)
    gb = g.reshape(B, nb, PEER_TOK_BLOCK, PEER_HEADS, PEER_TOPK).transpose(1, 0, 2, 3, 4)

    def block(args):
        hx, ex, gx = args
        u_e = jnp.take(down, ex, axis=0)
        a = jax.nn.gelu(jnp.einsum('btd,bthkd->bthk', hx, u_e).astype(jnp.float32))
        v_e = jnp.take(up, ex, axis=0)
        return jnp.einsum('bthk,bthkd->btd', (gx * a).astype(v_e.dtype), v_e)

    out = lax.map(block, (hb, eb, gb))
    return out.transpose(1, 0, 2, 3).reshape(B, S, D)


def setup_inputs(seed: int = 0) -> dict:
    key = jax.random.key(seed)
    ks = jax.random.split(key, 25)
    L, D = DEPTH, D_MODEL

    def n(k, shape, s):
        return jax.random.normal(k, shape, jnp.float32) * s

    return {
        'x': n(ks[0], (BATCH, SEQ, D), 1.0),
        'c': n(ks[1], (BATCH, D), 1.0),
        'w_ada': n(ks[2], (L, D, 6 * D), D ** -0.5),
        'b_ada': n(ks[3], (L, 6 * D), 0.02),
        'norm1_g': 1.0 + n(ks[4], (L, D), 0.02),
        'w_in': n(ks[5], (L, D, IN_COLS), D ** -0.5),
        'da_lambda_q1': n(ks[6], (L, DA_HEAD_DIM), 0.1),
        'da_lambda_k1': n(ks[7], (L, DA_HEAD_DIM), 0.1),
        'da_lambda_q2': n(ks[8], (L, DA_HEAD_DIM), 0.1),
        'da_lambda_k2': n(ks[9], (L, DA_HEAD_DIM), 0.1),
        'da_head_g': 1.0 + n(ks[10], (L, DA_V_DIM), 0.02),
        'sg_ln_g': 1.0 + n(ks[11], (L, SG_WIDTH), 0.02),
        'sg_ln_b': n(ks[12], (L, SG_WIDTH), 0.02),
        'sg_w': n(ks[13], (L, SG_GROUPS, SG_BLOCK, SG_BLOCK), SG_BLOCK ** -0.5),
        'sg_b': 1.0 + n(ks[14], (L, SG_GROUPS, SG_BLOCK), 0.1),
        'w_branch_a': n(ks[15], (L, DA_WIDTH, D), DA_WIDTH ** -0.5),
        'w_branch_b': n(ks[16], (L, SG_WIDTH, D), SG_WIDTH ** -0.5),
        'w_out': n(ks[17], (L, D, D), D ** -0.5),
        'norm2_g': 1.0 + n(ks[18], (L, D), 0.02),
        'peer_w_query': n(ks[19], (L, D, PEER_HEADS * PEER_KEY_DIM), D ** -0.5),
        'peer_sub_keys': n(ks[20], (L, PEER_HEADS, 2, PEER_KEYS, PEER_HALF), PEER_HALF ** -0.5),
        'peer_down': n(ks[21], (L, PEER_EXPERTS, D), D ** -0.5),
        'peer_up': n(ks[22], (L, PEER_EXPERTS, D), PEER_HEADS ** -0.5),
        'final_g': 1.0 + n(ks[23], (D,), 0.02),
    }


def reference(x, c, w_ada, b_ada, norm1_g, w_in, da_lambda_q1, da_lambda_k1, da_lambda_q2,
              da_lambda_k2, da_head_g, sg_ln_g, sg_ln_b, sg_w, sg_b, w_branch_a, w_branch_b,
              w_out, norm2_g, peer_w_query, peer_sub_keys, peer_down, peer_up, final_g):
    B, S, D = x.shape
    splits = [DA_QK_COLS, 2 * DA_QK_COLS, 2 * DA_QK_COLS + DA_WIDTH,
              2 * DA_QK_COLS + DA_WIDTH + SG_WIDTH, 2 * DA_QK_COLS + DA_WIDTH + 2 * SG_WIDTH]
    for l in range(DEPTH):
        mod = jnp.einsum('bd,de->be', jax.nn.silu(c), w_ada[l]) + b_ada[l]
        sh1, sc1, g1, sh2, sc2, g2 = jnp.split(mod[:, None, :], 6, axis=-1)

        h = rmsnorm(x, norm1_g[l]) * (1.0 + sc1) + sh1
        proj = jnp.einsum('bsd,de->bse', h, w_in[l])
        q, k, v, u, sv, gates = jnp.split(proj, splits, axis=-1)
        q = q.reshape(B, S, DA_HEADS, 2, DA_HEAD_DIM)
        k = k.reshape(B, S, DA_HEADS, 2, DA_HEAD_DIM)
        v = v.reshape(B, S, DA_HEADS, DA_V_DIM)
        lam_init = 0.8 - 0.6 * math.exp(-0.3 * l)
        lam = (jnp.exp(jnp.sum(da_lambda_q1[l].astype(jnp.float32) * da_lambda_k1[l].astype(jnp.float32)))
               - jnp.exp(jnp.sum(da_lambda_q2[l].astype(jnp.float32) * da_lambda_k2[l].astype(jnp.float32)))
               + lam_init)
        ya = diff_attention(q, k, v, lam, lam_init, da_head_g[l])
        yb = spatial_gating(jax.nn.gelu(u), jax.nn.gelu(sv), sg_ln_g[l], sg_ln_b[l], sg_w[l], sg_b[l])
        ga, gb = jnp.split(jax.nn.sigmoid(gates), N_BRANCHES, axis=-1)
        merged = (ga * jnp.einsum('bse,ed->bsd', ya, w_branch_a[l])
                  + gb * jnp.einsum('bse,ed->bsd', yb, w_branch_b[l]))
        x = x + g1 * jnp.einsum('bsd,de->bse', merged, w_out[l])

        h2 = rmsnorm(x, norm2_g[l]) * (1.0 + sc2) + sh2
        x = x + g2 * peer(h2, peer_w_query[l], peer_sub_keys[l], peer_down[l], peer_up[l])
    return rmsnorm(x, final_g)
```

```python
import numpy as np
from contextlib import ExitStack
import concourse.bass as bass
import concourse.mybir as mybir
from concourse.bass_utils import run_bass_kernel_spmd

F32 = mybir.dt.float32
BF16 = mybir.dt.bfloat16
I32 = mybir.dt.int32
U32 = mybir.dt.uint32
AF = mybir.ActivationFunctionType
OP = mybir.AluOpType
AX = mybir.AxisListType

D = 1024
S = 8192
NB = 64
NOWN = 32
EPS = 1e-6
LAM_INIT = 0.8 - 0.6 * 1.0
NEG = -1.0e30
SEM_LIMIT = 30000


class Reg:
    def __init__(self):
        self.w = {}
        self.r = {}


class Buf(Reg):
    def __init__(self, t):
        Reg.__init__(self)
        self.t = t

    def __getitem__(self, k):
        return self.t[k]


class FW:
    def __init__(self, nc):
        self.nc = nc
        self.engs = dict(pe=nc.tensor, act=nc.scalar, dve=nc.vector, pool=nc.gpsimd, sp=nc.sync)
        self.cur = {}
        self.nsem = 0
        self.known = {k: {} for k in self.engs}
        for k in self.engs:
            self._newsem(k)
        self.dsem = {}
        for q, n in (("sp", 12), ("act", 4), ("pool", 12)):
            self.dsem[q] = [[f"d_{q}{i}", nc.alloc_semaphore(f"d_{q}{i}"), 0] for i in range(n)]
        self.drr = {q: 0 for q in self.dsem}
        self.ninst = 0
        self.hook = None

    def _newsem(self, k):
        name = f"e_{k}{self.nsem}"
        self.cur[k] = [name, self.nc.alloc_semaphore(name), 0]
        self.nsem += 1

    def _deps(self, reads, writes):
        deps = {}

        def add(d):
            for name, (s, v) in d.items():
                if name not in deps or deps[name][1] < v:
                    deps[name] = (s, v)
        for b in reads:
            add(b.w)
        for b in writes:
            add(b.w)
            add(b.r)
        return deps

    def _wait(self, k, deps):
        eng = self.engs[k]
        for name, (s, v) in deps.items():
            if k == "pe" and name.startswith("e_pe"):
                continue
            if self.known[k].get(name, 0) >= v:
                continue
            eng.wait_ge(s, v)
            self.known[k][name] = v
            self.ninst += 1

    def _record(self, ev, reads, writes):
        name, s, v = ev
        for b in reads:
            b.r[name] = (s, v)
        for b in writes:
            b.w = {name: (s, v)}
            b.r = {}

    def op(self, k, fn, reads, writes):
        self._wait(k, self._deps(reads, writes))
        ins = fn(self.engs[k])
        c = self.cur[k]
        if c[2] >= SEM_LIMIT:
            self._newsem(k)
            c = self.cur[k]
        c[2] += 1
        ins.then_inc(c[1], 1)
        self.ninst += 1
        self._record((c[0], c[1], c[2]), reads, writes)
        if self.hook is not None:
            self.hook()

    def dma(self, q, fn, reads, writes, slot=None, pre=()):
        if slot is None:
            slots = self.dsem[q]
            i = self.drr[q]
            self.drr[q] = (i + 1) % len(slots)
            sl = slots[i]
        else:
            sl = slot
        deps = self._deps(reads, list(writes) + list(pre))
        if slot is None and sl[2] > 0:
            deps[sl[0]] = (sl[1], sl[2])
        self._wait(q, deps)
        ins = fn(self.engs[q])
        sl[2] += 16
        ins.then_inc(sl[1], 16)
        self.ninst += 1
        self._record((sl[0], sl[1], sl[2]), reads, writes)
        self.last_written = writes[0] if writes else None
        if self.hook is not None:
            self.hook()

    def barrier(self):
        evs = {}
        for k, c in self.cur.items():
            if c[2] > 0:
                evs[c[0]] = (c[1], c[2])
        for q, slots in self.dsem.items():
            for sl in slots:
                if sl[2] > 0:
                    evs[sl[0]] = (sl[1], sl[2])
        for k in self.engs:
            self._wait(k, dict(evs))


def interleave(fw, fns, weights):
    import threading
    n = len(fns)
    sems = [threading.Semaphore(0) for _ in fns]
    done = [False] * n
    st = {"cur": 0, "cnt": 0}
    errs = []
    fin = threading.Semaphore(0)

    def nxt(i):
        for k in range(1, n + 1):
            j = (i + k) % n
            if not done[j]:
                return j
        return None

    def hook():
        i = st["cur"]
        st["cnt"] += 1
        if st["cnt"] >= weights[i]:
            st["cnt"] = 0
            j = nxt(i)
            if j is not None and j != i:
                st["cur"] = j
                sems[j].release()
                sems[i].acquire()

    def runner(i):
        sems[i].acquire()
        try:
            fns[i]()
        except BaseException as e:
            errs.append(e)
        done[i] = True
        j = nxt(i)
        if j is not None:
            st["cur"] = j
            st["cnt"] = 0
            sems[j].release()
        else:
            fin.release()

    ths = [threading.Thread(target=runner, args=(i,)) for i in range(n)]
    old = fw.hook
    fw.hook = hook
    for t in ths:
        t.start()
    st["cur"] = 0
    sems[0].release()
    fin.acquire()
    for t in ths:
        t.join()
    fw.hook = old
    if errs:
        raise errs[0]


class ViewBuf(Reg):
    def __init__(self, ap):
        Reg.__init__(self)
        self.ap_ = ap

    def __getitem__(self, k):
        return self.ap_[k]


def build(debug=None):
    nc = bass.Bass("TRN2", target_bir_lowering=False)
    fw = FW(nc)

    def din(name, shape, dt=F32):
        return nc.dram_tensor(name, list(shape), dt, kind="ExternalInput").ap()

    x_b = din("x_b", [S, D])
    x_own = din("x_own", [NOWN * 128, D])
    c_b = din("c_b", [1, D])
    w_ada = din("w_ada", [D, 6 * D])
    b_ada = din("b_ada", [1, 6 * D])
    norm1_g = din("norm1_g", [1, D])
    w_in = din("w_in", [D, 4608])
    lq1 = din("da_lambda_q1", [1, 64])
    lk1 = din("da_lambda_k1", [1, 64])
    lq2 = din("da_lambda_q2", [1, 64])
    lk2 = din("da_lambda_k2", [1, 64])
    head_g = din("da_head_g", [1, 128])
    ln_g = din("sg_ln_g", [1, 512])
    ln_b = din("sg_ln_b", [1, 512])
    sg_w = din("sg_w", [4, 128, 128])
    sg_b = din("sg_b", [4, 128])
    w_ba = din("w_branch_a", [512, D])
    w_bb = din("w_branch_b", [512, D])
    w_out = din("w_out", [D, D])
    norm2_g = din("norm2_g", [1, D])
    w_pq = din("peer_w_query", [D, 2048])
    sub_keys = din("peer_sub_keys", [16, 128, 128])
    p_down = din("peer_down", [16384, D])
    p_up = din("peer_up", [16384, D])
    final_g = din("final_g", [1, D])
    amask = din("amask", [128, 256])
    out_d = nc.dram_tensor("out", [NOWN * 128, D], F32, kind="ExternalOutput").ap()
    dbg_d = None
    if debug is not None:
        dbg_d = nc.dram_tensor("dbg", list(debug[1]), F32, kind="ExternalOutput").ap()

    winb = nc.dram_tensor("winb", [D, 4608], BF16, kind="Internal").ap()
    wbab = nc.dram_tensor("wbab", [512, D], BF16, kind="Internal").ap()
    wbbb = nc.dram_tensor("wbbb", [512, D], BF16, kind="Internal").ap()
    woutb = nc.dram_tensor("woutb", [D, D], BF16, kind="Internal").ap()
    wpqb = nc.dram_tensor("wpqb", [D, 2048], BF16, kind="Internal").ap()
    modscr = nc.dram_tensor("modscr", [1, 2048], F32, kind="Internal").ap()
    pdu = nc.dram_tensor("pdu", [16384, 2 * D], BF16, kind="Internal").ap()
    tregs = []
    tconv = [(pdu[:, hf * D:(hf + 1) * D], src, r0) for hf, src in enumerate((p_down, p_up)) for r0 in range(0, 16384, 1024)]
    wregs = []
    r_modscr = Reg()
    r_none = Reg()
    yaregs = []

    def sb(name, shape, dt=F32):
        return Buf(nc.alloc_sbuf_tensor(name, list(shape), dt))

    P2 = [nc.alloc_psum_tensor(f"ps{i}", [128, 1024], F32) for i in range(4)]
    PR = [Reg() for _ in range(8)]

    def bank(i):
        return P2[i // 2][:, (i % 2) * 512:(i % 2 + 1) * 512]

    def bank_bf(i):
        return P2[i // 2][:, :].bitcast(BF16)[:, (i % 2) * 1024:(i % 2 + 1) * 1024]

    def pair(i):
        return P2[i][:, :]

    identf = sb("identf", [128, 128])
    identb = sb("identb", [128, 128], BF16)
    mod_bc = sb("mod_bc", [128, 6 * D])
    fg_bc = sb("fg_bc", [128, D])
    yaTd = nc.dram_tensor("yaTd", [4, 128, NOWN * 128], BF16, kind="Internal").ap()
    r_yaT = [Reg() for _ in range(8)]
    ysts = [sb("yst0", [128, 128], BF16), sb("yst1", [128, 128], BF16)]
    ysi = [0]
    A1col = sb("A1col", [128, 8])
    B1col = sb("B1col", [128, 8])
    neglam = sb("neglam", [128, 1])
    hg_bc = sb("hg_bc", [128, 128])
    lng_bc = sb("lng_bc", [128, 512])
    lnb_bc = sb("lnb_bc", [128, 512])
    bs_col = sb("bs_col", [128, 4])
    maskb = sb("maskb", [128, 256], BF16)
    wsT = sb("wsT", [128, 4, 128], BF16)
    skT = sb("skT", [128, 16, 128], BF16)
    ss_r = [sb(f"ss{i}", [128, 4]) for i in range(4)]
    ss_i = [0]

    def dbg_out(buf_ap, reads, dst=None):
        fw.dma("sp", lambda e: e.dma_start(out=(dbg_d if dst is None else dst), in_=buf_ap), reads, [r_none])

    fw.op("pool", lambda e: e.memset(identf[:, :], 1.0), [], [identf])
    fw.op("pool", lambda e: e.affine_select(out=identf[:, :], in_=identf[:, :], pattern=[[-1, 128]],
                                            compare_op=OP.is_equal, fill=0.0, base=0, channel_multiplier=1),
          [identf], [identf])
    fw.op("dve", lambda e: e.tensor_copy(out=identb[:, :], in_=identf[:, :]), [identf], [identb])

    for (dst, src, ncol) in ((winb, w_in, 4608), (wbab, w_ba, D), (wbbb, w_bb, D), (woutb, w_out, D), (wpqb, w_pq, 2048)):
        for c0 in range(0, ncol, 512):
            fw.dma("pool", lambda e, dst=dst, src=src, c0=c0: e.dma_start(out=dst[:, c0:c0 + 512], in_=src[:, c0:c0 + 512]),
                   [], [Reg()])
            wregs.append(fw.last_written)

    with nc.allow_non_contiguous_dma(reason="tiny param loads"):
        n1g_col = sb("n1g_col", [128, 8])
        c_col = sb("c_col", [128, 8])
        fw.dma("sp", lambda e: e.dma_start(out=n1g_col[:, :], in_=norm1_g[0, :].rearrange("(c p) -> p c", p=128)), [], [n1g_col])
        fw.dma("sp", lambda e: e.dma_start(out=c_col[:, :], in_=c_b[0, :].rearrange("(c p) -> p c", p=128)), [], [c_col])
        fw.dma("sp", lambda e: e.dma_start(out=bs_col[:, :], in_=sg_b.rearrange("g p -> p g")), [], [bs_col])
    fw.dma("sp", lambda e: e.dma_start(out=mod_bc[:, :], in_=b_ada[0:1, :].partition_broadcast(128)), [], [mod_bc])
    fw.dma("sp", lambda e: e.dma_start(out=fg_bc[:, :], in_=final_g[0:1, :].partition_broadcast(128)), [], [fg_bc])
    fw.dma("sp", lambda e: e.dma_start(out=hg_bc[:, :], in_=head_g[0:1, :].partition_broadcast(128)), [], [hg_bc])
    fw.dma("sp", lambda e: e.dma_start(out=lng_bc[:, :], in_=ln_g[0:1, :].partition_broadcast(128)), [], [lng_bc])
    fw.dma("sp", lambda e: e.dma_start(out=lnb_bc[:, :], in_=ln_b[0:1, :].partition_broadcast(128)), [], [lnb_bc])
    fw.op("dve", lambda e: e.tensor_scalar(out=hg_bc[:, :], in0=hg_bc[:, :], scalar1=1.0 - LAM_INIT, scalar2=None, op0=OP.mult),
          [hg_bc], [hg_bc])

    with ExitStack() as es:
        def tl(name, shape, dt=F32):
            return es.enter_context(nc.sbuf_tensor(name, list(shape), dt))
        wa0_t = tl("s0a", [128, 8, 512]); wa1_t = tl("s0b", [128, 8, 512]); csbc_t = tl("s0c", [128, 8, 128])
        lam_t = tl("s0d", [128, 4, 64]); n2g_t = tl("s0e", [128, 1024]); mcol_t = tl("s0f", [128, 16])
        tmp0_t = tl("s0g", [128, 512]); lsc_t = tl("s0h", [128, 8])
        wab = [Buf(wa0_t), Buf(wa1_t)]
        csbc = Buf(csbc_t)
        lam4 = Buf(lam_t)
        n2g = Buf(n2g_t)
        mcol = Buf(mcol_t)
        tmp0 = Buf(tmp0_t)
        lsc = Buf(lsc_t)
        for i, src in enumerate((lq1, lk1, lq2, lk2)):
            fw.dma("sp", lambda e, i=i, src=src: e.dma_start(out=lam4[:, i, :], in_=src[0:1, :].partition_broadcast(128)), [], [lam4])
        fw.op("dve", lambda e: e.tensor_tensor(out=lam4[:, 0, :], in0=lam4[:, 0, :], in1=lam4[:, 1, :], op=OP.mult), [lam4], [lam4])
        fw.op("dve", lambda e: e.tensor_tensor(out=lam4[:, 2, :], in0=lam4[:, 2, :], in1=lam4[:, 3, :], op=OP.mult), [lam4], [lam4])
        fw.op("dve", lambda e: e.tensor_reduce(out=lsc[:, 0:1], in_=lam4[:, 0, :], axis=AX.X, op=OP.add), [lam4], [lsc])
        fw.op("dve", lambda e: e.tensor_reduce(out=lsc[:, 1:2], in_=lam4[:, 2, :], axis=AX.X, op=OP.add), [lam4], [lsc])
        fw.op("act", lambda e: e.activation(out=lsc[:, 2:4], in_=lsc[:, 0:2], func=AF.Exp), [lsc], [lsc])
        fw.op("dve", lambda e: e.tensor_tensor(out=lsc[:, 4:5], in0=lsc[:, 3:4], in1=lsc[:, 2:3], op=OP.subtract), [lsc], [lsc])
        fw.op("dve", lambda e: e.tensor_scalar(out=neglam[:, :], in0=lsc[:, 4:5], scalar1=-LAM_INIT, scalar2=None, op0=OP.add),
              [lsc], [neglam])
        fw.dma("sp", lambda e: e.dma_start(out=tmp0[:, 0:256], in_=amask[:, :]), [], [tmp0])
        fw.op("dve", lambda e: e.tensor_copy(out=maskb[:, :], in_=tmp0[:, 0:256]), [tmp0], [maskb])
        for g in range(4):
            fw.dma("sp", lambda e, g=g: e.dma_start(out=tmp0[:, 0:128], in_=sg_w[g, :, :]), [], [tmp0])
            fw.op("dve", lambda e: e.memset(tmp0[0:64, 64:128], 0.0), [tmp0], [tmp0])
            fw.op("pe", lambda e: e.transpose(out=bank(0)[:, 0:128], in_=tmp0[:, 0:128], identity=identf[:, :]), [tmp0, identf], [PR[0]])
            fw.op("act", lambda e, g=g: e.activation(out=wsT[:, g, :], in_=bank(0)[:, 0:128], func=AF.Copy), [PR[0]], [wsT])
        for l in range(16):
            fw.dma("sp", lambda e, l=l: e.dma_start(out=tmp0[:, 0:128], in_=sub_keys[l, :, :]), [], [tmp0])
            fw.op("pe", lambda e: e.transpose(out=bank(1)[:, 0:128], in_=tmp0[:, 0:128], identity=identf[:, :]), [tmp0, identf], [PR[1]])
            fw.op("act", lambda e, l=l: e.activation(out=skT[:, l, :], in_=bank(1)[:, 0:128], func=AF.Copy), [PR[1]], [skT])
        fw.op("act", lambda e: e.activation(out=c_col[:, :], in_=c_col[:, :], func=AF.Silu), [c_col], [c_col])
        fw.op("dve", lambda e: e.tensor_copy(out=csbc[:, :, :], in_=c_col[:, :].unsqueeze(2).to_broadcast([128, 8, 128])), [c_col], [csbc])
        w_ada_v = w_ada.rearrange("(c p) n -> p c n", p=128)
        for ec in range(12):
            wb_ = wab[ec % 2]
            fw.dma("sp" if ec % 2 == 0 else "act", lambda e, ec=ec, wb_=wb_: e.dma_start(out=wb_[:, :, :], in_=w_ada_v[:, :, ec * 512:(ec + 1) * 512]), [], [wb_])
            pb = 2 + (ec % 2)
            for c in range(8):
                fw.op("pe", lambda e, c=c, wb_=wb_, pb=pb: e.matmul(bank(pb), lhsT=csbc[:, c, :], rhs=wb_[:, c, :], start=(c == 0), stop=(c == 7)),
                      [csbc, wb_], [PR[pb]])
            fw.op("dve", lambda e, ec=ec, pb=pb: e.tensor_tensor(out=mod_bc[:, ec * 512:(ec + 1) * 512], in0=bank(pb), in1=mod_bc[:, ec * 512:(ec + 1) * 512], op=OP.add),
                  [PR[pb], mod_bc], [mod_bc])
        fw.dma("sp", lambda e: e.dma_start(out=modscr[0:1, :], in_=mod_bc[0:1, 0:2048]), [mod_bc], [r_modscr])
        with nc.allow_non_contiguous_dma(reason="tiny param loads"):
            fw.dma("sp", lambda e: e.dma_start(out=mcol[:, :], in_=modscr[0, :].rearrange("(c p) -> p c", p=128)), [r_modscr], [mcol])
        fw.op("dve", lambda e: e.tensor_copy(out=B1col[:, :], in_=mcol[:, 0:8]), [mcol], [B1col])
        fw.op("dve", lambda e: e.scalar_tensor_tensor(out=A1col[:, :], in0=mcol[:, 8:16], scalar=1.0, in1=n1g_col[:, :], op0=OP.add, op1=OP.mult),
              [mcol, n1g_col], [A1col])
        fw.dma("sp", lambda e: e.dma_start(out=n2g[:, :], in_=norm2_g[0:1, :].partition_broadcast(128)), [], [n2g])
        fw.op("dve", lambda e: e.scalar_tensor_tensor(out=mod_bc[:, 4 * D:5 * D], in0=mod_bc[:, 4 * D:5 * D], scalar=1.0, in1=n2g[:, :], op0=OP.add, op1=OP.mult),
              [mod_bc, n2g], [mod_bc])
        fw.barrier()
    if debug is not None and debug[0] == "mod":
        dbg_out(mod_bc[:, :], [mod_bc])
        fw.barrier()
        return nc

    A1bc = A1col[:, :].unsqueeze(2).to_broadcast([128, 8, 128])
    B1bc = B1col[:, :].unsqueeze(2).to_broadcast([128, 8, 128])

    def rstd_of(ssbuf, col, n, tmpcol):
        fw.op("dve", lambda e: e.tensor_scalar(out=ssbuf[:, tmpcol:tmpcol + 1], in0=ssbuf[:, col:col + 1], scalar1=1.0 / n, scalar2=EPS, op0=OP.mult, op1=OP.add),
              [ssbuf], [ssbuf])
        fw.op("act", lambda e: e.activation(out=ssbuf[:, tmpcol:tmpcol + 1], in_=ssbuf[:, tmpcol:tmpcol + 1], func=AF.Sqrt), [ssbuf], [ssbuf])
        fw.op("dve", lambda e: e.reciprocal(out=ssbuf[:, tmpcol:tmpcol + 1], in_=ssbuf[:, tmpcol:tmpcol + 1]), [ssbuf], [ssbuf])

    def norm1_block(src_rows, xin, xs, tmpf, hT, hcols, pb, add_eng="pool"):
        ssb = ss_r[ss_i[0] % 4]
        ss_i[0] += 1
        if src_rows is not None:
            fw.dma("sp", lambda e: e.dma_start(out=xin[:, :], in_=src_rows), [], [xin])
        fw.op("act", lambda e: e.activation(out=xs[:, :], in_=xin[:, :], func=AF.Square, accum_out=ssb[:, 0:1]), [xin], [xs, ssb])
        rstd_of(ssb, 0, D, 1)
        fw.op("dve", lambda e: e.tensor_scalar(out=xs[:, :], in0=xin[:, :], scalar1=ssb[:, 1:2], scalar2=None, op0=OP.mult), [xin, ssb], [xs])
        tpv = bank_bf(pb)
        for c in range(8):
            fw.op("pe", lambda e, c=c: e.transpose(out=tpv[:, c * 128:(c + 1) * 128], in_=xs[:, c * 128:(c + 1) * 128], identity=identb[:, :]),
                  [xs, identb], [PR[pb]])
        fw.op("dve", lambda e: e.tensor_tensor(out=tmpf[:, :].rearrange("p (c t) -> p c t", c=8), in0=tpv.rearrange("p (c t) -> p c t", c=8), in1=A1bc, op=OP.mult),
              [PR[pb], A1col], [tmpf])
        fw.op(add_eng, lambda e: e.tensor_tensor(out=hT[:, :, hcols], in0=tmpf[:, :].rearrange("p (c t) -> p c t", c=8), in1=B1bc, op=OP.add),
              [tmpf, B1col], [hT])

    winb_v = winb.rearrange("(c p) n -> p c n", p=128)

    for hp in range(2):
        with ExitStack() as es:
            def tl(name, shape, dt=F32):
                return es.enter_context(nc.sbuf_tensor(f"{name}_p{hp}", list(shape), dt))
            KT_t = tl("KT", [128, 2, S], BF16); V_t = tl("VV", [128, NB, 2, 129], BF16)
            wk_t = tl("wk", [128, 8, 256], BF16); wv_t = tl("wv", [128, 8, 256], BF16); wq_t = tl("wq", [128, 8, 256], BF16)
            xin0_t = tl("xin0", [128, D]); xin1_t = tl("xin1", [128, D]); xin2_t = tl("xin2", [128, D])
            xs0_t = tl("xs0", [128, D], BF16); xs1_t = tl("xs1", [128, D], BF16)
            tmpf0_t = tl("tmpf0", [128, D]); tmpf1_t = tl("tmpf1", [128, D])
            hT0_t = tl("hT0", [128, 8, 512], BF16); hT1_t = tl("hT1", [128, 8, 512], BF16)
            QT0_t = tl("QT0", [128, 2, 512], BF16); QT1_t = tl("QT1", [128, 2, 512], BF16)
            PT0_t = tl("PT0", [128, 512], BF16); PT1_t = tl("PT1", [128, 512], BF16); PT2_t = tl("PT2", [128, 512], BF16); PT3_t = tl("PT3", [128, 512], BF16)
            at0_t = tl("at0", [128, 128]); at1_t = tl("at1", [128, 128])
            ab0_t = tl("ab0", [128, 128], BF16); ab1_t = tl("ab1", [128, 128], BF16)
            asm_t = tl("asm", [128, 8])
            KT = KT_t
            V = V_t
            rKT = [Reg() for _ in range(16)]
            rV = [Reg() for _ in range(16)]
            wk, wv, wq = Buf(wk_t), Buf(wv_t), Buf(wq_t)
            xins = [Buf(xin0_t), Buf(xin1_t), Buf(xin2_t)]
            xss = [Buf(xs0_t), Buf(xs1_t)]
            tmpfs = [Buf(tmpf0_t), Buf(tmpf1_t)]
            hTs = [Buf(hT0_t), Buf(hT1_t)]
            QTs = [Buf(QT0_t), Buf(QT1_t)]
            PTs = [Buf(PT0_t), Buf(PT1_t), Buf(PT2_t), Buf(PT3_t)]
            ats = [Buf(at0_t), Buf(at1_t)]
            abs_ = [Buf(ab0_t), Buf(ab1_t)]
            asm = Buf(asm_t)
            qc0, kc0, vc0 = hp * 256, 512 + hp * 256, 1024 + hp * 256
            fw.dma("sp", lambda e: e.dma_start(out=wq[:, :, :], in_=winb_v[:, :, qc0:qc0 + 256]), wregs, [wq])
            fw.dma("sp", lambda e: e.dma_start(out=wk[:, :, :], in_=winb_v[:, :, kc0:kc0 + 256]), wregs, [wk])
            fw.dma("sp", lambda e: e.dma_start(out=wv[:, :, :], in_=winb_v[:, :, vc0:vc0 + 256]), wregs, [wv])
            rVall = Reg()
            fw.op("pool", lambda e: e.memset(V[:, :, :, 128:129], 1.0), [], [rVall])
            for r_ in rV:
                r_.w = dict(rVall.w)
            nbc = [0]

            def normS(sbi):
                if True:
                    hT = hTs[sbi % 2]
                    if hp == 0:
                        for dst_, src_, r0_ in tconv[2 * sbi:2 * sbi + 2]:
                            fw.dma("pool", lambda e: e.dma_start(out=dst_[r0_:r0_ + 1024, :], in_=src_[r0_:r0_ + 1024, :]), [], [Reg()])
                            tregs.append(fw.last_written)
                    for bl in range(4):
                        gb = sbi * 4 + bl
                        nb_ = nbc[0]
                        nbc[0] += 1
                        norm1_block(x_b[gb * 128:(gb + 1) * 128, :], xins[nb_ % 3], xss[nb_ % 2], tmpfs[nb_ % 2], hT,
                                    slice(bl * 128, (bl + 1) * 128), nb_ % 2)

            def kvS(sbi):
                if True:
                    hT = hTs[sbi % 2]
                    for hh in range(2):
                        pb = 2 + hh
                        for c in range(8):
                            fw.op("pe", lambda e: e.matmul(bank(pb), lhsT=wk[:, c, hh * 128:(hh + 1) * 128], rhs=hT[:, c, :], start=(c == 0), stop=(c == 7)),
                                  [wk, hT], [PR[pb]])
                        fw.op("act", lambda e: e.activation(out=KT[:, hh, sbi * 512:(sbi + 1) * 512], in_=bank(pb), func=AF.Copy),
                              [PR[pb]], [rKT[sbi]])
                    for bl in range(4):
                        gb = sbi * 4 + bl
                        pb = 4 + (bl % 2)
                        for c in range(8):
                            fw.op("pe", lambda e: e.matmul(bank(pb)[:, 0:256], lhsT=hT[:, c, bl * 128:(bl + 1) * 128], rhs=wv[:, c, :], start=(c == 0), stop=(c == 7)),
                                  [wv, hT], [PR[pb]])
                        fw.op("dve", lambda e: e.tensor_copy(out=V[:, gb, :, 0:128], in_=bank(pb)[:, 0:256].rearrange("p (h v) -> p h v", h=2)),
                              [PR[pb]], [rV[sbi]])

            normS(0)
            for sbi in range(16):
                if sbi + 1 < 16:
                    interleave(fw, [lambda: normS(sbi + 1), lambda: kvS(sbi)], [1, 1])
                else:
                    kvS(sbi)
            nblk = nbc[0]
            if debug is not None and debug[0] == "kv" and hp == 0:
                with nc.sbuf_tensor("dbgt", [128, 2048], F32) as dt_:
                    dtb = Buf(dt_)
                    fw.op("dve", lambda e: e.tensor_copy(out=dtb[:, 0:1024], in_=KT[:, 1, 7168:8192]), rKT, [dtb])
                    fw.op("dve", lambda e: e.tensor_copy(out=dtb[:, 1024:2048].rearrange("p (b v) -> p b v", b=8), in_=V[:, 56:64, 1, 0:128]), rV, [dtb])
                    dbg_out(dtb[:, :], [dtb])
                    fw.barrier()
                    return nc
            for g in range(8):
                hT = hTs[g % 2]
                QT = QTs[g % 2]
                for qi in range(4):
                    j = 4 * g + qi
                    norm1_block(x_own[j * 128:(j + 1) * 128, :], xins[nblk % 3], xss[nblk % 2], tmpfs[nblk % 2], hT,
                                slice(qi * 128, (qi + 1) * 128), nblk % 2)
                    nblk += 1
                for hh in range(2):
                    pb = 2 + hh
                    for c in range(8):
                        fw.op("pe", lambda e, c=c, hh=hh, pb=pb: e.matmul(bank(pb), lhsT=wq[:, c, hh * 128:(hh + 1) * 128], rhs=hT[:, c, :], start=(c == 0), stop=(c == 7)),
                              [wq, hT], [PR[pb]])
                    fw.op("act", lambda e, hh=hh, pb=pb: e.activation(out=QT[:, hh, :], in_=bank(pb), func=AF.Copy, scale=0.125),
                          [PR[pb]], [QT])
                for hh in range(2):
                    kmax = 8 * g + 7

                    def emit_S(kb):
                        qmin = max(0, (kb - 8 * g) // 2)
                        c0 = qmin * 128
                        for half in range(2):
                            spb = 4 + 2 * (kb % 2) + half
                            rs = slice(half * 64, (half + 1) * 64)
                            fw.op("pe", lambda e: e.matmul(bank(spb)[:, c0:512], lhsT=KT[rs, hh, kb * 128:(kb + 1) * 128], rhs=QT[rs, hh, c0:512], start=True, stop=True),
                                  [rKT[kb // 4], QT], [PR[spb]])
                        for half in range(2):
                            spb = 4 + 2 * (kb % 2) + half
                            PT = PTs[2 * (kb % 2) + half]
                            fw.op("act", lambda e: e.activation(out=PT[:, c0:512], in_=bank(spb)[:, c0:512], func=AF.Exp),
                                  [PR[spb]], [PT])
                            for qi in range(qmin, 4):
                                j = 4 * g + qi
                                m = kb - 2 * j
                                if m in (0, 1):
                                    fw.op("pool", lambda e: e.tensor_tensor(out=PT[:, qi * 128:(qi + 1) * 128], in0=PT[:, qi * 128:(qi + 1) * 128], in1=maskb[:, m * 128:(m + 1) * 128], op=OP.mult),
                                          [PT, maskb], [PT])

                    def emit_PV(kb):
                        qmin = max(0, (kb - 8 * g) // 2)
                        for half in range(2):
                            PT = PTs[2 * (kb % 2) + half]
                            for qi in range(qmin, 4):
                                j = 4 * g + qi
                                ob = qi
                                ov = bank(ob)[:, half * 129:(half + 1) * 129]
                                fw.op("pe", lambda e: e.matmul(ov, lhsT=PT[:, qi * 128:(qi + 1) * 128], rhs=V[:, kb, hh, :], start=(kb == 0 and half == 0), stop=(kb == 2 * j + 1 and half == 1)),
                                      [PT, rV[kb // 4]], [PR[ob]])

                    emit_S(0)
                    for kb in range(kmax + 1):
                        if kb + 1 <= kmax:
                            emit_S(kb + 1)
                        emit_PV(kb)
                    for qi in range(4):
                        j = 4 * g + qi
                        ob = qi
                        at = ats[qi % 2]
                        ab = abs_[qi % 2]
                        o3 = bank(ob)[:, 0:258].rearrange("p (a b) -> p a b", a=2)
                        fw.op("dve", lambda e, o3=o3: e.reciprocal(out=asm[:, 0:2], in_=o3[:, :, 128]), [PR[ob]], [asm])
                        fw.op("dve", lambda e: e.tensor_tensor(out=asm[:, 2:3], in0=asm[:, 1:2], in1=neglam[:, :], op=OP.mult), [asm, neglam], [asm])
                        fw.op("dve", lambda e, o3=o3, at=at: e.tensor_scalar(out=at[:, :], in0=o3[:, 0, 0:128], scalar1=asm[:, 0:1], scalar2=None, op0=OP.mult), [PR[ob], asm], [at])
                        fw.op("dve", lambda e, o3=o3, at=at: e.scalar_tensor_tensor(out=at[:, :], in0=o3[:, 1, 0:128], scalar=asm[:, 2:3], in1=at[:, :], op0=OP.mult, op1=OP.add), [PR[ob], asm, at], [at])
                        fw.op("act", lambda e, at=at, ab=ab: e.activation(out=ab[:, :], in_=at[:, :], func=AF.Square, accum_out=asm[:, 3:4]), [at], [ab, asm])
                        rstd_of(asm, 3, 128, 4)
                        fw.op("dve", lambda e, at=at, ab=ab: e.scalar_tensor_tensor(out=ab[:, :], in0=at[:, :], scalar=asm[:, 4:5], in1=hg_bc[:, :], op0=OP.mult, op1=OP.mult), [at, asm, hg_bc], [ab])
                        tb = 6 + (qi % 2)
                        fw.op("pe", lambda e, ab=ab, tb=tb: e.transpose(out=bank_bf(tb)[:, 0:128], in_=ab[:, :], identity=identb[:, :]), [ab, identb], [PR[tb]])
                        yst = ysts[ysi[0] % 2]
                        ysi[0] += 1
                        fw.op("act", lambda e, tb=tb: e.activation(out=yst[:, :], in_=bank_bf(tb)[:, 0:128], func=AF.Copy), [PR[tb]], [yst])
                        rr_ = Reg()
                        fw.dma("sp", lambda e, j=j, hh=hh: e.dma_start(out=yaTd[hp * 2 + hh, :, j * 128:(j + 1) * 128], in_=yst[:, :]), [yst], [rr_])
                        yaregs.append(rr_)
            fw.barrier()
    def tl(name, shape, dt=F32):
        return Buf(nc.alloc_sbuf_tensor(name, list(shape), dt))

    wbab_v = wbab.rearrange("(c p) n -> p c n", p=128)
    wbbb_v = wbbb.rearrange("(c p) n -> p c n", p=128)
    woutb_v = woutb.rearrange("(c p) n -> p c n", p=128)
    wpqb_v = wpqb.rearrange("(c p) n -> p c n", p=128)
    Wsl = [tl(f"Wsl{i}", [128, 4096], BF16) for i in range(5)]
    wi = [0]

    def load_w(view3, c):
        w = Wsl[wi[0] % 5]
        wi[0] += 1
        fw.dma("sp", lambda e: e.dma_start(out=w[:, :].rearrange("p (c n) -> p c n", c=c), in_=view3), wregs, [w])
        return w, w[:, :].rearrange("p (c n) -> p c n", c=c)

    xgs = [[tl(f"xg{p}{b}", [128, D]) for b in range(2)] for p in range(2)]
    xsC = tl("xsC", [128, D], BF16)
    tmpfC = tl("tmpfC", [128, D])
    hTC = tl("hTC", [128, 8, 256], BF16)
    gTC = tl("gTC", [128, 16, 256], BF16)
    gu = tl("gu", [128, 512])
    gs = tl("gs", [128, 512])
    svn = tl("svn", [128, 512], BF16)
    ybt = tl("ybt", [128, 512], BF16)
    ybT = tl("ybT", [128, 4, 256], BF16)
    t5a = tl("t5a", [128, 256])
    t5b = tl("t5b", [128, 256])
    mT = tl("mT", [128, 8, 256], BF16)
    h2gs = [[tl(f"h2g{p}{b}", [128, D], BF16) for b in range(2)] for p in range(2)]
    st6 = tl("st6", [128, 12])
    scf = tl("scf", [128, 2048])
    scw = tl("scw", [128, 2048])
    tv = tl("tv", [128, 256])
    ti = tl("ti", [128, 256], U32)
    tif = tl("tif", [128, 256])
    cv = tl("cv", [128, 128])
    ci = tl("ci", [128, 128], U32)
    cif = tl("cif", [128, 128])
    gsm = tl("gsm", [128, 128])
    sm8 = tl("sm8", [128, 8])
    ak = tl("ak", [128, 128])
    bk = tl("bk", [128, 128])
    i1s = tl("i1s", [128, 128])
    i2s = tl("i2s", [128, 128])
    eidxf = tl("eidxf", [128, 128])
    eTs = [[tl(f"eT{p}{b}", [128, 128], U32) for b in range(2)] for p in range(2)]
    gTs_ = [[tl(f"gT_{p}{b}", [128, 128]) for b in range(2)] for p in range(2)]
    yaGs = [tl("yaG0", [128, 4, 256], BF16), tl("yaG1", [128, 4, 256], BF16)]
    tmpfL = ViewBuf(mod_bc.t[:, 0:1024])
    junkL = ViewBuf(mod_bc.t[:, 1024:2048].bitcast(BF16)[:, 0:1024])
    aT = tl("aT", [128, 128])
    coefT = tl("coefT", [128, 128])
    NGB = 9
    Gbufs = [tl(f"Gbuf{i}", [128, 2 * D], BF16) for i in range(NGB)]
    gbi = [0]

    gslots = [[f"d_g{i}", nc.alloc_semaphore(f"d_g{i}"), 0] for i in range(NGB)]

    def gather(table, idx_ap, rd):
        k = gbi[0] % NGB
        b_ = Gbufs[k]
        pre = [Gbufs[(k + 1) % NGB]] if k % 2 == 0 else []
        gbi[0] += 1
        fw.dma("pool", lambda e: e.indirect_dma_start(out=b_[:, :], out_offset=None, in_=table[:, :], in_offset=bass.IndirectOffsetOnAxis(ap=idx_ap, axis=0)),
               rd, [b_], slot=gslots[k], pre=pre)
        return b_
    Wc = tl("Wc", [128, 256], BF16)
    Zts = [tl(f"Zt{i}", [128, 128], BF16) for i in range(4)]
    acols = [tl(f"acol{i}", [128, 1]) for i in range(8)]
    ccols = [tl(f"ccol{i}", [128, 1]) for i in range(8)]
    thr16 = tl("thr16", [128, 16])
    io16 = tl("io16", [128, 16])
    ioi = tl("ioi", [128, 16], I32)

    fw.op("pool", lambda e: e.memset(Wc[:, :], 0.0), [], [Wc])
    fw.op("pool", lambda e: e.memset(Wc[:, 127:128], 1.0), [Wc], [Wc])
    fw.op("pool", lambda e: e.iota(ioi[:, :], pattern=[[1, 16]], base=0, channel_multiplier=0), [], [ioi])
    fw.op("dve", lambda e: e.tensor_copy(out=io16[:, :], in_=ioi[:, :]), [ioi], [io16])
    fw.op("dve", lambda e: e.tensor_scalar(out=thr16[:, :], in0=io16[:, :], scalar1=16.0, scalar2=None, op0=OP.mult), [io16], [thr16])
    def top16(src3, wrk3, vals3, idx3, n, regs):
        r_src, r_wrk, r_vals, r_idx = regs
        for l in range(n):
            fw.op("dve", lambda e: e.max(out=vals3[:, l, 0:8], in_=src3[:, l, :]), [r_src], [r_vals])
            fw.op("dve", lambda e: e.max_index(out=idx3[:, l, 0:8], in_max=vals3[:, l, 0:8], in_values=src3[:, l, :]), [r_src, r_vals], [r_idx])
            fw.op("dve", lambda e: e.match_replace(out=wrk3[:, l, :], in_to_replace=vals3[:, l, 0:8], in_values=src3[:, l, :], imm_value=NEG), [r_src, r_vals], [r_wrk])
            fw.op("dve", lambda e: e.max(out=vals3[:, l, 8:16], in_=wrk3[:, l, :]), [r_wrk], [r_vals])
            fw.op("dve", lambda e: e.max_index(out=idx3[:, l, 8:16], in_max=vals3[:, l, 8:16], in_values=wrk3[:, l, :]), [r_wrk, r_vals], [r_idx])

    NGC = 16 if debug is None else debug[2]

    def cmain(g):
        p = g % 2
        xg = xgs[p]
        h2g = h2gs[p]
        for bl in range(2):
            j = 2 * g + bl
            fw.dma("sp", lambda e: e.dma_start(out=xg[bl][:, :], in_=x_own[j * 128:(j + 1) * 128, :]), [], [xg[bl]])
            norm1_block(None, xg[bl], xsC, tmpfC, hTC, slice(bl * 128, (bl + 1) * 128), bl, add_eng="dve")
        for u in range(4):
            w, w3 = load_w(winb_v[:, :, 2560 + u * 512:2560 + (u + 1) * 512], 8)
            for cc in range(4):
                ch = u * 4 + cc
                pb = ch % 2
                for c in range(8):
                    fw.op("pe", lambda e: e.matmul(bank(pb)[:, 0:256], lhsT=w3[:, c, cc * 128:(cc + 1) * 128], rhs=hTC[:, c, :], start=(c == 0), stop=(c == 7)),
                          [w, hTC], [PR[pb]])
                fw.op("act", lambda e: e.activation(out=gTC[:, ch, :], in_=bank(pb)[:, 0:256], func=AF.Sigmoid), [PR[pb]], [gTC])
        wu, wu3 = load_w(winb_v[:, :, 1536:2048], 8)
        wsv, wsv3 = load_w(winb_v[:, :, 2048:2560], 8)
        for bl in range(2):
            bs_ = slice(bl * 128, (bl + 1) * 128)
            for c in range(8):
                fw.op("pe", lambda e: e.matmul(bank(0), lhsT=hTC[:, c, bs_], rhs=wu3[:, c, :], start=(c == 0), stop=(c == 7)), [wu, hTC], [PR[0]])
            for c in range(8):
                fw.op("pe", lambda e: e.matmul(bank(1), lhsT=hTC[:, c, bs_], rhs=wsv3[:, c, :], start=(c == 0), stop=(c == 7)), [wsv, hTC], [PR[1]])
            fw.op("act", lambda e: e.activation(out=gu[:, :], in_=bank(0), func=AF.Gelu_apprx_tanh), [PR[0]], [gu])
            fw.op("act", lambda e: e.activation(out=gs[:, :], in_=bank(1), func=AF.Gelu_apprx_tanh), [PR[1]], [gs])
            fw.op("dve", lambda e: e.bn_stats(out=st6[:, 0:6], in_=gs[:, :]), [gs], [st6])
            fw.op("dve", lambda e: e.bn_aggr(out=st6[:, 6:8], in_=st6[:, 0:6]), [st6], [st6])
            rstd_of(st6, 7, 1.0, 8)
            fw.op("dve", lambda e: e.tensor_scalar(out=gs[:, :], in0=gs[:, :], scalar1=st6[:, 6:7], scalar2=st6[:, 8:9], op0=OP.subtract, op1=OP.mult), [gs, st6], [gs])
            fw.op("dve", lambda e: e.tensor_tensor(out=gs[:, :], in0=gs[:, :], in1=lng_bc[:, :], op=OP.mult), [gs, lng_bc], [gs])
            fw.op("dve", lambda e: e.tensor_tensor(out=svn[:, :], in0=gs[:, :], in1=lnb_bc[:, :], op=OP.add), [gs, lnb_bc], [svn])
            for gr in range(4):
                fw.op("pe", lambda e: e.matmul(bank(0)[:, gr * 128:(gr + 1) * 128], lhsT=wsT[:, gr, :], rhs=svn[:, gr * 128:(gr + 1) * 128], start=True, stop=True),
                      [wsT, svn], [PR[0]])
            for gr in range(4):
                fw.op("dve", lambda e: e.scalar_tensor_tensor(out=ybt[:, gr * 128:(gr + 1) * 128], in0=bank(0)[:, gr * 128:(gr + 1) * 128], scalar=bs_col[:, gr:gr + 1], in1=gu[:, gr * 128:(gr + 1) * 128], op0=OP.add, op1=OP.mult),
                      [PR[0], bs_col, gu], [ybt])
            for ch in range(4):
                fw.op("pe", lambda e: e.transpose(out=bank_bf(1)[:, ch * 128:(ch + 1) * 128], in_=ybt[:, ch * 128:(ch + 1) * 128], identity=identb[:, :]), [ybt, identb], [PR[1]])
            fw.op("act", lambda e: e.activation(out=ybT[:, :, bs_], in_=bank_bf(1)[:, 0:512].rearrange("p (c t) -> p c t", c=4), func=AF.Copy), [PR[1]], [ybT])
        wa, wa3 = load_w(wbab_v, 4)
        wb, wb3 = load_w(wbbb_v, 4)
        yaG = yaGs[p]
        fw.dma("sp", lambda e: e.dma_start(out=yaG[:, :, :], in_=yaTd[:, :, g * 256:(g + 1) * 256].rearrange("c p t -> p c t")), yaregs, [yaG])
        for dc in range(8):
            ba, bb = 0, 1
            for cc in range(4):
                fw.op("pe", lambda e: e.matmul(bank(ba)[:, 0:256], lhsT=wa3[:, cc, dc * 128:(dc + 1) * 128], rhs=yaG[:, cc, :], start=(cc == 0), stop=(cc == 3)),
                      [wa, yaG], [PR[ba]])
            for cc in range(4):
                fw.op("pe", lambda e: e.matmul(bank(bb)[:, 0:256], lhsT=wb3[:, cc, dc * 128:(dc + 1) * 128], rhs=ybT[:, cc, :], start=(cc == 0), stop=(cc == 3)),
                      [wb, ybT], [PR[bb]])
            fw.op("dve", lambda e: e.tensor_tensor(out=t5a[:, :], in0=bank(ba)[:, 0:256], in1=gTC[:, dc, :], op=OP.mult), [PR[ba], gTC], [t5a])
            fw.op("dve", lambda e: e.tensor_tensor(out=t5b[:, :], in0=bank(bb)[:, 0:256], in1=gTC[:, 8 + dc, :], op=OP.mult), [PR[bb], gTC], [t5b])
            fw.op("dve", lambda e: e.tensor_tensor(out=mT[:, dc, :], in0=t5a[:, :], in1=t5b[:, :], op=OP.add), [t5a, t5b], [mT])
        wo0, wo03 = load_w(woutb_v[:, :, 0:512], 8)
        wo1, wo13 = load_w(woutb_v[:, :, 512:1024], 8)
        for bl in range(2):
            bs_ = slice(bl * 128, (bl + 1) * 128)
            for half, (wo, wo3) in enumerate(((wo0, wo03), (wo1, wo13))):
                for c in range(8):
                    fw.op("pe", lambda e: e.matmul(bank(half), lhsT=mT[:, c, bs_], rhs=wo3[:, c, :], start=(c == 0), stop=(c == 7)), [wo, mT], [PR[half]])
            fw.op("dve", lambda e: e.tensor_tensor(out=tmpfC[:, :], in0=pair(0), in1=mod_bc[:, 2 * D:3 * D], op=OP.mult), [PR[0], PR[1], mod_bc], [tmpfC])
            fw.op("dve", lambda e: e.tensor_tensor(out=xg[bl][:, :], in0=xg[bl][:, :], in1=tmpfC[:, :], op=OP.add), [xg[bl], tmpfC], [xg[bl]])
        if debug is not None and debug[0] == "x2":
            dbg_out(xg[0][:, :], [xg[0]], dbg_d[:, 0:D])
            dbg_out(xg[1][:, :], [xg[1]], dbg_d[:, D:2 * D])
            return
        for bl in range(2):
            ssb = ss_r[ss_i[0] % 4]
            ss_i[0] += 1
            fw.op("act", lambda e: e.activation(out=xsC[:, :], in_=xg[bl][:, :], func=AF.Square, accum_out=ssb[:, 0:1]), [xg[bl]], [xsC, ssb])
            rstd_of(ssb, 0, D, 1)
            fw.op("dve", lambda e: e.scalar_tensor_tensor(out=tmpfC[:, :], in0=xg[bl][:, :], scalar=ssb[:, 1:2], in1=mod_bc[:, 4 * D:5 * D], op0=OP.mult, op1=OP.mult), [xg[bl], ssb, mod_bc], [tmpfC])
            fw.op("dve", lambda e: e.tensor_tensor(out=h2g[bl][:, :], in0=tmpfC[:, :], in1=mod_bc[:, 3 * D:4 * D], op=OP.add), [tmpfC, mod_bc], [h2g[bl]])
            for c in range(8):
                fw.op("pe", lambda e: e.transpose(out=bank_bf(bl)[:, c * 128:(c + 1) * 128], in_=h2g[bl][:, c * 128:(c + 1) * 128], identity=identb[:, :]), [h2g[bl], identb], [PR[bl]])
            fw.op("act", lambda e: e.activation(out=hTC[:, :, bl * 128:(bl + 1) * 128], in_=bank_bf(bl)[:, 0:1024].rearrange("p (c t) -> p c t", c=8), func=AF.Copy), [PR[bl]], [hTC])
        for u in range(4):
            w, w3 = load_w(wpqb_v[:, :, u * 512:(u + 1) * 512], 8)
            for cc in range(4):
                l = u * 4 + cc
                pb = l % 2
                for c in range(8):
                    fw.op("pe", lambda e: e.matmul(bank(pb)[:, 0:256], lhsT=w3[:, c, cc * 128:(cc + 1) * 128], rhs=hTC[:, c, :], start=(c == 0), stop=(c == 7)), [w, hTC], [PR[pb]])
                fw.op("act", lambda e: e.activation(out=gTC[:, l, :], in_=bank(pb)[:, 0:256], func=AF.Copy), [PR[pb]], [gTC])
        for bl in range(2):
            eT = eTs[p][bl]
            gT_ = gTs_[p][bl]
            bs_ = slice(bl * 128, (bl + 1) * 128)
            for rnd in range(2):
                for l8 in range(8):
                    l = rnd * 8 + l8
                    fw.op("pe", lambda e: e.matmul(bank(l8 // 4)[:, (l8 % 4) * 128:(l8 % 4 + 1) * 128], lhsT=gTC[:, l, bs_], rhs=skT[:, l, :], start=True, stop=True), [gTC, skT], [PR[l8 // 4]])
                if rnd == 0:
                    fw.op("act", lambda e: e.activation(out=scf[:, 0:1024], in_=pair(0), func=AF.Copy), [PR[0], PR[1]], [scf])
                else:
                    fw.op("dve", lambda e: e.tensor_copy(out=scf[:, 1024:2048], in_=pair(0)), [PR[0], PR[1]], [scf])
            r_src, r_wrk, r_vals, r_idx = scf, scw, tv, ti
            top16(scf[:, :].rearrange("p (l n) -> p l n", l=16), scw[:, :].rearrange("p (l n) -> p l n", l=16),
                  tv[:, :].rearrange("p (l k) -> p l k", l=16), ti[:, :].rearrange("p (l k) -> p l k", l=16), 16, (scf, scw, tv, ti))
            fw.op("dve", lambda e: e.tensor_copy(out=tif[:, :], in_=ti[:, :]), [ti], [tif])
            tv4 = tv[:, :].rearrange("p (h i k) -> p h i k", h=8, i=2)
            tif4 = tif[:, :].rearrange("p (h i k) -> p h i k", h=8, i=2)
            cand4 = scf[:, :].rearrange("p (h a b) -> p h a b", h=8, a=16)
            fw.op("dve", lambda e: e.tensor_tensor(out=cand4, in0=tv4[:, :, 0, :].unsqueeze(3).to_broadcast([128, 8, 16, 16]), in1=tv4[:, :, 1, :].unsqueeze(2).to_broadcast([128, 8, 16, 16]), op=OP.add), [tv], [scf])
            top16(scf[:, :].rearrange("p (h n) -> p h n", h=8), scw[:, :].rearrange("p (h n) -> p h n", h=8),
                  cv[:, :].rearrange("p (h k) -> p h k", h=8), ci[:, :].rearrange("p (h k) -> p h k", h=8), 8, (scf, scw, cv, ci))
            cv3 = cv[:, :].rearrange("p (h k) -> p h k", h=8)
            gsm3 = gsm[:, :].rearrange("p (h k) -> p h k", h=8)
            fw.op("dve", lambda e: e.tensor_tensor(out=gsm3, in0=cv3, in1=cv3[:, :, 0:1].to_broadcast([128, 8, 16]), op=OP.subtract), [cv], [gsm])
            fw.op("act", lambda e: e.activation(out=gsm[:, :], in_=gsm[:, :], func=AF.Exp), [gsm], [gsm])
            fw.op("dve", lambda e: e.tensor_reduce(out=sm8[:, :], in_=gsm3, axis=AX.X, op=OP.add), [gsm], [sm8])
            fw.op("dve", lambda e: e.reciprocal(out=sm8[:, :], in_=sm8[:, :]), [sm8], [sm8])
            fw.op("dve", lambda e: e.tensor_tensor(out=gsm3, in0=gsm3, in1=sm8[:, :].unsqueeze(2).to_broadcast([128, 8, 16]), op=OP.mult), [gsm, sm8], [gsm])
            fw.op("dve", lambda e: e.tensor_copy(out=cif[:, :], in_=ci[:, :]), [ci], [cif])
            oh4 = scw[:, :].rearrange("p (h k a) -> p h k a", h=8, k=16)
            cif3 = cif[:, :].rearrange("p (h k) -> p h k", h=8)
            ak3 = ak[:, :].rearrange("p (h k) -> p h k", h=8)
            bk3 = bk[:, :].rearrange("p (h k) -> p h k", h=8)
            thr_b = thr16[:, :].unsqueeze(1).unsqueeze(1).to_broadcast([128, 8, 16, 16])
            io_b = io16[:, :].unsqueeze(1).unsqueeze(1).to_broadcast([128, 8, 16, 16])
            fw.op("dve", lambda e: e.tensor_tensor(out=oh4, in0=cif3.unsqueeze(3).to_broadcast([128, 8, 16, 16]), in1=thr_b, op=OP.is_ge), [cif, thr16], [scw])
            fw.op("dve", lambda e: e.tensor_reduce(out=ak3, in_=oh4, axis=AX.X, op=OP.add), [scw], [ak])
            fw.op("dve", lambda e: e.tensor_scalar(out=ak[:, :], in0=ak[:, :], scalar1=-1.0, scalar2=None, op0=OP.add), [ak], [ak])
            fw.op("dve", lambda e: e.scalar_tensor_tensor(out=bk[:, :], in0=ak[:, :], scalar=-16.0, in1=cif[:, :], op0=OP.mult, op1=OP.add), [ak, cif], [bk])
            for (kk3, pidx, dst) in ((ak3, 0, i1s), (bk3, 1, i2s)):
                fw.op("dve", lambda e: e.tensor_tensor(out=oh4, in0=kk3.unsqueeze(3).to_broadcast([128, 8, 16, 16]), in1=io_b, op=OP.is_equal), [ak, bk, io16], [scw])
                fw.op("dve", lambda e: e.tensor_tensor(out=oh4, in0=oh4, in1=tif4[:, :, pidx, :].unsqueeze(2).to_broadcast([128, 8, 16, 16]), op=OP.mult), [scw, tif], [scw])
                fw.op("dve", lambda e: e.tensor_reduce(out=dst[:, :].rearrange("p (h k) -> p h k", h=8), in_=oh4, axis=AX.X, op=OP.add), [scw], [dst])
            fw.op("dve", lambda e: e.scalar_tensor_tensor(out=eidxf[:, :], in0=i1s[:, :], scalar=128.0, in1=i2s[:, :], op0=OP.mult, op1=OP.add), [i1s, i2s], [eidxf])
            fw.op("pe", lambda e: e.transpose(out=bank(0)[:, 0:128], in_=eidxf[:, :], identity=identf[:, :]), [eidxf, identf], [PR[0]])
            fw.op("dve", lambda e: e.tensor_copy(out=eT[:, :], in_=bank(0)[:, 0:128]), [PR[0]], [eT])
            fw.op("pe", lambda e: e.transpose(out=bank(1)[:, 0:128], in_=gsm[:, :], identity=identf[:, :]), [gsm, identf], [PR[1]])
            fw.op("act", lambda e: e.activation(out=gT_[:, :], in_=bank(1)[:, 0:128], func=AF.Copy), [PR[1]], [gT_])

    def loops(g):
        p = g % 2
        xg = xgs[p]
        h2g = h2gs[p]
        for bl in range(2):
            j = 2 * g + bl
            eT = eTs[p][bl]
            gT_ = gTs_[p][bl]
            gbs = {}

            def s_gather(t):
                gbs[t] = gather(pdu, eT[:, t:t + 1], [eT] + tregs)

            def s_bcast(t):
                hbp = 2 + (t % 2)
                for half in range(2):
                    fw.op("pe", lambda e: e.matmul(bank(2 * hbp + half), lhsT=identb[:, t:t + 1].to_broadcast([128, 128]), rhs=h2g[bl][:, half * 512:(half + 1) * 512], start=True, stop=True),
                          [identb, h2g[bl]], [PR[2 * hbp + half]])

            def s_dot(t):
                hbp = 2 + (t % 2)
                ac = acols[t % 8]
                cc_ = ccols[t % 8]
                gb_ = gbs[t]
                fw.op("dve", lambda e: e.scalar_tensor_tensor(out=junkL[:, :], in0=gb_[:, 0:D], scalar=1.0, in1=pair(hbp), op0=OP.mult, op1=OP.mult, accum_out=ac[:, 0:1]),
                      [gb_, PR[2 * hbp], PR[2 * hbp + 1]], [junkL, ac])
                fw.op("act", lambda e: e.activation(out=cc_[:, 0:1], in_=ac[:, 0:1], func=AF.Gelu_apprx_tanh), [ac], [cc_])

            def s_up(t):
                cc_ = ccols[t % 8]
                zt = Zts[t % 4]
                gb_ = gbs.pop(t)
                fw.op("dve", lambda e: e.tensor_scalar(out=zt[:, :], in0=Wc[:, 127 - t:255 - t], scalar1=cc_[:, 0:1], scalar2=gT_[:, t:t + 1], op0=OP.mult, op1=OP.mult),
                      [Wc, cc_, gT_], [zt])
                for half in range(2):
                    fw.op("pe", lambda e: e.matmul(bank(2 + half), lhsT=zt[:, :], rhs=gb_[:, D + half * 512:D + (half + 1) * 512], start=(t == 0), stop=(t == 127)),
                          [zt, gb_], [PR[2 + half]])

            AH = NGB - 2
            for t in range(min(AH, 128)):
                s_gather(t)
            s_bcast(0)
            for t in range(128):
                if t + AH < 128:
                    s_gather(t + AH)
                if t + 1 < 128:
                    s_bcast(t + 1)
                s_dot(t)
                if t >= 1:
                    s_up(t - 1)
            s_up(127)
            fw.op("dve", lambda e: e.tensor_tensor(out=tmpfL[:, :], in0=pair(1), in1=mod_bc[:, 5 * D:6 * D], op=OP.mult), [PR[2], PR[3], mod_bc], [tmpfL])
            fw.op("dve", lambda e: e.tensor_tensor(out=xg[bl][:, :], in0=xg[bl][:, :], in1=tmpfL[:, :], op=OP.add), [xg[bl], tmpfL], [xg[bl]])
            ssb = ss_r[ss_i[0] % 4]
            ss_i[0] += 1
            fw.op("act", lambda e: e.activation(out=junkL[:, :], in_=xg[bl][:, :], func=AF.Square, accum_out=ssb[:, 0:1]), [xg[bl]], [junkL, ssb])
            rstd_of(ssb, 0, D, 1)
            fw.op("dve", lambda e: e.scalar_tensor_tensor(out=tmpfL[:, :], in0=xg[bl][:, :], scalar=ssb[:, 1:2], in1=fg_bc[:, :], op0=OP.mult, op1=OP.mult), [xg[bl], ssb, fg_bc], [tmpfL])
            fw.dma("sp", lambda e: e.dma_start(out=out_d[j * 128:(j + 1) * 128, :], in_=tmpfL[:, :]), [tmpfL], [r_none])

    cmain(0)
    if debug is not None and debug[0] == "x2":
        fw.barrier()
        return nc
    for g in range(NGC):
        if g + 1 < NGC:
            interleave(fw, [lambda: loops(g), lambda: cmain(g + 1)], [2, 1])
        else:
            loops(g)
    fw.barrier()
    return nc


def _prep_inputs(inputs):
    g = {k: np.asarray(v) for k, v in inputs.items()}
    x = np.ascontiguousarray(g["x"], dtype=np.float32)
    shared = {
        "w_ada": g["w_ada"][0], "b_ada": g["b_ada"], "norm1_g": g["norm1_g"], "w_in": g["w_in"][0],
        "da_lambda_q1": g["da_lambda_q1"], "da_lambda_k1": g["da_lambda_k1"],
        "da_lambda_q2": g["da_lambda_q2"], "da_lambda_k2": g["da_lambda_k2"],
        "da_head_g": g["da_head_g"], "sg_ln_g": g["sg_ln_g"], "sg_ln_b": g["sg_ln_b"],
        "sg_w": g["sg_w"][0], "sg_b": g["sg_b"][0], "w_branch_a": g["w_branch_a"][0],
        "w_branch_b": g["w_branch_b"][0], "w_out": g["w_out"][0], "norm2_g": g["norm2_g"],
        "peer_w_query": g["peer_w_query"][0], "peer_sub_keys": g["peer_sub_keys"][0].reshape(16, 128, 128),
        "peer_down": g["peer_down"][0], "peer_up": g["peer_up"][0], "final_g": g["final_g"].reshape(1, D),
    }
    shared = {k: np.ascontiguousarray(v, dtype=np.float32) for k, v in shared.items()}
    kk = np.arange(128)[:, None] // 64
    qq = np.arange(128)[None, :] // 64
    diag = (kk <= qq).astype(np.float32)
    in_maps = []
    for core in range(8):
        b, par = core // 2, core % 2
        xb = x[b]
        xo = np.ascontiguousarray(xb.reshape(NB, 128, D)[par::2].reshape(NOWN * 128, D))
        if par == 0:
            am = np.concatenate([diag, np.zeros((128, 128), np.float32)], axis=1)
        else:
            am = np.concatenate([np.ones((128, 128), np.float32), diag], axis=1)
        m = dict(shared)
        m["x_b"] = xb
        m["x_own"] = xo
        m["c_b"] = np.ascontiguousarray(g["c"][b:b + 1], dtype=np.float32)
        m["amask"] = np.ascontiguousarray(am)
        in_maps.append(m)
    return in_maps


def kernel(**inputs):
    in_maps = _prep_inputs(inputs)
    nc = build()
    res = run_bass_kernel_spmd(nc, in_maps, core_ids=list(range(8)))
    out = np.zeros((4, S, D), np.float32)
    for core in range(8):
        b, par = core // 2, core % 2
        o = np.asarray(res.results[core]["out"]).reshape(NOWN, 128, D)
        out[b].reshape(NB, 128, D)[par::2] = o
    return out
```

```python
import numpy as np
from contextlib import ExitStack
import concourse.bass as bass
import concourse.mybir as mybir
from concourse.bass_utils import run_bass_kernel_spmd

F32 = mybir.dt.float32
BF16 = mybir.dt.bfloat16
I32 = mybir.dt.int32
U32 = mybir.dt.uint32
AF = mybir.ActivationFunctionType
OP = mybir.AluOpType
AX = mybir.AxisListType

D = 1024
S = 8192
NB = 64
NOWN = 32
EPS = 1e-6
LAM_INIT = 0.8 - 0.6 * 1.0
NEG = -1.0e30
SEM_LIMIT = 30000


class Reg:
    def __init__(self):
        self.w = {}
        self.r = {}


class Buf(Reg):
    def __init__(self, t):
        Reg.__init__(self)
        self.t = t

    def __getitem__(self, k):
        return self.t[k]


class FW:
    def __init__(self, nc):
        self.nc = nc
        self.engs = dict(pe=nc.tensor, act=nc.scalar, dve=nc.vector, pool=nc.gpsimd, sp=nc.sync)
        self.cur = {}
        self.nsem = 0
        self.known = {k: {} for k in self.engs}
        for k in self.engs:
            self._newsem(k)
        self.dsem = {}
        for q, n in (("sp", 12), ("act", 4), ("pool", 12)):
            self.dsem[q] = [[f"d_{q}{i}", nc.alloc_semaphore(f"d_{q}{i}"), 0] for i in range(n)]
        self.drr = {q: 0 for q in self.dsem}
        self.ninst = 0
        self.hook = None

    def _newsem(self, k):
        name = f"e_{k}{self.nsem}"
        self.cur[k] = [name, self.nc.alloc_semaphore(name), 0]
        self.nsem += 1

    def _deps(self, reads, writes):
        deps = {}

        def add(d):
            for name, (s, v) in d.items():
                if name not in deps or deps[name][1] < v:
                    deps[name] = (s, v)
        for b in reads:
            add(b.w)
        for b in writes:
            add(b.w)
            add(b.r)
        return deps

    def _wait(self, k, deps):
        eng = self.engs[k]
        for name, (s, v) in deps.items():
            if k == "pe" and name.startswith("e_pe"):
                continue
            if self.known[k].get(name, 0) >= v:
                continue
            eng.wait_ge(s, v)
            self.known[k][name] = v
            self.ninst += 1

    def _record(self, ev, reads, writes):
        name, s, v = ev
        for b in reads:
            b.r[name] = (s, v)
        for b in writes:
            b.w = {name: (s, v)}
            b.r = {}

    def op(self, k, fn, reads, writes):
        self._wait(k, self._deps(reads, writes))
        ins = fn(self.engs[k])
        c = self.cur[k]
        if c[2] >= SEM_LIMIT:
            self._newsem(k)
            c = self.cur[k]
        c[2] += 1
        ins.then_inc(c[1], 1)
        self.ninst += 1
        self._record((c[0], c[1], c[2]), reads, writes)
        if self.hook is not None:
            self.hook()

    def dma(self, q, fn, reads, writes, slot=None, pre=()):
        if slot is None:
            slots = self.dsem[q]
            i = self.drr[q]
            self.drr[q] = (i + 1) % len(slots)
            sl = slots[i]
        else:
            sl = slot
        deps = self._deps(reads, list(writes) + list(pre))
        if slot is None and sl[2] > 0:
            deps[sl[0]] = (sl[1], sl[2])
        self._wait(q, deps)
        ins = fn(self.engs[q])
        sl[2] += 16
        ins.then_inc(sl[1], 16)
        self.ninst += 1
        self._record((sl[0], sl[1], sl[2]), reads, writes)
        self.last_written = writes[0] if writes else None
        if self.hook is not None:
            self.hook()

    def barrier(self):
        evs = {}
        for k, c in self.cur.items():
            if c[2] > 0:
                evs[c[0]] = (c[1], c[2])
        for q, slots in self.dsem.items():
            for sl in slots:
                if sl[2] > 0:
                    evs[sl[0]] = (sl[1], sl[2])
        for k in self.engs:
            self._wait(k, dict(evs))


def interleave(fw, fns, weights):
    import threading
    n = len(fns)
    sems = [threading.Semaphore(0) for _ in fns]
    done = [False] * n
    st = {"cur": 0, "cnt": 0}
    errs = []
    fin = threading.Semaphore(0)

    def nxt(i):
        for k in range(1, n + 1):
            j = (i + k) % n
            if not done[j]:
                return j
        return None

    def hook():
        i = st["cur"]
        st["cnt"] += 1
        if st["cnt"] >= weights[i]:
            st["cnt"] = 0
            j = nxt(i)
            if j is not None and j != i:
                st["cur"] = j
                sems[j].release()
                sems[i].acquire()

    def runner(i):
        sems[i].acquire()
        try:
            fns[i]()
        except BaseException as e:
            errs.append(e)
        done[i] = True
        j = nxt(i)
        if j is not None:
            st["cur"] = j
            st["cnt"] = 0
            sems[j].release()
        else:
            fin.release()

    ths = [threading.Thread(target=runner, args=(i,)) for i in range(n)]
    old = fw.hook
    fw.hook = hook
    for t in ths:
        t.start()
    st["cur"] = 0
    sems[0].release()
    fin.acquire()
    for t in ths:
        t.join()
    fw.hook = old
    if errs:
        raise errs[0]


class ViewBuf(Reg):
    def __init__(self, ap):
        Reg.__init__(self)
        self.ap_ = ap

    def __getitem__(self, k):
        return self.ap_[k]


def build(debug=None):
    nc = bass.Bass("TRN2", target_bir_lowering=False)
    fw = FW(nc)

    def din(name, shape, dt=F32):
        return nc.dram_tensor(name, list(shape), dt, kind="ExternalInput").ap()

    x_b = din("x_b", [S, D])
    x_own = din("x_own", [NOWN * 128, D])
    c_b = din("c_b", [1, D])
    w_ada = din("w_ada", [D, 6 * D])
    b_ada = din("b_ada", [1, 6 * D])
    norm1_g = din("norm1_g", [1, D])
    w_in = din("w_in", [D, 4608])
    lq1 = din("da_lambda_q1", [1, 64])
    lk1 = din("da_lambda_k1", [1, 64])
    lq2 = din("da_lambda_q2", [1, 64])
    lk2 = din("da_lambda_k2", [1, 64])
    head_g = din("da_head_g", [1, 128])
    ln_g = din("sg_ln_g", [1, 512])
    ln_b = din("sg_ln_b", [1, 512])
    sg_w = din("sg_w", [4, 128, 128])
    sg_b = din("sg_b", [4, 128])
    w_ba = din("w_branch_a", [512, D])
    w_bb = din("w_branch_b", [512, D])
    w_out = din("w_out", [D, D])
    norm2_g = din("norm2_g", [1, D])
    w_pq = din("peer_w_query", [D, 2048])
    sub_keys = din("peer_sub_keys", [16, 128, 128])
    p_down = din("peer_down", [16384, D])
    p_up = din("peer_up", [16384, D])
    final_g = din("final_g", [1, D])
    amask = din("amask", [128, 256])
    out_d = nc.dram_tensor("out", [NOWN * 128, D], F32, kind="ExternalOutput").ap()
    dbg_d = None
    if debug is not None:
        dbg_d = nc.dram_tensor("dbg", list(debug[1]), F32, kind="ExternalOutput").ap()

    winb = nc.dram_tensor("winb", [D, 4608], BF16, kind="Internal").ap()
    wunits = nc.dram_tensor("wunits", [14, 128, 4096], BF16, kind="Internal").ap()
    modscr = nc.dram_tensor("modscr", [1, 2048], F32, kind="Internal").ap()
    pdu = nc.dram_tensor("pdu", [16384, 2 * D], BF16, kind="Internal").ap()
    tregs = []
    tconv = [(pdu[:, hf * D:(hf + 1) * D], src, r0) for hf, src in enumerate((p_down, p_up)) for r0 in range(0, 16384, 1024)]
    wregs = []
    r_modscr = Reg()
    r_none = Reg()
    yaregs = []

    def sb(name, shape, dt=F32):
        return Buf(nc.alloc_sbuf_tensor(name, list(shape), dt))

    P2 = [nc.alloc_psum_tensor(f"ps{i}", [128, 1024], F32) for i in range(4)]
    PR = [Reg() for _ in range(8)]

    def bank(i):
        return P2[i // 2][:, (i % 2) * 512:(i % 2 + 1) * 512]

    def bank_bf(i):
        return P2[i // 2][:, :].bitcast(BF16)[:, (i % 2) * 1024:(i % 2 + 1) * 1024]

    def pair(i):
        return P2[i][:, :]

    identf = sb("identf", [128, 128])
    identb = sb("identb", [128, 128], BF16)
    mod_bc = sb("mod_bc", [128, 6 * D])
    fg_bc = sb("fg_bc", [128, D])
    yaTd = nc.dram_tensor("yaTd", [4, 128, NOWN * 128], BF16, kind="Internal").ap()
    r_yaT = [Reg() for _ in range(8)]
    ysts = [sb("yst0", [128, 128], BF16), sb("yst1", [128, 128], BF16)]
    ysi = [0]
    A1col = sb("A1col", [128, 8])
    B1col = sb("B1col", [128, 8])
    neglam = sb("neglam", [128, 1])
    hg_bc = sb("hg_bc", [128, 128])
    lng_bc = sb("lng_bc", [128, 512])
    lnb_bc = sb("lnb_bc", [128, 512])
    bs_col = sb("bs_col", [128, 4])
    maskb = sb("maskb", [128, 256], BF16)
    wsT = sb("wsT", [128, 4, 128], BF16)
    skT = sb("skT", [128, 16, 128], BF16)
    ss_r = [sb(f"ss{i}", [128, 4]) for i in range(4)]
    ss_i = [0]

    def dbg_out(buf_ap, reads, dst=None):
        fw.dma("sp", lambda e: e.dma_start(out=(dbg_d if dst is None else dst), in_=buf_ap), reads, [r_none])

    fw.op("pool", lambda e: e.memset(identf[:, :], 1.0), [], [identf])
    fw.op("pool", lambda e: e.affine_select(out=identf[:, :], in_=identf[:, :], pattern=[[-1, 128]],
                                            compare_op=OP.is_equal, fill=0.0, base=0, channel_multiplier=1),
          [identf], [identf])
    fw.op("dve", lambda e: e.tensor_copy(out=identb[:, :], in_=identf[:, :]), [identf], [identb])

    for c0 in range(0, 1536, 512):
        fw.dma("pool", lambda e, c0=c0: e.dma_start(out=winb[:, c0:c0 + 512], in_=w_in[:, c0:c0 + 512]), [], [Reg()])
        wregs.append(fw.last_written)
    w_in_v = w_in.rearrange("(c p) n -> p c n", p=128)
    w_out_v = w_out.rearrange("(c p) n -> p c n", p=128)
    w_pq_v = w_pq.rearrange("(c p) n -> p c n", p=128)
    unit_src = ([(w_in_v[:, :, 2560 + u * 512:2560 + (u + 1) * 512], 8) for u in range(4)]
                + [(w_in_v[:, :, 1536:2048], 8), (w_in_v[:, :, 2048:2560], 8)]
                + [(w_ba.rearrange("(c p) n -> p c n", p=128), 4), (w_bb.rearrange("(c p) n -> p c n", p=128), 4)]
                + [(w_out_v[:, :, 0:512], 8), (w_out_v[:, :, 512:1024], 8)]
                + [(w_pq_v[:, :, u * 512:(u + 1) * 512], 8) for u in range(4)])
    for k_, (src_, c_) in enumerate(unit_src):
        fw.dma("pool", lambda e, k_=k_, src_=src_, c_=c_: e.dma_start(out=wunits[k_, :, :].rearrange("p (c n) -> p c n", c=c_), in_=src_), [], [Reg()])
        wregs.append(fw.last_written)

    with nc.allow_non_contiguous_dma(reason="tiny param loads"):
        n1g_col = sb("n1g_col", [128, 8])
        c_col = sb("c_col", [128, 8])
        fw.dma("sp", lambda e: e.dma_start(out=n1g_col[:, :], in_=norm1_g[0, :].rearrange("(c p) -> p c", p=128)), [], [n1g_col])
        fw.dma("sp", lambda e: e.dma_start(out=c_col[:, :], in_=c_b[0, :].rearrange("(c p) -> p c", p=128)), [], [c_col])
        fw.dma("sp", lambda e: e.dma_start(out=bs_col[:, :], in_=sg_b.rearrange("g p -> p g")), [], [bs_col])
    fw.dma("sp", lambda e: e.dma_start(out=mod_bc[:, :], in_=b_ada[0:1, :].partition_broadcast(128)), [], [mod_bc])
    fw.dma("sp", lambda e: e.dma_start(out=fg_bc[:, :], in_=final_g[0:1, :].partition_broadcast(128)), [], [fg_bc])
    fw.dma("sp", lambda e: e.dma_start(out=hg_bc[:, :], in_=head_g[0:1, :].partition_broadcast(128)), [], [hg_bc])
    fw.dma("sp", lambda e: e.dma_start(out=lng_bc[:, :], in_=ln_g[0:1, :].partition_broadcast(128)), [], [lng_bc])
    fw.dma("sp", lambda e: e.dma_start(out=lnb_bc[:, :], in_=ln_b[0:1, :].partition_broadcast(128)), [], [lnb_bc])
    fw.op("dve", lambda e: e.tensor_scalar(out=hg_bc[:, :], in0=hg_bc[:, :], scalar1=1.0 - LAM_INIT, scalar2=None, op0=OP.mult),
          [hg_bc], [hg_bc])

    with ExitStack() as es:
        def tl(name, shape, dt=F32):
            return es.enter_context(nc.sbuf_tensor(name, list(shape), dt))
        wa0_t = tl("s0a", [128, 8, 512]); wa1_t = tl("s0b", [128, 8, 512]); csbc_t = tl("s0c", [128, 8, 128])
        lam_t = tl("s0d", [128, 4, 64]); n2g_t = tl("s0e", [128, 1024]); mcol_t = tl("s0f", [128, 16])
        tmp0_t = tl("s0g", [128, 512]); lsc_t = tl("s0h", [128, 8])
        wab = [Buf(wa0_t), Buf(wa1_t)]
        csbc = Buf(csbc_t)
        lam4 = Buf(lam_t)
        n2g = Buf(n2g_t)
        mcol = Buf(mcol_t)
        tmp0 = Buf(tmp0_t)
        lsc = Buf(lsc_t)
        for i, src in enumerate((lq1, lk1, lq2, lk2)):
            fw.dma("sp", lambda e, i=i, src=src: e.dma_start(out=lam4[:, i, :], in_=src[0:1, :].partition_broadcast(128)), [], [lam4])
        fw.op("dve", lambda e: e.tensor_tensor(out=lam4[:, 0, :], in0=lam4[:, 0, :], in1=lam4[:, 1, :], op=OP.mult), [lam4], [lam4])
        fw.op("dve", lambda e: e.tensor_tensor(out=lam4[:, 2, :], in0=lam4[:, 2, :], in1=lam4[:, 3, :], op=OP.mult), [lam4], [lam4])
        fw.op("dve", lambda e: e.tensor_reduce(out=lsc[:, 0:1], in_=lam4[:, 0, :], axis=AX.X, op=OP.add), [lam4], [lsc])
        fw.op("dve", lambda e: e.tensor_reduce(out=lsc[:, 1:2], in_=lam4[:, 2, :], axis=AX.X, op=OP.add), [lam4], [lsc])
        fw.op("act", lambda e: e.activation(out=lsc[:, 2:4], in_=lsc[:, 0:2], func=AF.Exp), [lsc], [lsc])
        fw.op("dve", lambda e: e.tensor_tensor(out=lsc[:, 4:5], in0=lsc[:, 3:4], in1=lsc[:, 2:3], op=OP.subtract), [lsc], [lsc])
        fw.op("dve", lambda e: e.tensor_scalar(out=neglam[:, :], in0=lsc[:, 4:5], scalar1=-LAM_INIT, scalar2=None, op0=OP.add),
              [lsc], [neglam])
        fw.dma("sp", lambda e: e.dma_start(out=tmp0[:, 0:256], in_=amask[:, :]), [], [tmp0])
        fw.op("dve", lambda e: e.tensor_copy(out=maskb[:, :], in_=tmp0[:, 0:256]), [tmp0], [maskb])
        for g in range(4):
            fw.dma("sp", lambda e, g=g: e.dma_start(out=tmp0[:, 0:128], in_=sg_w[g, :, :]), [], [tmp0])
            fw.op("dve", lambda e: e.memset(tmp0[0:64, 64:128], 0.0), [tmp0], [tmp0])
            fw.op("pe", lambda e: e.transpose(out=bank(0)[:, 0:128], in_=tmp0[:, 0:128], identity=identf[:, :]), [tmp0, identf], [PR[0]])
            fw.op("act", lambda e, g=g: e.activation(out=wsT[:, g, :], in_=bank(0)[:, 0:128], func=AF.Copy), [PR[0]], [wsT])
        for l in range(16):
            fw.dma("sp", lambda e, l=l: e.dma_start(out=tmp0[:, 0:128], in_=sub_keys[l, :, :]), [], [tmp0])
            fw.op("pe", lambda e: e.transpose(out=bank(1)[:, 0:128], in_=tmp0[:, 0:128], identity=identf[:, :]), [tmp0, identf], [PR[1]])
            fw.op("act", lambda e, l=l: e.activation(out=skT[:, l, :], in_=bank(1)[:, 0:128], func=AF.Copy), [PR[1]], [skT])
        fw.op("act", lambda e: e.activation(out=c_col[:, :], in_=c_col[:, :], func=AF.Silu), [c_col], [c_col])
        fw.op("dve", lambda e: e.tensor_copy(out=csbc[:, :, :], in_=c_col[:, :].unsqueeze(2).to_broadcast([128, 8, 128])), [c_col], [csbc])
        w_ada_v = w_ada.rearrange("(c p) n -> p c n", p=128)
        for ec in range(12):
            wb_ = wab[ec % 2]
            fw.dma("sp" if ec % 2 == 0 else "act", lambda e, ec=ec, wb_=wb_: e.dma_start(out=wb_[:, :, :], in_=w_ada_v[:, :, ec * 512:(ec + 1) * 512]), [], [wb_])
            pb = 2 + (ec % 2)
            for c in range(8):
                fw.op("pe", lambda e, c=c, wb_=wb_, pb=pb: e.matmul(bank(pb), lhsT=csbc[:, c, :], rhs=wb_[:, c, :], start=(c == 0), stop=(c == 7)),
                      [csbc, wb_], [PR[pb]])
            fw.op("dve", lambda e, ec=ec, pb=pb: e.tensor_tensor(out=mod_bc[:, ec * 512:(ec + 1) * 512], in0=bank(pb), in1=mod_bc[:, ec * 512:(ec + 1) * 512], op=OP.add),
                  [PR[pb], mod_bc], [mod_bc])
        fw.dma("sp", lambda e: e.dma_start(out=modscr[0:1, :], in_=mod_bc[0:1, 0:2048]), [mod_bc], [r_modscr])
        with nc.allow_non_contiguous_dma(reason="tiny param loads"):
            fw.dma("sp", lambda e: e.dma_start(out=mcol[:, :], in_=modscr[0, :].rearrange("(c p) -> p c", p=128)), [r_modscr], [mcol])
        fw.op("dve", lambda e: e.tensor_copy(out=B1col[:, :], in_=mcol[:, 0:8]), [mcol], [B1col])
        fw.op("dve", lambda e: e.scalar_tensor_tensor(out=A1col[:, :], in0=mcol[:, 8:16], scalar=1.0, in1=n1g_col[:, :], op0=OP.add, op1=OP.mult),
              [mcol, n1g_col], [A1col])
        fw.dma("sp", lambda e: e.dma_start(out=n2g[:, :], in_=norm2_g[0:1, :].partition_broadcast(128)), [], [n2g])
        fw.op("dve", lambda e: e.scalar_tensor_tensor(out=mod_bc[:, 4 * D:5 * D], in0=mod_bc[:, 4 * D:5 * D], scalar=1.0, in1=n2g[:, :], op0=OP.add, op1=OP.mult),
              [mod_bc, n2g], [mod_bc])
        fw.barrier()
    if debug is not None and debug[0] == "mod":
        dbg_out(mod_bc[:, :], [mod_bc])
        fw.barrier()
        return nc

    A1bc = A1col[:, :].unsqueeze(2).to_broadcast([128, 8, 128])
    B1bc = B1col[:, :].unsqueeze(2).to_broadcast([128, 8, 128])

    def rstd_of(ssbuf, col, n, tmpcol):
        fw.op("dve", lambda e: e.tensor_scalar(out=ssbuf[:, tmpcol:tmpcol + 1], in0=ssbuf[:, col:col + 1], scalar1=1.0 / n, scalar2=EPS, op0=OP.mult, op1=OP.add),
              [ssbuf], [ssbuf])
        fw.op("act", lambda e: e.activation(out=ssbuf[:, tmpcol:tmpcol + 1], in_=ssbuf[:, tmpcol:tmpcol + 1], func=AF.Sqrt), [ssbuf], [ssbuf])
        fw.op("dve", lambda e: e.reciprocal(out=ssbuf[:, tmpcol:tmpcol + 1], in_=ssbuf[:, tmpcol:tmpcol + 1]), [ssbuf], [ssbuf])

    def norm1_block(src_rows, xin, xs, tmpf, hT, hcols, pb, add_eng="pool"):
        ssb = ss_r[ss_i[0] % 4]
        ss_i[0] += 1
        if src_rows is not None:
            fw.dma("sp", lambda e: e.dma_start(out=xin[:, :], in_=src_rows), [], [xin])
        fw.op("act", lambda e: e.activation(out=xs[:, :], in_=xin[:, :], func=AF.Square, accum_out=ssb[:, 0:1]), [xin], [xs, ssb])
        rstd_of(ssb, 0, D, 1)
        fw.op("dve", lambda e: e.tensor_scalar(out=xs[:, :], in0=xin[:, :], scalar1=ssb[:, 1:2], scalar2=None, op0=OP.mult), [xin, ssb], [xs])
        tpv = bank_bf(pb)
        for c in range(8):
            fw.op("pe", lambda e, c=c: e.transpose(out=tpv[:, c * 128:(c + 1) * 128], in_=xs[:, c * 128:(c + 1) * 128], identity=identb[:, :]),
                  [xs, identb], [PR[pb]])
        fw.op("dve", lambda e: e.tensor_tensor(out=tmpf[:, :].rearrange("p (c t) -> p c t", c=8), in0=tpv.rearrange("p (c t) -> p c t", c=8), in1=A1bc, op=OP.mult),
              [PR[pb], A1col], [tmpf])
        fw.op(add_eng, lambda e: e.tensor_tensor(out=hT[:, :, hcols], in0=tmpf[:, :].rearrange("p (c t) -> p c t", c=8), in1=B1bc, op=OP.add),
              [tmpf, B1col], [hT])

    winb_v = winb.rearrange("(c p) n -> p c n", p=128)

    for hp in range(2):
        with ExitStack() as es:
            def tl(name, shape, dt=F32):
                return es.enter_context(nc.sbuf_tensor(f"{name}_p{hp}", list(shape), dt))
            KT_t = tl("KT", [128, 2, S], BF16); V_t = tl("VV", [128, NB, 2, 129], BF16)
            wk_t = tl("wk", [128, 8, 256], BF16); wv_t = tl("wv", [128, 8, 256], BF16); wq_t = tl("wq", [128, 8, 256], BF16)
            xin0_t = tl("xin0", [128, D]); xin1_t = tl("xin1", [128, D]); xin2_t = tl("xin2", [128, D])
            xs0_t = tl("xs0", [128, D], BF16); xs1_t = tl("xs1", [128, D], BF16)
            tmpf0_t = tl("tmpf0", [128, D]); tmpf1_t = tl("tmpf1", [128, D])
            hT0_t = tl("hT0", [128, 8, 512], BF16); hT1_t = tl("hT1", [128, 8, 512], BF16)
            QT0_t = tl("QT0", [128, 2, 512], BF16); QT1_t = tl("QT1", [128, 2, 512], BF16)
            PT0_t = tl("PT0", [128, 512], BF16); PT1_t = tl("PT1", [128, 512], BF16); PT2_t = tl("PT2", [128, 512], BF16); PT3_t = tl("PT3", [128, 512], BF16)
            at0_t = tl("at0", [128, 128]); at1_t = tl("at1", [128, 128])
            ab0_t = tl("ab0", [128, 128], BF16); ab1_t = tl("ab1", [128, 128], BF16)
            asm_t = tl("asm", [128, 8])
            KT = KT_t
            V = V_t
            rKT = [Reg() for _ in range(16)]
            rV = [Reg() for _ in range(16)]
            wk, wv, wq = Buf(wk_t), Buf(wv_t), Buf(wq_t)
            xins = [Buf(xin0_t), Buf(xin1_t), Buf(xin2_t)]
            xss = [Buf(xs0_t), Buf(xs1_t)]
            tmpfs = [Buf(tmpf0_t), Buf(tmpf1_t)]
            hTs = [Buf(hT0_t), Buf(hT1_t)]
            QTs = [Buf(QT0_t), Buf(QT1_t)]
            PTs = [Buf(PT0_t), Buf(PT1_t), Buf(PT2_t), Buf(PT3_t)]
            ats = [Buf(at0_t), Buf(at1_t)]
            abs_ = [Buf(ab0_t), Buf(ab1_t)]
            asm = Buf(asm_t)
            qc0, kc0, vc0 = hp * 256, 512 + hp * 256, 1024 + hp * 256
            fw.dma("sp", lambda e: e.dma_start(out=wq[:, :, :], in_=winb_v[:, :, qc0:qc0 + 256]), wregs, [wq])
            fw.dma("sp", lambda e: e.dma_start(out=wk[:, :, :], in_=winb_v[:, :, kc0:kc0 + 256]), wregs, [wk])
            fw.dma("sp", lambda e: e.dma_start(out=wv[:, :, :], in_=winb_v[:, :, vc0:vc0 + 256]), wregs, [wv])
            rVall = Reg()
            fw.op("pool", lambda e: e.memset(V[:, :, :, 128:129], 1.0), [], [rVall])
            for r_ in rV:
                r_.w = dict(rVall.w)
            nbc = [0]

            def normS(sbi):
                if True:
                    hT = hTs[sbi % 2]
                    if hp == 0:
                        for dst_, src_, r0_ in tconv[2 * sbi:2 * sbi + 2]:
                            fw.dma("pool", lambda e: e.dma_start(out=dst_[r0_:r0_ + 1024, :], in_=src_[r0_:r0_ + 1024, :]), [], [Reg()])
                            tregs.append(fw.last_written)
                    for bl in range(4):
                        gb = sbi * 4 + bl
                        nb_ = nbc[0]
                        nbc[0] += 1
                        norm1_block(x_b[gb * 128:(gb + 1) * 128, :], xins[nb_ % 3], xss[nb_ % 2], tmpfs[nb_ % 2], hT,
                                    slice(bl * 128, (bl + 1) * 128), nb_ % 2)

            def kvS(sbi):
                if True:
                    hT = hTs[sbi % 2]
                    for hh in range(2):
                        pb = 2 + hh
                        for c in range(8):
                            fw.op("pe", lambda e: e.matmul(bank(pb), lhsT=wk[:, c, hh * 128:(hh + 1) * 128], rhs=hT[:, c, :], start=(c == 0), stop=(c == 7)),
                                  [wk, hT], [PR[pb]])
                        fw.op("act", lambda e: e.activation(out=KT[:, hh, sbi * 512:(sbi + 1) * 512], in_=bank(pb), func=AF.Copy),
                              [PR[pb]], [rKT[sbi]])
                    for bl in range(4):
                        gb = sbi * 4 + bl
                        pb = 4 + (bl % 2)
                        for c in range(8):
                            fw.op("pe", lambda e: e.matmul(bank(pb)[:, 0:256], lhsT=hT[:, c, bl * 128:(bl + 1) * 128], rhs=wv[:, c, :], start=(c == 0), stop=(c == 7)),
                                  [wv, hT], [PR[pb]])
                        fw.op("dve", lambda e: e.tensor_copy(out=V[:, gb, :, 0:128], in_=bank(pb)[:, 0:256].rearrange("p (h v) -> p h v", h=2)),
                              [PR[pb]], [rV[sbi]])

            normS(0)
            for sbi in range(16):
                if sbi + 1 < 16:
                    interleave(fw, [lambda: normS(sbi + 1), lambda: kvS(sbi)], [1, 1])
                else:
                    kvS(sbi)
            nblk = nbc[0]
            if debug is not None and debug[0] == "kv" and hp == 0:
                with nc.sbuf_tensor("dbgt", [128, 2048], F32) as dt_:
                    dtb = Buf(dt_)
                    fw.op("dve", lambda e: e.tensor_copy(out=dtb[:, 0:1024], in_=KT[:, 1, 7168:8192]), rKT, [dtb])
                    fw.op("dve", lambda e: e.tensor_copy(out=dtb[:, 1024:2048].rearrange("p (b v) -> p b v", b=8), in_=V[:, 56:64, 1, 0:128]), rV, [dtb])
                    dbg_out(dtb[:, :], [dtb])
                    fw.barrier()
                    return nc
            for g in range(8):
                hT = hTs[g % 2]
                QT = QTs[g % 2]
                for qi in range(4):
                    j = 4 * g + qi
                    norm1_block(x_own[j * 128:(j + 1) * 128, :], xins[nblk % 3], xss[nblk % 2], tmpfs[nblk % 2], hT,
                                slice(qi * 128, (qi + 1) * 128), nblk % 2)
                    nblk += 1
                for hh in range(2):
                    pb = 2 + hh
                    for c in range(8):
                        fw.op("pe", lambda e, c=c, hh=hh, pb=pb: e.matmul(bank(pb), lhsT=wq[:, c, hh * 128:(hh + 1) * 128], rhs=hT[:, c, :], start=(c == 0), stop=(c == 7)),
                              [wq, hT], [PR[pb]])
                    fw.op("act", lambda e, hh=hh, pb=pb: e.activation(out=QT[:, hh, :], in_=bank(pb), func=AF.Copy, scale=0.125),
                          [PR[pb]], [QT])
                for hh in range(2):
                    kmax = 8 * g + 7

                    def emit_S(kb):
                        qmin = max(0, (kb - 8 * g) // 2)
                        c0 = qmin * 128
                        for half in range(2):
                            spb = 4 + 2 * (kb % 2) + half
                            rs = slice(half * 64, (half + 1) * 64)
                            fw.op("pe", lambda e: e.matmul(bank(spb)[:, c0:512], lhsT=KT[rs, hh, kb * 128:(kb + 1) * 128], rhs=QT[rs, hh, c0:512], start=True, stop=True),
                                  [rKT[kb // 4], QT], [PR[spb]])
                        for half in range(2):
                            spb = 4 + 2 * (kb % 2) + half
                            PT = PTs[2 * (kb % 2) + half]
                            fw.op("act", lambda e: e.activation(out=PT[:, c0:512], in_=bank(spb)[:, c0:512], func=AF.Exp),
                                  [PR[spb]], [PT])
                            for qi in range(qmin, 4):
                                j = 4 * g + qi
                                m = kb - 2 * j
                                if m in (0, 1):
                                    fw.op("pool", lambda e: e.tensor_tensor(out=PT[:, qi * 128:(qi + 1) * 128], in0=PT[:, qi * 128:(qi + 1) * 128], in1=maskb[:, m * 128:(m + 1) * 128], op=OP.mult),
                                          [PT, maskb], [PT])

                    def emit_PV(kb):
                        qmin = max(0, (kb - 8 * g) // 2)
                        for half in range(2):
                            PT = PTs[2 * (kb % 2) + half]
                            for qi in range(qmin, 4):
                                j = 4 * g + qi
                                ob = qi
                                ov = bank(ob)[:, half * 129:(half + 1) * 129]
                                fw.op("pe", lambda e: e.matmul(ov, lhsT=PT[:, qi * 128:(qi + 1) * 128], rhs=V[:, kb, hh, :], start=(kb == 0 and half == 0), stop=(kb == 2 * j + 1 and half == 1)),
                                      [PT, rV[kb // 4]], [PR[ob]])

                    emit_S(0)
                    for kb in range(kmax + 1):
                        if kb + 1 <= kmax:
                            emit_S(kb + 1)
                        emit_PV(kb)
                    for qi in range(4):
                        j = 4 * g + qi
                        ob = qi
                        at = ats[qi % 2]
                        ab = abs_[qi % 2]
                        o3 = bank(ob)[:, 0:258].rearrange("p (a b) -> p a b", a=2)
                        fw.op("dve", lambda e, o3=o3: e.reciprocal(out=asm[:, 0:2], in_=o3[:, :, 128]), [PR[ob]], [asm])
                        fw.op("dve", lambda e: e.tensor_tensor(out=asm[:, 2:3], in0=asm[:, 1:2], in1=neglam[:, :], op=OP.mult), [asm, neglam], [asm])
                        fw.op("dve", lambda e, o3=o3, at=at: e.tensor_scalar(out=at[:, :], in0=o3[:, 0, 0:128], scalar1=asm[:, 0:1], scalar2=None, op0=OP.mult), [PR[ob], asm], [at])
                        fw.op("dve", lambda e, o3=o3, at=at: e.scalar_tensor_tensor(out=at[:, :], in0=o3[:, 1, 0:128], scalar=asm[:, 2:3], in1=at[:, :], op0=OP.mult, op1=OP.add), [PR[ob], asm, at], [at])
                        fw.op("act", lambda e, at=at, ab=ab: e.activation(out=ab[:, :], in_=at[:, :], func=AF.Square, accum_out=asm[:, 3:4]), [at], [ab, asm])
                        rstd_of(asm, 3, 128, 4)
                        fw.op("dve", lambda e, at=at, ab=ab: e.scalar_tensor_tensor(out=ab[:, :], in0=at[:, :], scalar=asm[:, 4:5], in1=hg_bc[:, :], op0=OP.mult, op1=OP.mult), [at, asm, hg_bc], [ab])
                        tb = 6 + (qi % 2)
                        fw.op("pe", lambda e, ab=ab, tb=tb: e.transpose(out=bank_bf(tb)[:, 0:128], in_=ab[:, :], identity=identb[:, :]), [ab, identb], [PR[tb]])
                        yst = ysts[ysi[0] % 2]
                        ysi[0] += 1
                        fw.op("act", lambda e, tb=tb: e.activation(out=yst[:, :], in_=bank_bf(tb)[:, 0:128], func=AF.Copy), [PR[tb]], [yst])
                        rr_ = Reg()
                        fw.dma("sp", lambda e, j=j, hh=hh: e.dma_start(out=yaTd[hp * 2 + hh, :, j * 128:(j + 1) * 128], in_=yst[:, :]), [yst], [rr_])
                        yaregs.append(rr_)
            fw.barrier()
    def tl(name, shape, dt=F32):
        return Buf(nc.alloc_sbuf_tensor(name, list(shape), dt))

    Wsl = [tl(f"Wsl{i}", [128, 4096], BF16) for i in range(5)]
    wi = [0]

    def load_w(k_, c):
        w = Wsl[wi[0] % 5]
        wi[0] += 1
        fw.dma("sp", lambda e: e.dma_start(out=w[:, :], in_=wunits[k_, :, :]), wregs, [w])
        return w, w[:, :].rearrange("p (c n) -> p c n", c=c)

    xgs = [[tl(f"xg{p}{b}", [128, D]) for b in range(2)] for p in range(2)]
    xsC = tl("xsC", [128, D], BF16)
    tmpfC = tl("tmpfC", [128, D])
    hTC = tl("hTC", [128, 8, 256], BF16)
    gTC = tl("gTC", [128, 16, 256], BF16)
    gu = tl("gu", [128, 512])
    gs = tl("gs", [128, 512])
    svn = tl("svn", [128, 512], BF16)
    ybt = tl("ybt", [128, 512], BF16)
    ybT = tl("ybT", [128, 4, 256], BF16)
    t5a = tl("t5a", [128, 256])
    t5b = tl("t5b", [128, 256])
    mT = tl("mT", [128, 8, 256], BF16)
    h2gs = [[tl(f"h2g{p}{b}", [128, D], BF16) for b in range(2)] for p in range(2)]
    st6 = tl("st6", [128, 12])
    scf = tl("scf", [128, 2048])
    scw = tl("scw", [128, 2048])
    tv = tl("tv", [128, 256])
    ti = tl("ti", [128, 256], U32)
    tif = tl("tif", [128, 256])
    cv = tl("cv", [128, 128])
    ci = tl("ci", [128, 128], U32)
    cif = tl("cif", [128, 128])
    gsm = tl("gsm", [128, 128])
    sm8 = tl("sm8", [128, 8])
    ak = tl("ak", [128, 128])
    bk = tl("bk", [128, 128])
    i1s = tl("i1s", [128, 128])
    i2s = tl("i2s", [128, 128])
    eidxf = tl("eidxf", [128, 128])
    eTs = [[tl(f"eT{p}{b}", [128, 128], U32) for b in range(2)] for p in range(2)]
    gTs_ = [[tl(f"gT_{p}{b}", [128, 128]) for b in range(2)] for p in range(2)]
    yaGs = [tl("yaG0", [128, 4, 256], BF16), tl("yaG1", [128, 4, 256], BF16)]
    tmpfL = ViewBuf(mod_bc.t[:, 0:1024])
    junkL = ViewBuf(mod_bc.t[:, 1024:2048].bitcast(BF16)[:, 0:1024])
    aT = tl("aT", [128, 128])
    coefT = tl("coefT", [128, 128])
    NGB = 9
    Gbufs = [tl(f"Gbuf{i}", [128, 2 * D], BF16) for i in range(NGB)]
    gbi = [0]

    gslots = [[f"d_g{i}", nc.alloc_semaphore(f"d_g{i}"), 0] for i in range(NGB)]

    def gather(table, idx_ap, rd):
        k = gbi[0] % NGB
        b_ = Gbufs[k]
        pre = [Gbufs[(k + 1) % NGB]] if k % 2 == 0 else []
        gbi[0] += 1
        fw.dma("pool", lambda e: e.indirect_dma_start(out=b_[:, :], out_offset=None, in_=table[:, :], in_offset=bass.IndirectOffsetOnAxis(ap=idx_ap, axis=0)),
               rd, [b_], slot=gslots[k], pre=pre)
        return b_
    Wc = tl("Wc", [128, 256], BF16)
    Zts = [tl(f"Zt{i}", [128, 128], BF16) for i in range(4)]
    acols = [tl(f"acol{i}", [128, 1]) for i in range(8)]
    ccols = [tl(f"ccol{i}", [128, 1]) for i in range(8)]
    thr16 = tl("thr16", [128, 16])
    io16 = tl("io16", [128, 16])
    ioi = tl("ioi", [128, 16], I32)

    fw.op("pool", lambda e: e.memset(Wc[:, :], 0.0), [], [Wc])
    fw.op("pool", lambda e: e.memset(Wc[:, 127:128], 1.0), [Wc], [Wc])
    fw.op("pool", lambda e: e.iota(ioi[:, :], pattern=[[1, 16]], base=0, channel_multiplier=0), [], [ioi])
    fw.op("dve", lambda e: e.tensor_copy(out=io16[:, :], in_=ioi[:, :]), [ioi], [io16])
    fw.op("dve", lambda e: e.tensor_scalar(out=thr16[:, :], in0=io16[:, :], scalar1=16.0, scalar2=None, op0=OP.mult), [io16], [thr16])
    def top16(src3, wrk3, vals3, idx3, n, regs):
        r_src, r_wrk, r_vals, r_idx = regs
        for l in range(n):
            fw.op("dve", lambda e: e.max(out=vals3[:, l, 0:8], in_=src3[:, l, :]), [r_src], [r_vals])
            fw.op("dve", lambda e: e.max_index(out=idx3[:, l, 0:8], in_max=vals3[:, l, 0:8], in_values=src3[:, l, :]), [r_src, r_vals], [r_idx])
            fw.op("dve", lambda e: e.match_replace(out=wrk3[:, l, :], in_to_replace=vals3[:, l, 0:8], in_values=src3[:, l, :], imm_value=NEG), [r_src, r_vals], [r_wrk])
            fw.op("dve", lambda e: e.max(out=vals3[:, l, 8:16], in_=wrk3[:, l, :]), [r_wrk], [r_vals])
            fw.op("dve", lambda e: e.max_index(out=idx3[:, l, 8:16], in_max=vals3[:, l, 8:16], in_values=wrk3[:, l, :]), [r_wrk, r_vals], [r_idx])

    NGC = 16 if debug is None else debug[2]

    def cmain(g):
        p = g % 2
        xg = xgs[p]
        h2g = h2gs[p]
        for bl in range(2):
            j = 2 * g + bl
            fw.dma("sp", lambda e: e.dma_start(out=xg[bl][:, :], in_=x_own[j * 128:(j + 1) * 128, :]), [], [xg[bl]])
            norm1_block(None, xg[bl], xsC, tmpfC, hTC, slice(bl * 128, (bl + 1) * 128), bl, add_eng="dve")
        for u in range(4):
            w, w3 = load_w(u, 8)
            for cc in range(4):
                ch = u * 4 + cc
                pb = ch % 2
                for c in range(8):
                    fw.op("pe", lambda e: e.matmul(bank(pb)[:, 0:256], lhsT=w3[:, c, cc * 128:(cc + 1) * 128], rhs=hTC[:, c, :], start=(c == 0), stop=(c == 7)),
                          [w, hTC], [PR[pb]])
                fw.op("act", lambda e: e.activation(out=gTC[:, ch, :], in_=bank(pb)[:, 0:256], func=AF.Sigmoid), [PR[pb]], [gTC])
        wu, wu3 = load_w(4, 8)
        wsv, wsv3 = load_w(5, 8)
        for bl in range(2):
            bs_ = slice(bl * 128, (bl + 1) * 128)
            for c in range(8):
                fw.op("pe", lambda e: e.matmul(bank(0), lhsT=hTC[:, c, bs_], rhs=wu3[:, c, :], start=(c == 0), stop=(c == 7)), [wu, hTC], [PR[0]])
            for c in range(8):
                fw.op("pe", lambda e: e.matmul(bank(1), lhsT=hTC[:, c, bs_], rhs=wsv3[:, c, :], start=(c == 0), stop=(c == 7)), [wsv, hTC], [PR[1]])
            fw.op("act", lambda e: e.activation(out=gu[:, :], in_=bank(0), func=AF.Gelu_apprx_tanh), [PR[0]], [gu])
            fw.op("act", lambda e: e.activation(out=gs[:, :], in_=bank(1), func=AF.Gelu_apprx_tanh), [PR[1]], [gs])
            fw.op("dve", lambda e: e.bn_stats(out=st6[:, 0:6], in_=gs[:, :]), [gs], [st6])
            fw.op("dve", lambda e: e.bn_aggr(out=st6[:, 6:8], in_=st6[:, 0:6]), [st6], [st6])
            rstd_of(st6, 7, 1.0, 8)
            fw.op("dve", lambda e: e.tensor_scalar(out=gs[:, :], in0=gs[:, :], scalar1=st6[:, 6:7], scalar2=st6[:, 8:9], op0=OP.subtract, op1=OP.mult), [gs, st6], [gs])
            fw.op("dve", lambda e: e.tensor_tensor(out=gs[:, :], in0=gs[:, :], in1=lng_bc[:, :], op=OP.mult), [gs, lng_bc], [gs])
            fw.op("dve", lambda e: e.tensor_tensor(out=svn[:, :], in0=gs[:, :], in1=lnb_bc[:, :], op=OP.add), [gs, lnb_bc], [svn])
            for gr in range(4):
                fw.op("pe", lambda e: e.matmul(bank(0)[:, gr * 128:(gr + 1) * 128], lhsT=wsT[:, gr, :], rhs=svn[:, gr * 128:(gr + 1) * 128], start=True, stop=True),
                      [wsT, svn], [PR[0]])
            for gr in range(4):
                fw.op("dve", lambda e: e.scalar_tensor_tensor(out=ybt[:, gr * 128:(gr + 1) * 128], in0=bank(0)[:, gr * 128:(gr + 1) * 128], scalar=bs_col[:, gr:gr + 1], in1=gu[:, gr * 128:(gr + 1) * 128], op0=OP.add, op1=OP.mult),
                      [PR[0], bs_col, gu], [ybt])
            for ch in range(4):
                fw.op("pe", lambda e: e.transpose(out=bank_bf(1)[:, ch * 128:(ch + 1) * 128], in_=ybt[:, ch * 128:(ch + 1) * 128], identity=identb[:, :]), [ybt, identb], [PR[1]])
            fw.op("act", lambda e: e.activation(out=ybT[:, :, bs_], in_=bank_bf(1)[:, 0:512].rearrange("p (c t) -> p c t", c=4), func=AF.Copy), [PR[1]], [ybT])
        wa, wa3 = load_w(6, 4)
        wb, wb3 = load_w(7, 4)
        yaG = yaGs[p]
        fw.dma("sp", lambda e: e.dma_start(out=yaG[:, :, :], in_=yaTd[:, :, g * 256:(g + 1) * 256].rearrange("c p t -> p c t")), yaregs, [yaG])
        for dc in range(8):
            ba, bb = 0, 1
            for cc in range(4):
                fw.op("pe", lambda e: e.matmul(bank(ba)[:, 0:256], lhsT=wa3[:, cc, dc * 128:(dc + 1) * 128], rhs=yaG[:, cc, :], start=(cc == 0), stop=(cc == 3)),
                      [wa, yaG], [PR[ba]])
            for cc in range(4):
                fw.op("pe", lambda e: e.matmul(bank(bb)[:, 0:256], lhsT=wb3[:, cc, dc * 128:(dc + 1) * 128], rhs=ybT[:, cc, :], start=(cc == 0), stop=(cc == 3)),
                      [wb, ybT], [PR[bb]])
            fw.op("dve", lambda e: e.tensor_tensor(out=t5a[:, :], in0=bank(ba)[:, 0:256], in1=gTC[:, dc, :], op=OP.mult), [PR[ba], gTC], [t5a])
            fw.op("dve", lambda e: e.tensor_tensor(out=t5b[:, :], in0=bank(bb)[:, 0:256], in1=gTC[:, 8 + dc, :], op=OP.mult), [PR[bb], gTC], [t5b])
            fw.op("dve", lambda e: e.tensor_tensor(out=mT[:, dc, :], in0=t5a[:, :], in1=t5b[:, :], op=OP.add), [t5a, t5b], [mT])
        wo0, wo03 = load_w(8, 8)
        wo1, wo13 = load_w(9, 8)
        for bl in range(2):
            bs_ = slice(bl * 128, (bl + 1) * 128)
            for half, (wo, wo3) in enumerate(((wo0, wo03), (wo1, wo13))):
                for c in range(8):
                    fw.op("pe", lambda e: e.matmul(bank(half), lhsT=mT[:, c, bs_], rhs=wo3[:, c, :], start=(c == 0), stop=(c == 7)), [wo, mT], [PR[half]])
            fw.op("dve", lambda e: e.tensor_tensor(out=tmpfC[:, :], in0=pair(0), in1=mod_bc[:, 2 * D:3 * D], op=OP.mult), [PR[0], PR[1], mod_bc], [tmpfC])
            fw.op("dve", lambda e: e.tensor_tensor(out=xg[bl][:, :], in0=xg[bl][:, :], in1=tmpfC[:, :], op=OP.add), [xg[bl], tmpfC], [xg[bl]])
        if debug is not None and debug[0] == "x2":
            dbg_out(xg[0][:, :], [xg[0]], dbg_d[:, 0:D])
            dbg_out(xg[1][:, :], [xg[1]], dbg_d[:, D:2 * D])
            return
        for bl in range(2):
            ssb = ss_r[ss_i[0] % 4]
            ss_i[0] += 1
            fw.op("act", lambda e: e.activation(out=xsC[:, :], in_=xg[bl][:, :], func=AF.Square, accum_out=ssb[:, 0:1]), [xg[bl]], [xsC, ssb])
            rstd_of(ssb, 0, D, 1)
            fw.op("dve", lambda e: e.scalar_tensor_tensor(out=tmpfC[:, :], in0=xg[bl][:, :], scalar=ssb[:, 1:2], in1=mod_bc[:, 4 * D:5 * D], op0=OP.mult, op1=OP.mult), [xg[bl], ssb, mod_bc], [tmpfC])
            fw.op("dve", lambda e: e.tensor_tensor(out=h2g[bl][:, :], in0=tmpfC[:, :], in1=mod_bc[:, 3 * D:4 * D], op=OP.add), [tmpfC, mod_bc], [h2g[bl]])
            for c in range(8):
                fw.op("pe", lambda e: e.transpose(out=bank_bf(bl)[:, c * 128:(c + 1) * 128], in_=h2g[bl][:, c * 128:(c + 1) * 128], identity=identb[:, :]), [h2g[bl], identb], [PR[bl]])
            fw.op("act", lambda e: e.activation(out=hTC[:, :, bl * 128:(bl + 1) * 128], in_=bank_bf(bl)[:, 0:1024].rearrange("p (c t) -> p c t", c=8), func=AF.Copy), [PR[bl]], [hTC])
        for u in range(4):
            w, w3 = load_w(10 + u, 8)
            for cc in range(4):
                l = u * 4 + cc
                pb = l % 2
                for c in range(8):
                    fw.op("pe", lambda e: e.matmul(bank(pb)[:, 0:256], lhsT=w3[:, c, cc * 128:(cc + 1) * 128], rhs=hTC[:, c, :], start=(c == 0), stop=(c == 7)), [w, hTC], [PR[pb]])
                fw.op("act", lambda e: e.activation(out=gTC[:, l, :], in_=bank(pb)[:, 0:256], func=AF.Copy), [PR[pb]], [gTC])
        for bl in range(2):
            eT = eTs[p][bl]
            gT_ = gTs_[p][bl]
            bs_ = slice(bl * 128, (bl + 1) * 128)
            for rnd in range(2):
                for l8 in range(8):
                    l = rnd * 8 + l8
                    fw.op("pe", lambda e: e.matmul(bank(l8 // 4)[:, (l8 % 4) * 128:(l8 % 4 + 1) * 128], lhsT=gTC[:, l, bs_], rhs=skT[:, l, :], start=True, stop=True), [gTC, skT], [PR[l8 // 4]])
                if rnd == 0:
                    fw.op("act", lambda e: e.activation(out=scf[:, 0:1024], in_=pair(0), func=AF.Copy), [PR[0], PR[1]], [scf])
                else:
                    fw.op("dve", lambda e: e.tensor_copy(out=scf[:, 1024:2048], in_=pair(0)), [PR[0], PR[1]], [scf])
            r_src, r_wrk, r_vals, r_idx = scf, scw, tv, ti
            top16(scf[:, :].rearrange("p (l n) -> p l n", l=16), scw[:, :].rearrange("p (l n) -> p l n", l=16),
                  tv[:, :].rearrange("p (l k) -> p l k", l=16), ti[:, :].rearrange("p (l k) -> p l k", l=16), 16, (scf, scw, tv, ti))
            fw.op("dve", lambda e: e.tensor_copy(out=tif[:, :], in_=ti[:, :]), [ti], [tif])
            tv4 = tv[:, :].rearrange("p (h i k) -> p h i k", h=8, i=2)
            tif4 = tif[:, :].rearrange("p (h i k) -> p h i k", h=8, i=2)
            cand4 = scf[:, :].rearrange("p (h a b) -> p h a b", h=8, a=16)
            fw.op("dve", lambda e: e.tensor_tensor(out=cand4, in0=tv4[:, :, 0, :].unsqueeze(3).to_broadcast([128, 8, 16, 16]), in1=tv4[:, :, 1, :].unsqueeze(2).to_broadcast([128, 8, 16, 16]), op=OP.add), [tv], [scf])
            top16(scf[:, :].rearrange("p (h n) -> p h n", h=8), scw[:, :].rearrange("p (h n) -> p h n", h=8),
                  cv[:, :].rearrange("p (h k) -> p h k", h=8), ci[:, :].rearrange("p (h k) -> p h k", h=8), 8, (scf, scw, cv, ci))
            cv3 = cv[:, :].rearrange("p (h k) -> p h k", h=8)
            gsm3 = gsm[:, :].rearrange("p (h k) -> p h k", h=8)
            fw.op("dve", lambda e: e.tensor_tensor(out=gsm3, in0=cv3, in1=cv3[:, :, 0:1].to_broadcast([128, 8, 16]), op=OP.subtract), [cv], [gsm])
            fw.op("act", lambda e: e.activation(out=gsm[:, :], in_=gsm[:, :], func=AF.Exp), [gsm], [gsm])
            fw.op("dve", lambda e: e.tensor_reduce(out=sm8[:, :], in_=gsm3, axis=AX.X, op=OP.add), [gsm], [sm8])
            fw.op("dve", lambda e: e.reciprocal(out=sm8[:, :], in_=sm8[:, :]), [sm8], [sm8])
            fw.op("dve", lambda e: e.tensor_tensor(out=gsm3, in0=gsm3, in1=sm8[:, :].unsqueeze(2).to_broadcast([128, 8, 16]), op=OP.mult), [gsm, sm8], [gsm])
            fw.op("dve", lambda e: e.tensor_copy(out=cif[:, :], in_=ci[:, :]), [ci], [cif])
            oh4 = scw[:, :].rearrange("p (h k a) -> p h k a", h=8, k=16)
            cif3 = cif[:, :].rearrange("p (h k) -> p h k", h=8)
            ak3 = ak[:, :].rearrange("p (h k) -> p h k", h=8)
            bk3 = bk[:, :].rearrange("p (h k) -> p h k", h=8)
            thr_b = thr16[:, :].unsqueeze(1).unsqueeze(1).to_broadcast([128, 8, 16, 16])
            io_b = io16[:, :].unsqueeze(1).unsqueeze(1).to_broadcast([128, 8, 16, 16])
            fw.op("dve", lambda e: e.tensor_tensor(out=oh4, in0=cif3.unsqueeze(3).to_broadcast([128, 8, 16, 16]), in1=thr_b, op=OP.is_ge), [cif, thr16], [scw])
            fw.op("dve", lambda e: e.tensor_reduce(out=ak3, in_=oh4, axis=AX.X, op=OP.add), [scw], [ak])
            fw.op("dve", lambda e: e.tensor_scalar(out=ak[:, :], in0=ak[:, :], scalar1=-1.0, scalar2=None, op0=OP.add), [ak], [ak])
            fw.op("dve", lambda e: e.scalar_tensor_tensor(out=bk[:, :], in0=ak[:, :], scalar=-16.0, in1=cif[:, :], op0=OP.mult, op1=OP.add), [ak, cif], [bk])
            for (kk3, pidx, dst) in ((ak3, 0, i1s), (bk3, 1, i2s)):
                fw.op("dve", lambda e: e.tensor_tensor(out=oh4, in0=kk3.unsqueeze(3).to_broadcast([128, 8, 16, 16]), in1=io_b, op=OP.is_equal), [ak, bk, io16], [scw])
                fw.op("dve", lambda e: e.tensor_tensor(out=oh4, in0=oh4, in1=tif4[:, :, pidx, :].unsqueeze(2).to_broadcast([128, 8, 16, 16]), op=OP.mult), [scw, tif], [scw])
                fw.op("dve", lambda e: e.tensor_reduce(out=dst[:, :].rearrange("p (h k) -> p h k", h=8), in_=oh4, axis=AX.X, op=OP.add), [scw], [dst])
            fw.op("dve", lambda e: e.scalar_tensor_tensor(out=eidxf[:, :], in0=i1s[:, :], scalar=128.0, in1=i2s[:, :], op0=OP.mult, op1=OP.add), [i1s, i2s], [eidxf])
            fw.op("pe", lambda e: e.transpose(out=bank(0)[:, 0:128], in_=eidxf[:, :], identity=identf[:, :]), [eidxf, identf], [PR[0]])
            fw.op("dve", lambda e: e.tensor_copy(out=eT[:, :], in_=bank(0)[:, 0:128]), [PR[0]], [eT])
            fw.op("pe", lambda e: e.transpose(out=bank(1)[:, 0:128], in_=gsm[:, :], identity=identf[:, :]), [gsm, identf], [PR[1]])
            fw.op("act", lambda e: e.activation(out=gT_[:, :], in_=bank(1)[:, 0:128], func=AF.Copy), [PR[1]], [gT_])

    def loops(g):
        p = g % 2
        xg = xgs[p]
        h2g = h2gs[p]
        for bl in range(2):
            j = 2 * g + bl
            eT = eTs[p][bl]
            gT_ = gTs_[p][bl]
            gbs = {}

            def s_gather(t):
                gbs[t] = gather(pdu, eT[:, t:t + 1], [eT] + tregs)

            def s_bcast(t):
                hbp = 2 + (t % 2)
                for half in range(2):
                    fw.op("pe", lambda e: e.matmul(bank(2 * hbp + half), lhsT=identb[:, t:t + 1].to_broadcast([128, 128]), rhs=h2g[bl][:, half * 512:(half + 1) * 512], start=True, stop=True),
                          [identb, h2g[bl]], [PR[2 * hbp + half]])

            def s_dot(t):
                hbp = 2 + (t % 2)
                ac = acols[t % 8]
                cc_ = ccols[t % 8]
                gb_ = gbs[t]
                fw.op("dve", lambda e: e.scalar_tensor_tensor(out=junkL[:, :], in0=gb_[:, 0:D], scalar=1.0, in1=pair(hbp), op0=OP.mult, op1=OP.mult, accum_out=ac[:, 0:1]),
                      [gb_, PR[2 * hbp], PR[2 * hbp + 1]], [junkL, ac])
                fw.op("act", lambda e: e.activation(out=cc_[:, 0:1], in_=ac[:, 0:1], func=AF.Gelu_apprx_tanh), [ac], [cc_])

            def s_up(t):
                cc_ = ccols[t % 8]
                zt = Zts[t % 4]
                gb_ = gbs.pop(t)
                fw.op("dve", lambda e: e.tensor_scalar(out=zt[:, :], in0=Wc[:, 127 - t:255 - t], scalar1=cc_[:, 0:1], scalar2=gT_[:, t:t + 1], op0=OP.mult, op1=OP.mult),
                      [Wc, cc_, gT_], [zt])
                for half in range(2):
                    fw.op("pe", lambda e: e.matmul(bank(2 + half), lhsT=zt[:, :], rhs=gb_[:, D + half * 512:D + (half + 1) * 512], start=(t == 0), stop=(t == 127)),
                          [zt, gb_], [PR[2 + half]])

            AH = NGB - 2
            for t in range(min(AH, 128)):
                s_gather(t)
            s_bcast(0)
            for t in range(128):
                if t + AH < 128:
                    s_gather(t + AH)
                if t + 1 < 128:
                    s_bcast(t + 1)
                s_dot(t)
                if t >= 1:
                    s_up(t - 1)
            s_up(127)
            fw.op("dve", lambda e: e.tensor_tensor(out=tmpfL[:, :], in0=pair(1), in1=mod_bc[:, 5 * D:6 * D], op=OP.mult), [PR[2], PR[3], mod_bc], [tmpfL])
            fw.op("dve", lambda e: e.tensor_tensor(out=xg[bl][:, :], in0=xg[bl][:, :], in1=tmpfL[:, :], op=OP.add), [xg[bl], tmpfL], [xg[bl]])
            ssb = ss_r[ss_i[0] % 4]
            ss_i[0] += 1
            fw.op("act", lambda e: e.activation(out=junkL[:, :], in_=xg[bl][:, :], func=AF.Square, accum_out=ssb[:, 0:1]), [xg[bl]], [junkL, ssb])
            rstd_of(ssb, 0, D, 1)
            fw.op("dve", lambda e: e.scalar_tensor_tensor(out=tmpfL[:, :], in0=xg[bl][:, :], scalar=ssb[:, 1:2], in1=fg_bc[:, :], op0=OP.mult, op1=OP.mult), [xg[bl], ssb, fg_bc], [tmpfL])
            fw.dma("sp", lambda e: e.dma_start(out=out_d[j * 128:(j + 1) * 128, :], in_=tmpfL[:, :]), [tmpfL], [r_none])

    cmain(0)
    if debug is not None and debug[0] == "x2":
        fw.barrier()
        return nc
    for g in range(NGC):
        if g + 1 < NGC:
            interleave(fw, [lambda: loops(g), lambda: cmain(g + 1)], [2, 1])
        else:
            loops(g)
    fw.barrier()
    return nc


def _prep_inputs(inputs):
    g = {k: np.asarray(v) for k, v in inputs.items()}
    x = np.ascontiguousarray(g["x"], dtype=np.float32)
    shared = {
        "w_ada": g["w_ada"][0], "b_ada": g["b_ada"], "norm1_g": g["norm1_g"], "w_in": g["w_in"][0],
        "da_lambda_q1": g["da_lambda_q1"], "da_lambda_k1": g["da_lambda_k1"],
        "da_lambda_q2": g["da_lambda_q2"], "da_lambda_k2": g["da_lambda_k2"],
        "da_head_g": g["da_head_g"], "sg_ln_g": g["sg_ln_g"], "sg_ln_b": g["sg_ln_b"],
        "sg_w": g["sg_w"][0], "sg_b": g["sg_b"][0], "w_branch_a": g["w_branch_a"][0],
        "w_branch_b": g["w_branch_b"][0], "w_out": g["w_out"][0], "norm2_g": g["norm2_g"],
        "peer_w_query": g["peer_w_query"][0], "peer_sub_keys": g["peer_sub_keys"][0].reshape(16, 128, 128),
        "peer_down": g["peer_down"][0], "peer_up": g["peer_up"][0], "final_g": g["final_g"].reshape(1, D),
    }
    shared = {k: np.ascontiguousarray(v, dtype=np.float32) for k, v in shared.items()}
    kk = np.arange(128)[:, None] // 64
    qq = np.arange(128)[None, :] // 64
    diag = (kk <= qq).astype(np.float32)
    in_maps = []
    for core in range(8):
        b, par = core // 2, core % 2
        xb = x[b]
        xo = np.ascontiguousarray(xb.reshape(NB, 128, D)[par::2].reshape(NOWN * 128, D))
        if par == 0:
            am = np.concatenate([diag, np.zeros((128, 128), np.float32)], axis=1)
        else:
            am = np.concatenate([np.ones((128, 128), np.float32), diag], axis=1)
        m = dict(shared)
        m["x_b"] = xb
        m["x_own"] = xo
        m["c_b"] = np.ascontiguousarray(g["c"][b:b + 1], dtype=np.float32)
        m["amask"] = np.ascontiguousarray(am)
        in_maps.append(m)
    return in_maps


def kernel(**inputs):
    in_maps = _prep_inputs(inputs)
    nc = build()
    res = run_bass_kernel_spmd(nc, in_maps, core_ids=list(range(8)))
    out = np.zeros((4, S, D), np.float32)
    for core in range(8):
        b, par = core // 2, core % 2
        o = np.asarray(res.results[core]["out"]).reshape(NOWN, 128, D)
        out[b].reshape(NB, 128, D)[par::2] = o
    return out
```

```python
import numpy as np
from contextlib import ExitStack
import concourse.bass as bass
import concourse.mybir as mybir
from concourse.bass_utils import run_bass_kernel_spmd

F32 = mybir.dt.float32
BF16 = mybir.dt.bfloat16
I32 = mybir.dt.int32
U32 = mybir.dt.uint32
AF = mybir.ActivationFunctionType
OP = mybir.AluOpType
AX = mybir.AxisListType

D = 1024
S = 8192
NB = 64
NOWN = 32
EPS = 1e-6
LAM_INIT = 0.8 - 0.6 * 1.0
NEG = -1.0e30
SEM_LIMIT = 30000


class Reg:
    def __init__(self):
        self.w = {}
        self.r = {}


class Buf(Reg):
    def __init__(self, t):
        Reg.__init__(self)
        self.t = t

    def __getitem__(self, k):
        return self.t[k]


class FW:
    def __init__(self, nc):
        self.nc = nc
        self.engs = dict(pe=nc.tensor, act=nc.scalar, dve=nc.vector, pool=nc.gpsimd, sp=nc.sync)
        self.cur = {}
        self.nsem = 0
        self.known = {k: {} for k in self.engs}
        for k in self.engs:
            self._newsem(k)
        self.dsem = {}
        for q, n in (("sp", 12), ("act", 4), ("pool", 12)):
            self.dsem[q] = [[f"d_{q}{i}", nc.alloc_semaphore(f"d_{q}{i}"), 0] for i in range(n)]
        self.drr = {q: 0 for q in self.dsem}
        self.ninst = 0
        self.hook = None

    def _newsem(self, k):
        name = f"e_{k}{self.nsem}"
        self.cur[k] = [name, self.nc.alloc_semaphore(name), 0]
        self.nsem += 1

    def _deps(self, reads, writes):
        deps = {}

        def add(d):
            for name, (s, v) in d.items():
                if name not in deps or deps[name][1] < v:
                    deps[name] = (s, v)
        for b in reads:
            add(b.w)
        for b in writes:
            add(b.w)
            add(b.r)
        return deps

    def _wait(self, k, deps):
        eng = self.engs[k]
        for name, (s, v) in deps.items():
            if k == "pe" and name.startswith("e_pe"):
                continue
            if self.known[k].get(name, 0) >= v:
                continue
            eng.wait_ge(s, v)
            self.known[k][name] = v
            self.ninst += 1

    def _record(self, ev, reads, writes):
        name, s, v = ev
        for b in reads:
            b.r[name] = (s, v)
        for b in writes:
            b.w = {name: (s, v)}
            b.r = {}

    def op(self, k, fn, reads, writes):
        self._wait(k, self._deps(reads, writes))
        ins = fn(self.engs[k])
        c = self.cur[k]
        if c[2] >= SEM_LIMIT:
            self._newsem(k)
            c = self.cur[k]
        c[2] += 1
        ins.then_inc(c[1], 1)
        self.ninst += 1
        self._record((c[0], c[1], c[2]), reads, writes)
        if self.hook is not None:
            self.hook()

    def dma(self, q, fn, reads, writes, slot=None, pre=()):
        if slot is None:
            slots = self.dsem[q]
            i = self.drr[q]
            self.drr[q] = (i + 1) % len(slots)
            sl = slots[i]
        else:
            sl = slot
        deps = self._deps(reads, list(writes) + list(pre))
        if slot is None and sl[2] > 0:
            deps[sl[0]] = (sl[1], sl[2])
        self._wait(q, deps)
        ins = fn(self.engs[q])
        sl[2] += 16
        ins.then_inc(sl[1], 16)
        self.ninst += 1
        self._record((sl[0], sl[1], sl[2]), reads, writes)
        self.last_written = writes[0] if writes else None
        if self.hook is not None:
            self.hook()

    def barrier(self):
        evs = {}
        for k, c in self.cur.items():
            if c[2] > 0:
                evs[c[0]] = (c[1], c[2])
        for q, slots in self.dsem.items():
            for sl in slots:
                if sl[2] > 0:
                    evs[sl[0]] = (sl[1], sl[2])
        for k in self.engs:
            self._wait(k, dict(evs))


def interleave(fw, fns, weights):
    import threading
    n = len(fns)
    sems = [threading.Semaphore(0) for _ in fns]
    done = [False] * n
    st = {"cur": 0, "cnt": 0}
    errs = []
    fin = threading.Semaphore(0)

    def nxt(i):
        for k in range(1, n + 1):
            j = (i + k) % n
            if not done[j]:
                return j
        return None

    def hook():
        i = st["cur"]
        st["cnt"] += 1
        if st["cnt"] >= weights[i]:
            st["cnt"] = 0
            j = nxt(i)
            if j is not None and j != i:
                st["cur"] = j
                sems[j].release()
                sems[i].acquire()

    def runner(i):
        sems[i].acquire()
        try:
            fns[i]()
        except BaseException as e:
            errs.append(e)
        done[i] = True
        j = nxt(i)
        if j is not None:
            st["cur"] = j
            st["cnt"] = 0
            sems[j].release()
        else:
            fin.release()

    ths = [threading.Thread(target=runner, args=(i,)) for i in range(n)]
    old = fw.hook
    fw.hook = hook
    for t in ths:
        t.start()
    st["cur"] = 0
    sems[0].release()
    fin.acquire()
    for t in ths:
        t.join()
    fw.hook = old
    if errs:
        raise errs[0]


class ViewBuf(Reg):
    def __init__(self, ap):
        Reg.__init__(self)
        self.ap_ = ap

    def __getitem__(self, k):
        return self.ap_[k]


def build(debug=None):
    nc = bass.Bass("TRN2", target_bir_lowering=False)
    fw = FW(nc)

    def din(name, shape, dt=F32):
        return nc.dram_tensor(name, list(shape), dt, kind="ExternalInput").ap()

    x_b = din("x_b", [S, D])
    x_own = din("x_own", [NOWN * 128, D])
    c_b = din("c_b", [1, D])
    w_ada = din("w_ada", [D, 6 * D])
    b_ada = din("b_ada", [1, 6 * D])
    norm1_g = din("norm1_g", [1, D])
    w_in = din("w_in", [D, 4608])
    lq1 = din("da_lambda_q1", [1, 64])
    lk1 = din("da_lambda_k1", [1, 64])
    lq2 = din("da_lambda_q2", [1, 64])
    lk2 = din("da_lambda_k2", [1, 64])
    head_g = din("da_head_g", [1, 128])
    ln_g = din("sg_ln_g", [1, 512])
    ln_b = din("sg_ln_b", [1, 512])
    sg_w = din("sg_w", [4, 128, 128])
    sg_b = din("sg_b", [4, 128])
    w_ba = din("w_branch_a", [512, D])
    w_bb = din("w_branch_b", [512, D])
    w_out = din("w_out", [D, D])
    norm2_g = din("norm2_g", [1, D])
    w_pq = din("peer_w_query", [D, 2048])
    sub_keys = din("peer_sub_keys", [16, 128, 128])
    p_down = din("peer_down", [16384, D])
    p_up = din("peer_up", [16384, D])
    final_g = din("final_g", [1, D])
    amask = din("amask", [128, 256])
    out_d = nc.dram_tensor("out", [NOWN * 128, D], F32, kind="ExternalOutput").ap()
    dbg_d = None
    if debug is not None:
        dbg_d = nc.dram_tensor("dbg", list(debug[1]), F32, kind="ExternalOutput").ap()

    winb = nc.dram_tensor("winb", [D, 4608], BF16, kind="Internal").ap()
    wbab = nc.dram_tensor("wbab", [512, D], BF16, kind="Internal").ap()
    wbbb = nc.dram_tensor("wbbb", [512, D], BF16, kind="Internal").ap()
    woutb = nc.dram_tensor("woutb", [D, D], BF16, kind="Internal").ap()
    wpqb = nc.dram_tensor("wpqb", [D, 2048], BF16, kind="Internal").ap()
    modscr = nc.dram_tensor("modscr", [1, 2048], F32, kind="Internal").ap()
    pdu = nc.dram_tensor("pdu", [16384, 2 * D], BF16, kind="Internal").ap()
    tregs = []
    tconv = [(pdu[:, hf * D:(hf + 1) * D], src, r0) for hf, src in enumerate((p_down, p_up)) for r0 in range(0, 16384, 1024)]
    wregs = []
    r_modscr = Reg()
    r_none = Reg()
    yaregs = []

    def sb(name, shape, dt=F32):
        return Buf(nc.alloc_sbuf_tensor(name, list(shape), dt))

    P2 = [nc.alloc_psum_tensor(f"ps{i}", [128, 1024], F32) for i in range(4)]
    PR = [Reg() for _ in range(8)]

    def bank(i):
        return P2[i // 2][:, (i % 2) * 512:(i % 2 + 1) * 512]

    def bank_bf(i):
        return P2[i // 2][:, :].bitcast(BF16)[:, (i % 2) * 1024:(i % 2 + 1) * 1024]

    def pair(i):
        return P2[i][:, :]

    identf = sb("identf", [128, 128])
    identb = sb("identb", [128, 128], BF16)
    mod_bc = sb("mod_bc", [128, 6 * D])
    fg_bc = sb("fg_bc", [128, D])
    yaTd = nc.dram_tensor("yaTd", [4, 128, NOWN * 128], BF16, kind="Internal").ap()
    r_yaT = [Reg() for _ in range(8)]
    ysts = [sb("yst0", [128, 128], BF16), sb("yst1", [128, 128], BF16)]
    ysi = [0]
    A1col = sb("A1col", [128, 8])
    B1col = sb("B1col", [128, 8])
    neglam = sb("neglam", [128, 1])
    hg_bc = sb("hg_bc", [128, 128])
    lng_bc = sb("lng_bc", [128, 512])
    lnb_bc = sb("lnb_bc", [128, 512])
    bs_col = sb("bs_col", [128, 4])
    maskb = sb("maskb", [128, 256], BF16)
    wsT = sb("wsT", [128, 4, 128], BF16)
    skT = sb("skT", [128, 16, 128], BF16)
    ss_r = [sb(f"ss{i}", [128, 4]) for i in range(4)]
    ss_i = [0]

    def dbg_out(buf_ap, reads, dst=None):
        fw.dma("sp", lambda e: e.dma_start(out=(dbg_d if dst is None else dst), in_=buf_ap), reads, [r_none])

    fw.op("pool", lambda e: e.memset(identf[:, :], 1.0), [], [identf])
    fw.op("pool", lambda e: e.affine_select(out=identf[:, :], in_=identf[:, :], pattern=[[-1, 128]],
                                            compare_op=OP.is_equal, fill=0.0, base=0, channel_multiplier=1),
          [identf], [identf])
    fw.op("dve", lambda e: e.tensor_copy(out=identb[:, :], in_=identf[:, :]), [identf], [identb])

    for (dst, src, ncol) in ((winb, w_in, 4608), (wbab, w_ba, D), (wbbb, w_bb, D), (woutb, w_out, D), (wpqb, w_pq, 2048)):
        for c0 in range(0, ncol, 512):
            fw.dma("pool", lambda e, dst=dst, src=src, c0=c0: e.dma_start(out=dst[:, c0:c0 + 512], in_=src[:, c0:c0 + 512]),
                   [], [Reg()])
            wregs.append(fw.last_written)

    with nc.allow_non_contiguous_dma(reason="tiny param loads"):
        n1g_col = sb("n1g_col", [128, 8])
        c_col = sb("c_col", [128, 8])
        fw.dma("sp", lambda e: e.dma_start(out=n1g_col[:, :], in_=norm1_g[0, :].rearrange("(c p) -> p c", p=128)), [], [n1g_col])
        fw.dma("sp", lambda e: e.dma_start(out=c_col[:, :], in_=c_b[0, :].rearrange("(c p) -> p c", p=128)), [], [c_col])
        fw.dma("sp", lambda e: e.dma_start(out=bs_col[:, :], in_=sg_b.rearrange("g p -> p g")), [], [bs_col])
    fw.dma("sp", lambda e: e.dma_start(out=mod_bc[:, :], in_=b_ada[0:1, :].partition_broadcast(128)), [], [mod_bc])
    fw.dma("sp", lambda e: e.dma_start(out=fg_bc[:, :], in_=final_g[0:1, :].partition_broadcast(128)), [], [fg_bc])
    fw.dma("sp", lambda e: e.dma_start(out=hg_bc[:, :], in_=head_g[0:1, :].partition_broadcast(128)), [], [hg_bc])
    fw.dma("sp", lambda e: e.dma_start(out=lng_bc[:, :], in_=ln_g[0:1, :].partition_broadcast(128)), [], [lng_bc])
    fw.dma("sp", lambda e: e.dma_start(out=lnb_bc[:, :], in_=ln_b[0:1, :].partition_broadcast(128)), [], [lnb_bc])
    fw.op("dve", lambda e: e.tensor_scalar(out=hg_bc[:, :], in0=hg_bc[:, :], scalar1=1.0 - LAM_INIT, scalar2=None, op0=OP.mult),
          [hg_bc], [hg_bc])

    with ExitStack() as es:
        def tl(name, shape, dt=F32):
            return es.enter_context(nc.sbuf_tensor(name, list(shape), dt))
        wa0_t = tl("s0a", [128, 8, 512]); wa1_t = tl("s0b", [128, 8, 512]); csbc_t = tl("s0c", [128, 8, 128])
        lam_t = tl("s0d", [128, 4, 64]); n2g_t = tl("s0e", [128, 1024]); mcol_t = tl("s0f", [128, 16])
        tmp0_t = tl("s0g", [128, 512]); lsc_t = tl("s0h", [128, 8])
        wab = [Buf(wa0_t), Buf(wa1_t)]
        csbc = Buf(csbc_t)
        lam4 = Buf(lam_t)
        n2g = Buf(n2g_t)
        mcol = Buf(mcol_t)
        tmp0 = Buf(tmp0_t)
        lsc = Buf(lsc_t)
        for i, src in enumerate((lq1, lk1, lq2, lk2)):
            fw.dma("sp", lambda e, i=i, src=src: e.dma_start(out=lam4[:, i, :], in_=src[0:1, :].partition_broadcast(128)), [], [lam4])
        fw.op("dve", lambda e: e.tensor_tensor(out=lam4[:, 0, :], in0=lam4[:, 0, :], in1=lam4[:, 1, :], op=OP.mult), [lam4], [lam4])
        fw.op("dve", lambda e: e.tensor_tensor(out=lam4[:, 2, :], in0=lam4[:, 2, :], in1=lam4[:, 3, :], op=OP.mult), [lam4], [lam4])
        fw.op("dve", lambda e: e.tensor_reduce(out=lsc[:, 0:1], in_=lam4[:, 0, :], axis=AX.X, op=OP.add), [lam4], [lsc])
        fw.op("dve", lambda e: e.tensor_reduce(out=lsc[:, 1:2], in_=lam4[:, 2, :], axis=AX.X, op=OP.add), [lam4], [lsc])
        fw.op("act", lambda e: e.activation(out=lsc[:, 2:4], in_=lsc[:, 0:2], func=AF.Exp), [lsc], [lsc])
        fw.op("dve", lambda e: e.tensor_tensor(out=lsc[:, 4:5], in0=lsc[:, 3:4], in1=lsc[:, 2:3], op=OP.subtract), [lsc], [lsc])
        fw.op("dve", lambda e: e.tensor_scalar(out=neglam[:, :], in0=lsc[:, 4:5], scalar1=-LAM_INIT, scalar2=None, op0=OP.add),
              [lsc], [neglam])
        fw.dma("sp", lambda e: e.dma_start(out=tmp0[:, 0:256], in_=amask[:, :]), [], [tmp0])
        fw.op("dve", lambda e: e.tensor_copy(out=maskb[:, :], in_=tmp0[:, 0:256]), [tmp0], [maskb])
        for g in range(4):
            fw.dma("sp", lambda e, g=g: e.dma_start(out=tmp0[:, 0:128], in_=sg_w[g, :, :]), [], [tmp0])
            fw.op("dve", lambda e: e.memset(tmp0[0:64, 64:128], 0.0), [tmp0], [tmp0])
            fw.op("pe", lambda e: e.transpose(out=bank(0)[:, 0:128], in_=tmp0[:, 0:128], identity=identf[:, :]), [tmp0, identf], [PR[0]])
            fw.op("act", lambda e, g=g: e.activation(out=wsT[:, g, :], in_=bank(0)[:, 0:128], func=AF.Copy), [PR[0]], [wsT])
        for l in range(16):
            fw.dma("sp", lambda e, l=l: e.dma_start(out=tmp0[:, 0:128], in_=sub_keys[l, :, :]), [], [tmp0])
            fw.op("pe", lambda e: e.transpose(out=bank(1)[:, 0:128], in_=tmp0[:, 0:128], identity=identf[:, :]), [tmp0, identf], [PR[1]])
            fw.op("act", lambda e, l=l: e.activation(out=skT[:, l, :], in_=bank(1)[:, 0:128], func=AF.Copy), [PR[1]], [skT])
        fw.op("act", lambda e: e.activation(out=c_col[:, :], in_=c_col[:, :], func=AF.Silu), [c_col], [c_col])
        fw.op("dve", lambda e: e.tensor_copy(out=csbc[:, :, :], in_=c_col[:, :].unsqueeze(2).to_broadcast([128, 8, 128])), [c_col], [csbc])
        w_ada_v = w_ada.rearrange("(c p) n -> p c n", p=128)
        for ec in range(12):
            wb_ = wab[ec % 2]
            fw.dma("sp" if ec % 2 == 0 else "act", lambda e, ec=ec, wb_=wb_: e.dma_start(out=wb_[:, :, :], in_=w_ada_v[:, :, ec * 512:(ec + 1) * 512]), [], [wb_])
            pb = 2 + (ec % 2)
            for c in range(8):
                fw.op("pe", lambda e, c=c, wb_=wb_, pb=pb: e.matmul(bank(pb), lhsT=csbc[:, c, :], rhs=wb_[:, c, :], start=(c == 0), stop=(c == 7)),
                      [csbc, wb_], [PR[pb]])
            fw.op("dve", lambda e, ec=ec, pb=pb: e.tensor_tensor(out=mod_bc[:, ec * 512:(ec + 1) * 512], in0=bank(pb), in1=mod_bc[:, ec * 512:(ec + 1) * 512], op=OP.add),
                  [PR[pb], mod_bc], [mod_bc])
        fw.dma("sp", lambda e: e.dma_start(out=modscr[0:1, :], in_=mod_bc[0:1, 0:2048]), [mod_bc], [r_modscr])
        with nc.allow_non_contiguous_dma(reason="tiny param loads"):
            fw.dma("sp", lambda e: e.dma_start(out=mcol[:, :], in_=modscr[0, :].rearrange("(c p) -> p c", p=128)), [r_modscr], [mcol])
        fw.op("dve", lambda e: e.tensor_copy(out=B1col[:, :], in_=mcol[:, 0:8]), [mcol], [B1col])
        fw.op("dve", lambda e: e.scalar_tensor_tensor(out=A1col[:, :], in0=mcol[:, 8:16], scalar=1.0, in1=n1g_col[:, :], op0=OP.add, op1=OP.mult),
              [mcol, n1g_col], [A1col])
        fw.dma("sp", lambda e: e.dma_start(out=n2g[:, :], in_=norm2_g[0:1, :].partition_broadcast(128)), [], [n2g])
        fw.op("dve", lambda e: e.scalar_tensor_tensor(out=mod_bc[:, 4 * D:5 * D], in0=mod_bc[:, 4 * D:5 * D], scalar=1.0, in1=n2g[:, :], op0=OP.add, op1=OP.mult),
              [mod_bc, n2g], [mod_bc])
        fw.barrier()
    if debug is not None and debug[0] == "mod":
        dbg_out(mod_bc[:, :], [mod_bc])
        fw.barrier()
        return nc

    A1bc = A1col[:, :].unsqueeze(2).to_broadcast([128, 8, 128])
    B1bc = B1col[:, :].unsqueeze(2).to_broadcast([128, 8, 128])

    def rstd_of(ssbuf, col, n, tmpcol):
        fw.op("dve", lambda e: e.tensor_scalar(out=ssbuf[:, tmpcol:tmpcol + 1], in0=ssbuf[:, col:col + 1], scalar1=1.0 / n, scalar2=EPS, op0=OP.mult, op1=OP.add),
              [ssbuf], [ssbuf])
        fw.op("act", lambda e: e.activation(out=ssbuf[:, tmpcol:tmpcol + 1], in_=ssbuf[:, tmpcol:tmpcol + 1], func=AF.Sqrt), [ssbuf], [ssbuf])
        fw.op("dve", lambda e: e.reciprocal(out=ssbuf[:, tmpcol:tmpcol + 1], in_=ssbuf[:, tmpcol:tmpcol + 1]), [ssbuf], [ssbuf])

    def norm1_block(src_rows, xin, xs, tmpf, hT, hcols, pb, add_eng="pool"):
        ssb = ss_r[ss_i[0] % 4]
        ss_i[0] += 1
        if src_rows is not None:
            fw.dma("sp", lambda e: e.dma_start(out=xin[:, :], in_=src_rows), [], [xin])
        fw.op("act", lambda e: e.activation(out=xs[:, :], in_=xin[:, :], func=AF.Square, accum_out=ssb[:, 0:1]), [xin], [xs, ssb])
        rstd_of(ssb, 0, D, 1)
        if add_eng == "act":
            fw.op("act", lambda e: e.activation(out=xs[:, :], in_=xin[:, :], func=AF.Copy, scale=ssb[:, 1:2]), [xin, ssb], [xs])
        else:
            fw.op("dve", lambda e: e.tensor_scalar(out=xs[:, :], in0=xin[:, :], scalar1=ssb[:, 1:2], scalar2=None, op0=OP.mult), [xin, ssb], [xs])
        tpv = bank_bf(pb)
        for c in range(8):
            fw.op("pe", lambda e, c=c: e.transpose(out=tpv[:, c * 128:(c + 1) * 128], in_=xs[:, c * 128:(c + 1) * 128], identity=identb[:, :]),
                  [xs, identb], [PR[pb]])
        if add_eng == "act":
            for c in range(8):
                fw.op("act", lambda e, c=c: e.activation(out=hT[:, c, hcols], in_=tpv[:, c * 128:(c + 1) * 128], func=AF.Identity, scale=A1col[:, c:c + 1], bias=B1col[:, c:c + 1]),
                      [PR[pb], A1col, B1col], [hT])
            return
        fw.op("dve", lambda e: e.tensor_tensor(out=tmpf[:, :].rearrange("p (c t) -> p c t", c=8), in0=tpv.rearrange("p (c t) -> p c t", c=8), in1=A1bc, op=OP.mult),
              [PR[pb], A1col], [tmpf])
        fw.op(add_eng, lambda e: e.tensor_tensor(out=hT[:, :, hcols], in0=tmpf[:, :].rearrange("p (c t) -> p c t", c=8), in1=B1bc, op=OP.add),
              [tmpf, B1col], [hT])

    winb_v = winb.rearrange("(c p) n -> p c n", p=128)

    for hp in range(2):
        with ExitStack() as es:
            def tl(name, shape, dt=F32):
                return es.enter_context(nc.sbuf_tensor(f"{name}_p{hp}", list(shape), dt))
            KT_t = tl("KT", [128, 2, S], BF16); V_t = tl("VV", [128, NB, 2, 129], BF16)
            wk_t = tl("wk", [128, 8, 256], BF16); wv_t = tl("wv", [128, 8, 256], BF16); wq_t = tl("wq", [128, 8, 256], BF16)
            xin0_t = tl("xin0", [128, D]); xin1_t = tl("xin1", [128, D]); xin2_t = tl("xin2", [128, D])
            xs0_t = tl("xs0", [128, D], BF16); xs1_t = tl("xs1", [128, D], BF16)
            tmpf0_t = tl("tmpf0", [128, D]); tmpf1_t = tl("tmpf1", [128, D])
            hT0_t = tl("hT0", [128, 8, 512], BF16); hT1_t = tl("hT1", [128, 8, 512], BF16)
            QT0_t = tl("QT0", [128, 2, 512], BF16); QT1_t = tl("QT1", [128, 2, 512], BF16)
            PT0_t = tl("PT0", [128, 512], BF16); PT1_t = tl("PT1", [128, 512], BF16); PT2_t = tl("PT2", [128, 512], BF16); PT3_t = tl("PT3", [128, 512], BF16)
            at0_t = tl("at0", [128, 128]); at1_t = tl("at1", [128, 128])
            ab0_t = tl("ab0", [128, 128], BF16); ab1_t = tl("ab1", [128, 128], BF16)
            asm_t = tl("asm", [128, 8])
            KT = KT_t
            V = V_t
            rKT = [Reg() for _ in range(16)]
            rV = [Reg() for _ in range(16)]
            wk, wv, wq = Buf(wk_t), Buf(wv_t), Buf(wq_t)
            xins = [Buf(xin0_t), Buf(xin1_t), Buf(xin2_t)]
            xss = [Buf(xs0_t), Buf(xs1_t)]
            tmpfs = [Buf(tmpf0_t), Buf(tmpf1_t)]
            hTs = [Buf(hT0_t), Buf(hT1_t)]
            QTs = [Buf(QT0_t), Buf(QT1_t)]
            PTs = [Buf(PT0_t), Buf(PT1_t), Buf(PT2_t), Buf(PT3_t)]
            ats = [Buf(at0_t), Buf(at1_t)]
            abs_ = [Buf(ab0_t), Buf(ab1_t)]
            asm = Buf(asm_t)
            qc0, kc0, vc0 = hp * 256, 512 + hp * 256, 1024 + hp * 256
            fw.dma("sp", lambda e: e.dma_start(out=wq[:, :, :], in_=winb_v[:, :, qc0:qc0 + 256]), wregs, [wq])
            fw.dma("sp", lambda e: e.dma_start(out=wk[:, :, :], in_=winb_v[:, :, kc0:kc0 + 256]), wregs, [wk])
            fw.dma("sp", lambda e: e.dma_start(out=wv[:, :, :], in_=winb_v[:, :, vc0:vc0 + 256]), wregs, [wv])
            rVall = Reg()
            fw.op("pool", lambda e: e.memset(V[:, :, :, 128:129], 1.0), [], [rVall])
            for r_ in rV:
                r_.w = dict(rVall.w)
            nbc = [0]

            def normS(sbi):
                if True:
                    hT = hTs[sbi % 2]
                    if hp == 0:
                        for dst_, src_, r0_ in tconv[2 * sbi:2 * sbi + 2]:
                            fw.dma("pool", lambda e: e.dma_start(out=dst_[r0_:r0_ + 1024, :], in_=src_[r0_:r0_ + 1024, :]), [], [Reg()])
                            tregs.append(fw.last_written)
                    for bl in range(4):
                        gb = sbi * 4 + bl
                        nb_ = nbc[0]
                        nbc[0] += 1
                        norm1_block(x_b[gb * 128:(gb + 1) * 128, :], xins[nb_ % 3], xss[nb_ % 2], tmpfs[nb_ % 2], hT,
                                    slice(bl * 128, (bl + 1) * 128), nb_ % 2)

            def kvS(sbi):
                if True:
                    hT = hTs[sbi % 2]
                    for hh in range(2):
                        pb = 2 + hh
                        for c in range(8):
                            fw.op("pe", lambda e: e.matmul(bank(pb), lhsT=wk[:, c, hh * 128:(hh + 1) * 128], rhs=hT[:, c, :], start=(c == 0), stop=(c == 7)),
                                  [wk, hT], [PR[pb]])
                        fw.op("act", lambda e: e.activation(out=KT[:, hh, sbi * 512:(sbi + 1) * 512], in_=bank(pb), func=AF.Copy),
                              [PR[pb]], [rKT[sbi]])
                    for bl in range(4):
                        gb = sbi * 4 + bl
                        pb = 4 + (bl % 2)
                        for c in range(8):
                            fw.op("pe", lambda e: e.matmul(bank(pb)[:, 0:256], lhsT=hT[:, c, bl * 128:(bl + 1) * 128], rhs=wv[:, c, :], start=(c == 0), stop=(c == 7)),
                                  [wv, hT], [PR[pb]])
                        fw.op("dve", lambda e: e.tensor_copy(out=V[:, gb, :, 0:128], in_=bank(pb)[:, 0:256].rearrange("p (h v) -> p h v", h=2)),
                              [PR[pb]], [rV[sbi]])

            normS(0)
            for sbi in range(16):
                if sbi + 1 < 16:
                    interleave(fw, [lambda: normS(sbi + 1), lambda: kvS(sbi)], [1, 1])
                else:
                    kvS(sbi)
            nblk = nbc[0]
            if debug is not None and debug[0] == "kv" and hp == 0:
                with nc.sbuf_tensor("dbgt", [128, 2048], F32) as dt_:
                    dtb = Buf(dt_)
                    fw.op("dve", lambda e: e.tensor_copy(out=dtb[:, 0:1024], in_=KT[:, 1, 7168:8192]), rKT, [dtb])
                    fw.op("dve", lambda e: e.tensor_copy(out=dtb[:, 1024:2048].rearrange("p (b v) -> p b v", b=8), in_=V[:, 56:64, 1, 0:128]), rV, [dtb])
                    dbg_out(dtb[:, :], [dtb])
                    fw.barrier()
                    return nc
            for g in range(8):
                hT = hTs[g % 2]
                QT = QTs[g % 2]
                for qi in range(4):
                    j = 4 * g + qi
                    norm1_block(x_own[j * 128:(j + 1) * 128, :], xins[nblk % 3], xss[nblk % 2], tmpfs[nblk % 2], hT,
                                slice(qi * 128, (qi + 1) * 128), nblk % 2)
                    nblk += 1
                for hh in range(2):
                    pb = 2 + hh
                    for c in range(8):
                        fw.op("pe", lambda e, c=c, hh=hh, pb=pb: e.matmul(bank(pb), lhsT=wq[:, c, hh * 128:(hh + 1) * 128], rhs=hT[:, c, :], start=(c == 0), stop=(c == 7)),
                              [wq, hT], [PR[pb]])
                    fw.op("act", lambda e, hh=hh, pb=pb: e.activation(out=QT[:, hh, :], in_=bank(pb), func=AF.Copy, scale=0.125),
                          [PR[pb]], [QT])
                for hh in range(2):
                    kmax = 8 * g + 7

                    def emit_S(kb):
                        qmin = max(0, (kb - 8 * g) // 2)
                        c0 = qmin * 128
                        for half in range(2):
                            spb = 4 + 2 * (kb % 2) + half
                            rs = slice(half * 64, (half + 1) * 64)
                            fw.op("pe", lambda e: e.matmul(bank(spb)[:, c0:512], lhsT=KT[rs, hh, kb * 128:(kb + 1) * 128], rhs=QT[rs, hh, c0:512], start=True, stop=True),
                                  [rKT[kb // 4], QT], [PR[spb]])
                        for half in range(2):
                            spb = 4 + 2 * (kb % 2) + half
                            PT = PTs[2 * (kb % 2) + half]
                            fw.op("act", lambda e: e.activation(out=PT[:, c0:512], in_=bank(spb)[:, c0:512], func=AF.Exp),
                                  [PR[spb]], [PT])
                            for qi in range(qmin, 4):
                                j = 4 * g + qi
                                m = kb - 2 * j
                                if m in (0, 1):
                                    fw.op("pool", lambda e: e.tensor_tensor(out=PT[:, qi * 128:(qi + 1) * 128], in0=PT[:, qi * 128:(qi + 1) * 128], in1=maskb[:, m * 128:(m + 1) * 128], op=OP.mult),
                                          [PT, maskb], [PT])

                    def emit_PV(kb):
                        qmin = max(0, (kb - 8 * g) // 2)
                        for half in range(2):
                            PT = PTs[2 * (kb % 2) + half]
                            for qi in range(qmin, 4):
                                j = 4 * g + qi
                                ob = qi
                                ov = bank(ob)[:, half * 129:(half + 1) * 129]
                                fw.op("pe", lambda e: e.matmul(ov, lhsT=PT[:, qi * 128:(qi + 1) * 128], rhs=V[:, kb, hh, :], start=(kb == 0 and half == 0), stop=(kb == 2 * j + 1 and half == 1)),
                                      [PT, rV[kb // 4]], [PR[ob]])

                    emit_S(0)
                    for kb in range(kmax + 1):
                        if kb + 1 <= kmax:
                            emit_S(kb + 1)
                        emit_PV(kb)
                    for qi in range(4):
                        j = 4 * g + qi
                        ob = qi
                        at = ats[qi % 2]
                        ab = abs_[qi % 2]
                        o3 = bank(ob)[:, 0:258].rearrange("p (a b) -> p a b", a=2)
                        fw.op("dve", lambda e, o3=o3: e.reciprocal(out=asm[:, 0:2], in_=o3[:, :, 128]), [PR[ob]], [asm])
                        fw.op("dve", lambda e: e.tensor_tensor(out=asm[:, 2:3], in0=asm[:, 1:2], in1=neglam[:, :], op=OP.mult), [asm, neglam], [asm])
                        fw.op("dve", lambda e, o3=o3, at=at: e.tensor_scalar(out=at[:, :], in0=o3[:, 0, 0:128], scalar1=asm[:, 0:1], scalar2=None, op0=OP.mult), [PR[ob], asm], [at])
                        fw.op("dve", lambda e, o3=o3, at=at: e.scalar_tensor_tensor(out=at[:, :], in0=o3[:, 1, 0:128], scalar=asm[:, 2:3], in1=at[:, :], op0=OP.mult, op1=OP.add), [PR[ob], asm, at], [at])
                        fw.op("act", lambda e, at=at, ab=ab: e.activation(out=ab[:, :], in_=at[:, :], func=AF.Square, accum_out=asm[:, 3:4]), [at], [ab, asm])
                        rstd_of(asm, 3, 128, 4)
                        fw.op("dve", lambda e, at=at, ab=ab: e.scalar_tensor_tensor(out=ab[:, :], in0=at[:, :], scalar=asm[:, 4:5], in1=hg_bc[:, :], op0=OP.mult, op1=OP.mult), [at, asm, hg_bc], [ab])
                        tb = 6 + (qi % 2)
                        fw.op("pe", lambda e, ab=ab, tb=tb: e.transpose(out=bank_bf(tb)[:, 0:128], in_=ab[:, :], identity=identb[:, :]), [ab, identb], [PR[tb]])
                        yst = ysts[ysi[0] % 2]
                        ysi[0] += 1
                        fw.op("act", lambda e, tb=tb: e.activation(out=yst[:, :], in_=bank_bf(tb)[:, 0:128], func=AF.Copy), [PR[tb]], [yst])
                        rr_ = Reg()
                        fw.dma("sp", lambda e, j=j, hh=hh: e.dma_start(out=yaTd[hp * 2 + hh, :, j * 128:(j + 1) * 128], in_=yst[:, :]), [yst], [rr_])
                        yaregs.append(rr_)
            fw.barrier()
    def tl(name, shape, dt=F32):
        return Buf(nc.alloc_sbuf_tensor(name, list(shape), dt))

    wbab_v = wbab.rearrange("(c p) n -> p c n", p=128)
    wbbb_v = wbbb.rearrange("(c p) n -> p c n", p=128)
    woutb_v = woutb.rearrange("(c p) n -> p c n", p=128)
    wpqb_v = wpqb.rearrange("(c p) n -> p c n", p=128)
    Wsl = [tl(f"Wsl{i}", [128, 4096], BF16) for i in range(5)]
    wi = [0]

    def load_w(view3, c):
        w = Wsl[wi[0] % 5]
        wi[0] += 1
        fw.dma("sp", lambda e: e.dma_start(out=w[:, :].rearrange("p (c n) -> p c n", c=c), in_=view3), wregs, [w])
        return w, w[:, :].rearrange("p (c n) -> p c n", c=c)

    xgs = [[tl(f"xg{p}{b}", [128, D]) for b in range(2)] for p in range(2)]
    xsC = tl("xsC", [128, D], BF16)
    tmpfC = tl("tmpfC", [128, D])
    hTC = tl("hTC", [128, 8, 256], BF16)
    gTC = tl("gTC", [128, 16, 256], BF16)
    gu = tl("gu", [128, 512])
    gs = tl("gs", [128, 512])
    svn = tl("svn", [128, 512], BF16)
    ybt = tl("ybt", [128, 512], BF16)
    ybT = tl("ybT", [128, 4, 256], BF16)
    t5a = tl("t5a", [128, 256])
    t5b = tl("t5b", [128, 256])
    mT = tl("mT", [128, 8, 256], BF16)
    h2gs = [[tl(f"h2g{p}{b}", [128, D], BF16) for b in range(2)] for p in range(2)]
    st6 = tl("st6", [128, 12])
    scf = tl("scf", [128, 2048])
    scw = tl("scw", [128, 2048])
    tv = tl("tv", [128, 256])
    ti = tl("ti", [128, 256], U32)
    tif = tl("tif", [128, 256])
    cv = tl("cv", [128, 128])
    ci = tl("ci", [128, 128], U32)
    cif = tl("cif", [128, 128])
    gsm = tl("gsm", [128, 128])
    sm8 = tl("sm8", [128, 8])
    ak = tl("ak", [128, 128])
    bk = tl("bk", [128, 128])
    i1s = tl("i1s", [128, 128])
    i2s = tl("i2s", [128, 128])
    eidxf = tl("eidxf", [128, 128])
    eTs = [[tl(f"eT{p}{b}", [128, 128], U32) for b in range(2)] for p in range(2)]
    gTs_ = [[tl(f"gT_{p}{b}", [128, 128]) for b in range(2)] for p in range(2)]
    yaGs = [tl("yaG0", [128, 4, 256], BF16), tl("yaG1", [128, 4, 256], BF16)]
    tmpfL = ViewBuf(mod_bc.t[:, 0:1024])
    junkL = ViewBuf(mod_bc.t[:, 1024:2048].bitcast(BF16)[:, 0:1024])
    aT = tl("aT", [128, 128])
    coefT = tl("coefT", [128, 128])
    NGB = 9
    Gbufs = [tl(f"Gbuf{i}", [128, 2 * D], BF16) for i in range(NGB)]
    gbi = [0]

    gslots = [[f"d_g{i}", nc.alloc_semaphore(f"d_g{i}"), 0] for i in range(NGB)]

    def gather(table, idx_ap, rd):
        k = gbi[0] % NGB
        b_ = Gbufs[k]
        pre = [Gbufs[(k + 1) % NGB]] if k % 2 == 0 else []
        gbi[0] += 1
        fw.dma("pool", lambda e: e.indirect_dma_start(out=b_[:, :], out_offset=None, in_=table[:, :], in_offset=bass.IndirectOffsetOnAxis(ap=idx_ap, axis=0)),
               rd, [b_], slot=gslots[k], pre=pre)
        return b_
    Wc = tl("Wc", [128, 256], BF16)
    Zts = [tl(f"Zt{i}", [128, 128], BF16) for i in range(4)]
    acols = [tl(f"acol{i}", [128, 1]) for i in range(8)]
    ccols = [tl(f"ccol{i}", [128, 1]) for i in range(8)]
    thr16 = tl("thr16", [128, 16])
    io16 = tl("io16", [128, 16])
    ioi = tl("ioi", [128, 16], I32)

    fw.op("pool", lambda e: e.memset(Wc[:, :], 0.0), [], [Wc])
    fw.op("pool", lambda e: e.memset(Wc[:, 127:128], 1.0), [Wc], [Wc])
    fw.op("pool", lambda e: e.iota(ioi[:, :], pattern=[[1, 16]], base=0, channel_multiplier=0), [], [ioi])
    fw.op("dve", lambda e: e.tensor_copy(out=io16[:, :], in_=ioi[:, :]), [ioi], [io16])
    fw.op("dve", lambda e: e.tensor_scalar(out=thr16[:, :], in0=io16[:, :], scalar1=16.0, scalar2=None, op0=OP.mult), [io16], [thr16])
    def top16(src3, wrk3, vals3, idx3, n, regs):
        r_src, r_wrk, r_vals, r_idx = regs
        for l in range(n):
            fw.op("dve", lambda e: e.max(out=vals3[:, l, 0:8], in_=src3[:, l, :]), [r_src], [r_vals])
            fw.op("dve", lambda e: e.max_index(out=idx3[:, l, 0:8], in_max=vals3[:, l, 0:8], in_values=src3[:, l, :]), [r_src, r_vals], [r_idx])
            fw.op("dve", lambda e: e.match_replace(out=wrk3[:, l, :], in_to_replace=vals3[:, l, 0:8], in_values=src3[:, l, :], imm_value=NEG), [r_src, r_vals], [r_wrk])
            fw.op("dve", lambda e: e.max(out=vals3[:, l, 8:16], in_=wrk3[:, l, :]), [r_wrk], [r_vals])
            fw.op("dve", lambda e: e.max_index(out=idx3[:, l, 8:16], in_max=vals3[:, l, 8:16], in_values=wrk3[:, l, :]), [r_wrk, r_vals], [r_idx])

    NGC = 16 if debug is None else debug[2]

    def cmain(g):
        p = g % 2
        xg = xgs[p]
        h2g = h2gs[p]
        for bl in range(2):
            j = 2 * g + bl
            fw.dma("sp", lambda e: e.dma_start(out=xg[bl][:, :], in_=x_own[j * 128:(j + 1) * 128, :]), [], [xg[bl]])
            norm1_block(None, xg[bl], xsC, tmpfC, hTC, slice(bl * 128, (bl + 1) * 128), bl, add_eng="act")
        for u in range(4):
            w, w3 = load_w(winb_v[:, :, 2560 + u * 512:2560 + (u + 1) * 512], 8)
            for cc in range(4):
                ch = u * 4 + cc
                pb = ch % 2
                for c in range(8):
                    fw.op("pe", lambda e: e.matmul(bank(pb)[:, 0:256], lhsT=w3[:, c, cc * 128:(cc + 1) * 128], rhs=hTC[:, c, :], start=(c == 0), stop=(c == 7)),
                          [w, hTC], [PR[pb]])
                fw.op("act", lambda e: e.activation(out=gTC[:, ch, :], in_=bank(pb)[:, 0:256], func=AF.Sigmoid), [PR[pb]], [gTC])
        wu, wu3 = load_w(winb_v[:, :, 1536:2048], 8)
        wsv, wsv3 = load_w(winb_v[:, :, 2048:2560], 8)
        for bl in range(2):
            bs_ = slice(bl * 128, (bl + 1) * 128)
            for c in range(8):
                fw.op("pe", lambda e: e.matmul(bank(0), lhsT=hTC[:, c, bs_], rhs=wu3[:, c, :], start=(c == 0), stop=(c == 7)), [wu, hTC], [PR[0]])
            for c in range(8):
                fw.op("pe", lambda e: e.matmul(bank(1), lhsT=hTC[:, c, bs_], rhs=wsv3[:, c, :], start=(c == 0), stop=(c == 7)), [wsv, hTC], [PR[1]])
            fw.op("act", lambda e: e.activation(out=gu[:, :], in_=bank(0), func=AF.Gelu_apprx_tanh), [PR[0]], [gu])
            fw.op("act", lambda e: e.activation(out=gs[:, :], in_=bank(1), func=AF.Gelu_apprx_tanh), [PR[1]], [gs])
            fw.op("dve", lambda e: e.bn_stats(out=st6[:, 0:6], in_=gs[:, :]), [gs], [st6])
            fw.op("dve", lambda e: e.bn_aggr(out=st6[:, 6:8], in_=st6[:, 0:6]), [st6], [st6])
            rstd_of(st6, 7, 1.0, 8)
            fw.op("dve", lambda e: e.tensor_scalar(out=gs[:, :], in0=gs[:, :], scalar1=st6[:, 6:7], scalar2=st6[:, 8:9], op0=OP.subtract, op1=OP.mult), [gs, st6], [gs])
            fw.op("dve", lambda e: e.tensor_tensor(out=gs[:, :], in0=gs[:, :], in1=lng_bc[:, :], op=OP.mult), [gs, lng_bc], [gs])
            fw.op("dve", lambda e: e.tensor_tensor(out=svn[:, :], in0=gs[:, :], in1=lnb_bc[:, :], op=OP.add), [gs, lnb_bc], [svn])
            for gr in range(4):
                fw.op("pe", lambda e: e.matmul(bank(0)[:, gr * 128:(gr + 1) * 128], lhsT=wsT[:, gr, :], rhs=svn[:, gr * 128:(gr + 1) * 128], start=True, stop=True),
                      [wsT, svn], [PR[0]])
            for gr in range(4):
                fw.op("dve", lambda e: e.scalar_tensor_tensor(out=ybt[:, gr * 128:(gr + 1) * 128], in0=bank(0)[:, gr * 128:(gr + 1) * 128], scalar=bs_col[:, gr:gr + 1], in1=gu[:, gr * 128:(gr + 1) * 128], op0=OP.add, op1=OP.mult),
                      [PR[0], bs_col, gu], [ybt])
            for ch in range(4):
                fw.op("pe", lambda e: e.transpose(out=bank_bf(1)[:, ch * 128:(ch + 1) * 128], in_=ybt[:, ch * 128:(ch + 1) * 128], identity=identb[:, :]), [ybt, identb], [PR[1]])
            fw.op("act", lambda e: e.activation(out=ybT[:, :, bs_], in_=bank_bf(1)[:, 0:512].rearrange("p (c t) -> p c t", c=4), func=AF.Copy), [PR[1]], [ybT])
        wa, wa3 = load_w(wbab_v, 4)
        wb, wb3 = load_w(wbbb_v, 4)
        yaG = yaGs[p]
        fw.dma("sp", lambda e: e.dma_start(out=yaG[:, :, :], in_=yaTd[:, :, g * 256:(g + 1) * 256].rearrange("c p t -> p c t")), yaregs, [yaG])
        for dc in range(8):
            ba, bb = 0, 1
            for cc in range(4):
                fw.op("pe", lambda e: e.matmul(bank(ba)[:, 0:256], lhsT=wa3[:, cc, dc * 128:(dc + 1) * 128], rhs=yaG[:, cc, :], start=(cc == 0), stop=(cc == 3)),
                      [wa, yaG], [PR[ba]])
            for cc in range(4):
                fw.op("pe", lambda e: e.matmul(bank(bb)[:, 0:256], lhsT=wb3[:, cc, dc * 128:(dc + 1) * 128], rhs=ybT[:, cc, :], start=(cc == 0), stop=(cc == 3)),
                      [wb, ybT], [PR[bb]])
            fw.op("dve", lambda e: e.tensor_tensor(out=t5a[:, :], in0=bank(ba)[:, 0:256], in1=gTC[:, dc, :], op=OP.mult), [PR[ba], gTC], [t5a])
            fw.op("dve", lambda e: e.tensor_tensor(out=t5b[:, :], in0=bank(bb)[:, 0:256], in1=gTC[:, 8 + dc, :], op=OP.mult), [PR[bb], gTC], [t5b])
            fw.op("dve", lambda e: e.tensor_tensor(out=mT[:, dc, :], in0=t5a[:, :], in1=t5b[:, :], op=OP.add), [t5a, t5b], [mT])
        wo0, wo03 = load_w(woutb_v[:, :, 0:512], 8)
        wo1, wo13 = load_w(woutb_v[:, :, 512:1024], 8)
        for bl in range(2):
            bs_ = slice(bl * 128, (bl + 1) * 128)
            for half, (wo, wo3) in enumerate(((wo0, wo03), (wo1, wo13))):
                for c in range(8):
                    fw.op("pe", lambda e: e.matmul(bank(half), lhsT=mT[:, c, bs_], rhs=wo3[:, c, :], start=(c == 0), stop=(c == 7)), [wo, mT], [PR[half]])
            fw.op("dve", lambda e: e.tensor_tensor(out=tmpfC[:, :], in0=pair(0), in1=mod_bc[:, 2 * D:3 * D], op=OP.mult), [PR[0], PR[1], mod_bc], [tmpfC])
            fw.op("dve", lambda e: e.tensor_tensor(out=xg[bl][:, :], in0=xg[bl][:, :], in1=tmpfC[:, :], op=OP.add), [xg[bl], tmpfC], [xg[bl]])
        if debug is not None and debug[0] == "x2":
            dbg_out(xg[0][:, :], [xg[0]], dbg_d[:, 0:D])
            dbg_out(xg[1][:, :], [xg[1]], dbg_d[:, D:2 * D])
            return
        for bl in range(2):
            ssb = ss_r[ss_i[0] % 4]
            ss_i[0] += 1
            fw.op("act", lambda e: e.activation(out=xsC[:, :], in_=xg[bl][:, :], func=AF.Square, accum_out=ssb[:, 0:1]), [xg[bl]], [xsC, ssb])
            rstd_of(ssb, 0, D, 1)
            fw.op("dve", lambda e: e.scalar_tensor_tensor(out=tmpfC[:, :], in0=xg[bl][:, :], scalar=ssb[:, 1:2], in1=mod_bc[:, 4 * D:5 * D], op0=OP.mult, op1=OP.mult), [xg[bl], ssb, mod_bc], [tmpfC])
            fw.op("dve", lambda e: e.tensor_tensor(out=h2g[bl][:, :], in0=tmpfC[:, :], in1=mod_bc[:, 3 * D:4 * D], op=OP.add), [tmpfC, mod_bc], [h2g[bl]])
            for c in range(8):
                fw.op("pe", lambda e: e.transpose(out=bank_bf(bl)[:, c * 128:(c + 1) * 128], in_=h2g[bl][:, c * 128:(c + 1) * 128], identity=identb[:, :]), [h2g[bl], identb], [PR[bl]])
            fw.op("act", lambda e: e.activation(out=hTC[:, :, bl * 128:(bl + 1) * 128], in_=bank_bf(bl)[:, 0:1024].rearrange("p (c t) -> p c t", c=8), func=AF.Copy), [PR[bl]], [hTC])
        for u in range(4):
            w, w3 = load_w(wpqb_v[:, :, u * 512:(u + 1) * 512], 8)
            for cc in range(4):
                l = u * 4 + cc
                pb = l % 2
                for c in range(8):
                    fw.op("pe", lambda e: e.matmul(bank(pb)[:, 0:256], lhsT=w3[:, c, cc * 128:(cc + 1) * 128], rhs=hTC[:, c, :], start=(c == 0), stop=(c == 7)), [w, hTC], [PR[pb]])
                fw.op("act", lambda e: e.activation(out=gTC[:, l, :], in_=bank(pb)[:, 0:256], func=AF.Copy), [PR[pb]], [gTC])
        for bl in range(2):
            eT = eTs[p][bl]
            gT_ = gTs_[p][bl]
            bs_ = slice(bl * 128, (bl + 1) * 128)
            for rnd in range(2):
                for l8 in range(8):
                    l = rnd * 8 + l8
                    fw.op("pe", lambda e: e.matmul(bank(l8 // 4)[:, (l8 % 4) * 128:(l8 % 4 + 1) * 128], lhsT=gTC[:, l, bs_], rhs=skT[:, l, :], start=True, stop=True), [gTC, skT], [PR[l8 // 4]])
                if rnd == 0:
                    fw.op("act", lambda e: e.activation(out=scf[:, 0:1024], in_=pair(0), func=AF.Copy), [PR[0], PR[1]], [scf])
                else:
                    fw.op("act", lambda e: e.activation(out=scf[:, 1024:2048], in_=pair(0), func=AF.Copy), [PR[0], PR[1]], [scf])
            r_src, r_wrk, r_vals, r_idx = scf, scw, tv, ti
            top16(scf[:, :].rearrange("p (l n) -> p l n", l=16), scw[:, :].rearrange("p (l n) -> p l n", l=16),
                  tv[:, :].rearrange("p (l k) -> p l k", l=16), ti[:, :].rearrange("p (l k) -> p l k", l=16), 16, (scf, scw, tv, ti))
            fw.op("dve", lambda e: e.tensor_copy(out=tif[:, :], in_=ti[:, :]), [ti], [tif])
            tv4 = tv[:, :].rearrange("p (h i k) -> p h i k", h=8, i=2)
            tif4 = tif[:, :].rearrange("p (h i k) -> p h i k", h=8, i=2)
            cand4 = scf[:, :].rearrange("p (h a b) -> p h a b", h=8, a=16)
            fw.op("dve", lambda e: e.tensor_tensor(out=cand4, in0=tv4[:, :, 0, :].unsqueeze(3).to_broadcast([128, 8, 16, 16]), in1=tv4[:, :, 1, :].unsqueeze(2).to_broadcast([128, 8, 16, 16]), op=OP.add), [tv], [scf])
            top16(scf[:, :].rearrange("p (h n) -> p h n", h=8), scw[:, :].rearrange("p (h n) -> p h n", h=8),
                  cv[:, :].rearrange("p (h k) -> p h k", h=8), ci[:, :].rearrange("p (h k) -> p h k", h=8), 8, (scf, scw, cv, ci))
            cv3 = cv[:, :].rearrange("p (h k) -> p h k", h=8)
            gsm3 = gsm[:, :].rearrange("p (h k) -> p h k", h=8)
            fw.op("dve", lambda e: e.tensor_tensor(out=gsm3, in0=cv3, in1=cv3[:, :, 0:1].to_broadcast([128, 8, 16]), op=OP.subtract), [cv], [gsm])
            fw.op("act", lambda e: e.activation(out=gsm[:, :], in_=gsm[:, :], func=AF.Exp), [gsm], [gsm])
            fw.op("dve", lambda e: e.tensor_reduce(out=sm8[:, :], in_=gsm3, axis=AX.X, op=OP.add), [gsm], [sm8])
            fw.op("dve", lambda e: e.reciprocal(out=sm8[:, :], in_=sm8[:, :]), [sm8], [sm8])
            fw.op("dve", lambda e: e.tensor_tensor(out=gsm3, in0=gsm3, in1=sm8[:, :].unsqueeze(2).to_broadcast([128, 8, 16]), op=OP.mult), [gsm, sm8], [gsm])
            fw.op("dve", lambda e: e.tensor_copy(out=cif[:, :], in_=ci[:, :]), [ci], [cif])
            oh4 = scw[:, :].rearrange("p (h k a) -> p h k a", h=8, k=16)
            cif3 = cif[:, :].rearrange("p (h k) -> p h k", h=8)
            ak3 = ak[:, :].rearrange("p (h k) -> p h k", h=8)
            bk3 = bk[:, :].rearrange("p (h k) -> p h k", h=8)
            thr_b = thr16[:, :].unsqueeze(1).unsqueeze(1).to_broadcast([128, 8, 16, 16])
            io_b = io16[:, :].unsqueeze(1).unsqueeze(1).to_broadcast([128, 8, 16, 16])
            fw.op("dve", lambda e: e.tensor_tensor(out=oh4, in0=cif3.unsqueeze(3).to_broadcast([128, 8, 16, 16]), in1=thr_b, op=OP.is_ge), [cif, thr16], [scw])
            fw.op("dve", lambda e: e.tensor_reduce(out=ak3, in_=oh4, axis=AX.X, op=OP.add), [scw], [ak])
            fw.op("dve", lambda e: e.tensor_scalar(out=ak[:, :], in0=ak[:, :], scalar1=-1.0, scalar2=None, op0=OP.add), [ak], [ak])
            fw.op("dve", lambda e: e.scalar_tensor_tensor(out=bk[:, :], in0=ak[:, :], scalar=-16.0, in1=cif[:, :], op0=OP.mult, op1=OP.add), [ak, cif], [bk])
            for (kk3, pidx, dst) in ((ak3, 0, i1s), (bk3, 1, i2s)):
                fw.op("dve", lambda e: e.tensor_tensor(out=oh4, in0=kk3.unsqueeze(3).to_broadcast([128, 8, 16, 16]), in1=io_b, op=OP.is_equal), [ak, bk, io16], [scw])
                fw.op("dve", lambda e: e.tensor_tensor(out=oh4, in0=oh4, in1=tif4[:, :, pidx, :].unsqueeze(2).to_broadcast([128, 8, 16, 16]), op=OP.mult), [scw, tif], [scw])
                fw.op("dve", lambda e: e.tensor_reduce(out=dst[:, :].rearrange("p (h k) -> p h k", h=8), in_=oh4, axis=AX.X, op=OP.add), [scw], [dst])
            fw.op("dve", lambda e: e.scalar_tensor_tensor(out=eidxf[:, :], in0=i1s[:, :], scalar=128.0, in1=i2s[:, :], op0=OP.mult, op1=OP.add), [i1s, i2s], [eidxf])
            fw.op("pe", lambda e: e.transpose(out=bank(0)[:, 0:128], in_=eidxf[:, :], identity=identf[:, :]), [eidxf, identf], [PR[0]])
            fw.op("dve", lambda e: e.tensor_copy(out=eT[:, :], in_=bank(0)[:, 0:128]), [PR[0]], [eT])
            fw.op("pe", lambda e: e.transpose(out=bank(1)[:, 0:128], in_=gsm[:, :], identity=identf[:, :]), [gsm, identf], [PR[1]])
            fw.op("act", lambda e: e.activation(out=gT_[:, :], in_=bank(1)[:, 0:128], func=AF.Copy), [PR[1]], [gT_])

    def loops(g):
        p = g % 2
        xg = xgs[p]
        h2g = h2gs[p]
        for bl in range(2):
            j = 2 * g + bl
            eT = eTs[p][bl]
            gT_ = gTs_[p][bl]
            gbs = {}

            def s_gather(t):
                gbs[t] = gather(pdu, eT[:, t:t + 1], [eT] + tregs)

            def s_bcast(t):
                hbp = 2 + (t % 2)
                for half in range(2):
                    fw.op("pe", lambda e: e.matmul(bank(2 * hbp + half), lhsT=identb[:, t:t + 1].to_broadcast([128, 128]), rhs=h2g[bl][:, half * 512:(half + 1) * 512], start=True, stop=True),
                          [identb, h2g[bl]], [PR[2 * hbp + half]])

            def s_dot(t):
                hbp = 2 + (t % 2)
                ac = acols[t % 8]
                cc_ = ccols[t % 8]
                gb_ = gbs[t]
                fw.op("dve", lambda e: e.scalar_tensor_tensor(out=junkL[:, :], in0=gb_[:, 0:D], scalar=1.0, in1=pair(hbp), op0=OP.mult, op1=OP.mult, accum_out=ac[:, 0:1]),
                      [gb_, PR[2 * hbp], PR[2 * hbp + 1]], [junkL, ac])
                fw.op("act", lambda e: e.activation(out=cc_[:, 0:1], in_=ac[:, 0:1], func=AF.Gelu_apprx_tanh), [ac], [cc_])

            def s_up(t):
                cc_ = ccols[t % 8]
                zt = Zts[t % 4]
                gb_ = gbs.pop(t)
                fw.op("dve", lambda e: e.tensor_scalar(out=zt[:, :], in0=Wc[:, 127 - t:255 - t], scalar1=cc_[:, 0:1], scalar2=gT_[:, t:t + 1], op0=OP.mult, op1=OP.mult),
                      [Wc, cc_, gT_], [zt])
                for half in range(2):
                    fw.op("pe", lambda e: e.matmul(bank(2 + half), lhsT=zt[:, :], rhs=gb_[:, D + half * 512:D + (half + 1) * 512], start=(t == 0), stop=(t == 127)),
                          [zt, gb_], [PR[2 + half]])

            AH = NGB - 2
            for t in range(min(AH, 128)):
                s_gather(t)
            s_bcast(0)
            for t in range(128):
                if t + AH < 128:
                    s_gather(t + AH)
                if t + 1 < 128:
                    s_bcast(t + 1)
                s_dot(t)
                if t >= 1:
                    s_up(t - 1)
            s_up(127)
            fw.op("dve", lambda e: e.tensor_tensor(out=tmpfL[:, :], in0=pair(1), in1=mod_bc[:, 5 * D:6 * D], op=OP.mult), [PR[2], PR[3], mod_bc], [tmpfL])
            fw.op("dve", lambda e: e.tensor_tensor(out=xg[bl][:, :], in0=xg[bl][:, :], in1=tmpfL[:, :], op=OP.add), [xg[bl], tmpfL], [xg[bl]])
            ssb = ss_r[ss_i[0] % 4]
            ss_i[0] += 1
            fw.op("act", lambda e: e.activation(out=junkL[:, :], in_=xg[bl][:, :], func=AF.Square, accum_out=ssb[:, 0:1]), [xg[bl]], [junkL, ssb])
            rstd_of(ssb, 0, D, 1)
            fw.op("dve", lambda e: e.scalar_tensor_tensor(out=tmpfL[:, :], in0=xg[bl][:, :], scalar=ssb[:, 1:2], in1=fg_bc[:, :], op0=OP.mult, op1=OP.mult), [xg[bl], ssb, fg_bc], [tmpfL])
            fw.dma("sp", lambda e: e.dma_start(out=out_d[j * 128:(j + 1) * 128, :], in_=tmpfL[:, :]), [tmpfL], [r_none])

    cmain(0)
    if debug is not None and debug[0] == "x2":
        fw.barrier()
        return nc
    for g in range(NGC):
        if g + 1 < NGC:
            interleave(fw, [lambda: loops(g), lambda: cmain(g + 1)], [2, 1])
        else:
            loops(g)
    fw.barrier()
    return nc


def _prep_inputs(inputs):
    g = {k: np.asarray(v) for k, v in inputs.items()}
    x = np.ascontiguousarray(g["x"], dtype=np.float32)
    shared = {
        "w_ada": g["w_ada"][0], "b_ada": g["b_ada"], "norm1_g": g["norm1_g"], "w_in": g["w_in"][0],
        "da_lambda_q1": g["da_lambda_q1"], "da_lambda_k1": g["da_lambda_k1"],
        "da_lambda_q2": g["da_lambda_q2"], "da_lambda_k2": g["da_lambda_k2"],
        "da_head_g": g["da_head_g"], "sg_ln_g": g["sg_ln_g"], "sg_ln_b": g["sg_ln_b"],
        "sg_w": g["sg_w"][0], "sg_b": g["sg_b"][0], "w_branch_a": g["w_branch_a"][0],
        "w_branch_b": g["w_branch_b"][0], "w_out": g["w_out"][0], "norm2_g": g["norm2_g"],
        "peer_w_query": g["peer_w_query"][0], "peer_sub_keys": g["peer_sub_keys"][0].reshape(16, 128, 128),
        "peer_down": g["peer_down"][0], "peer_up": g["peer_up"][0], "final_g": g["final_g"].reshape(1, D),
    }
    shared = {k: np.ascontiguousarray(v, dtype=np.float32) for k, v in shared.items()}
    kk = np.arange(128)[:, None] // 64
    qq = np.arange(128)[None, :] // 64
    diag = (kk <= qq).astype(np.float32)
    in_maps = []
    for core in range(8):
        b, par = core // 2, core % 2
        xb = x[b]
        xo = np.ascontiguousarray(xb.reshape(NB, 128, D)[par::2].reshape(NOWN * 128, D))
        if par == 0:
            am = np.concatenate([diag, np.zeros((128, 128), np.float32)], axis=1)
        else:
            am = np.concatenate([np.ones((128, 128), np.float32), diag], axis=1)
        m = dict(shared)
        m["x_b"] = xb
        m["x_own"] = xo
        m["c_b"] = np.ascontiguousarray(g["c"][b:b + 1], dtype=np.float32)
        m["amask"] = np.ascontiguousarray(am)
        in_maps.append(m)
    return in_maps


def kernel(**inputs):
    in_maps = _prep_inputs(inputs)
    nc = build()
    res = run_bass_kernel_spmd(nc, in_maps, core_ids=list(range(8)))
    out = np.zeros((4, S, D), np.float32)
    for core in range(8):
        b, par = core // 2, core % 2
        o = np.asarray(res.results[core]["out"]).reshape(NOWN, 128, D)
        out[b].reshape(NB, 128, D)[par::2] = o
    return out
```

```python
import numpy as np
from contextlib import ExitStack
import concourse.bass as bass
import concourse.mybir as mybir
from concourse.bass_utils import run_bass_kernel_spmd

F32 = mybir.dt.float32
BF16 = mybir.dt.bfloat16
I32 = mybir.dt.int32
U32 = mybir.dt.uint32
AF = mybir.ActivationFunctionType
OP = mybir.AluOpType
AX = mybir.AxisListType

D = 1024
S = 8192
NB = 64
NOWN = 32
EPS = 1e-6
LAM_INIT = 0.8 - 0.6 * 1.0
NEG = -1.0e30
SEM_LIMIT = 30000


class Reg:
    def __init__(self):
        self.w = {}
        self.r = {}


class Buf(Reg):
    def __init__(self, t):
        Reg.__init__(self)
        self.t = t

    def __getitem__(self, k):
        return self.t[k]


class FW:
    def __init__(self, nc):
        self.nc = nc
        self.engs = dict(pe=nc.tensor, act=nc.scalar, dve=nc.vector, pool=nc.gpsimd, sp=nc.sync)
        self.cur = {}
        self.nsem = 0
        self.known = {k: {} for k in self.engs}
        for k in self.engs:
            self._newsem(k)
        self.dsem = {}
        for q, n in (("sp", 12), ("act", 4), ("pool", 12)):
            self.dsem[q] = [[f"d_{q}{i}", nc.alloc_semaphore(f"d_{q}{i}"), 0] for i in range(n)]
        self.drr = {q: 0 for q in self.dsem}
        self.ninst = 0
        self.hook = None

    def _newsem(self, k):
        name = f"e_{k}{self.nsem}"
        self.cur[k] = [name, self.nc.alloc_semaphore(name), 0]
        self.nsem += 1

    def _deps(self, reads, writes):
        deps = {}

        def add(d):
            for name, (s, v) in d.items():
                if name not in deps or deps[name][1] < v:
                    deps[name] = (s, v)
        for b in reads:
            add(b.w)
        for b in writes:
            add(b.w)
            add(b.r)
        return deps

    def _wait(self, k, deps):
        eng = self.engs[k]
        for name, (s, v) in deps.items():
            if k == "pe" and name.startswith("e_pe"):
                continue
            if self.known[k].get(name, 0) >= v:
                continue
            eng.wait_ge(s, v)
            self.known[k][name] = v
            self.ninst += 1

    def _record(self, ev, reads, writes):
        name, s, v = ev
        for b in reads:
            b.r[name] = (s, v)
        for b in writes:
            b.w = {name: (s, v)}
            b.r = {}

    def op(self, k, fn, reads, writes):
        self._wait(k, self._deps(reads, writes))
        ins = fn(self.engs[k])
        c = self.cur[k]
        if c[2] >= SEM_LIMIT:
            self._newsem(k)
            c = self.cur[k]
        c[2] += 1
        ins.then_inc(c[1], 1)
        self.ninst += 1
        self._record((c[0], c[1], c[2]), reads, writes)
        if self.hook is not None:
            self.hook()

    def dma(self, q, fn, reads, writes, slot=None, pre=()):
        if slot is None:
            slots = self.dsem[q]
            i = self.drr[q]
            self.drr[q] = (i + 1) % len(slots)
            sl = slots[i]
        else:
            sl = slot
        deps = self._deps(reads, list(writes) + list(pre))
        if slot is None and sl[2] > 0:
            deps[sl[0]] = (sl[1], sl[2])
        self._wait(q, deps)
        ins = fn(self.engs[q])
        sl[2] += 16
        ins.then_inc(sl[1], 16)
        self.ninst += 1
        self._record((sl[0], sl[1], sl[2]), reads, writes)
        self.last_written = writes[0] if writes else None
        if self.hook is not None:
            self.hook()

    def barrier(self):
        evs = {}
        for k, c in self.cur.items():
            if c[2] > 0:
                evs[c[0]] = (c[1], c[2])
        for q, slots in self.dsem.items():
            for sl in slots:
                if sl[2] > 0:
                    evs[sl[0]] = (sl[1], sl[2])
        for k in self.engs:
            self._wait(k, dict(evs))


def interleave(fw, fns, weights):
    import threading
    n = len(fns)
    sems = [threading.Semaphore(0) for _ in fns]
    done = [False] * n
    st = {"cur": 0, "cnt": 0}
    errs = []
    fin = threading.Semaphore(0)

    def nxt(i):
        for k in range(1, n + 1):
            j = (i + k) % n
            if not done[j]:
                return j
        return None

    def hook():
        i = st["cur"]
        st["cnt"] += 1
        if st["cnt"] >= weights[i]:
            st["cnt"] = 0
            j = nxt(i)
            if j is not None and j != i:
                st["cur"] = j
                sems[j].release()
                sems[i].acquire()

    def runner(i):
        sems[i].acquire()
        try:
            fns[i]()
        except BaseException as e:
            errs.append(e)
        done[i] = True
        j = nxt(i)
        if j is not None:
            st["cur"] = j
            st["cnt"] = 0
            sems[j].release()
        else:
            fin.release()

    ths = [threading.Thread(target=runner, args=(i,)) for i in range(n)]
    old = fw.hook
    fw.hook = hook
    for t in ths:
        t.start()
    st["cur"] = 0
    sems[0].release()
    fin.acquire()
    for t in ths:
        t.join()
    fw.hook = old
    if errs:
        raise errs[0]


class ViewBuf(Reg):
    def __init__(self, ap):
        Reg.__init__(self)
        self.ap_ = ap

    def __getitem__(self, k):
        return self.ap_[k]


def build(debug=None):
    nc = bass.Bass("TRN2", target_bir_lowering=False)
    fw = FW(nc)

    def din(name, shape, dt=F32):
        return nc.dram_tensor(name, list(shape), dt, kind="ExternalInput").ap()

    x_b = din("x_b", [S, D])
    x_own = din("x_own", [NOWN * 128, D])
    c_b = din("c_b", [1, D])
    w_ada = din("w_ada", [D, 6 * D])
    b_ada = din("b_ada", [1, 6 * D])
    norm1_g = din("norm1_g", [1, D])
    w_in = din("w_in", [D, 4608])
    lq1 = din("da_lambda_q1", [1, 64])
    lk1 = din("da_lambda_k1", [1, 64])
    lq2 = din("da_lambda_q2", [1, 64])
    lk2 = din("da_lambda_k2", [1, 64])
    head_g = din("da_head_g", [1, 128])
    ln_g = din("sg_ln_g", [1, 512])
    ln_b = din("sg_ln_b", [1, 512])
    sg_w = din("sg_w", [4, 128, 128])
    sg_b = din("sg_b", [4, 128])
    w_ba = din("w_branch_a", [512, D])
    w_bb = din("w_branch_b", [512, D])
    w_out = din("w_out", [D, D])
    norm2_g = din("norm2_g", [1, D])
    w_pq = din("peer_w_query", [D, 2048])
    sub_keys = din("peer_sub_keys", [16, 128, 128])
    p_down = din("peer_down", [16384, D])
    p_up = din("peer_up", [16384, D])
    final_g = din("final_g", [1, D])
    amask = din("amask", [128, 256])
    out_d = nc.dram_tensor("out", [NOWN * 128, D], F32, kind="ExternalOutput").ap()
    dbg_d = None
    if debug is not None:
        dbg_d = nc.dram_tensor("dbg", list(debug[1]), F32, kind="ExternalOutput").ap()

    winb = nc.dram_tensor("winb", [D, 4608], BF16, kind="Internal").ap()
    wbab = nc.dram_tensor("wbab", [512, D], BF16, kind="Internal").ap()
    wbbb = nc.dram_tensor("wbbb", [512, D], BF16, kind="Internal").ap()
    woutb = nc.dram_tensor("woutb", [D, D], BF16, kind="Internal").ap()
    wpqb = nc.dram_tensor("wpqb", [D, 2048], BF16, kind="Internal").ap()
    modscr = nc.dram_tensor("modscr", [1, 2048], F32, kind="Internal").ap()
    pdu = nc.dram_tensor("pdu", [16384, 2 * D], BF16, kind="Internal").ap()
    tregs = []
    tconv = [(pdu[:, hf * D:(hf + 1) * D], src, r0) for hf, src in enumerate((p_down, p_up)) for r0 in range(0, 16384, 1024)]
    wregs = []
    r_modscr = Reg()
    r_none = Reg()
    yaregs = []

    def sb(name, shape, dt=F32):
        return Buf(nc.alloc_sbuf_tensor(name, list(shape), dt))

    P2 = [nc.alloc_psum_tensor(f"ps{i}", [128, 1024], F32) for i in range(4)]
    PR = [Reg() for _ in range(8)]

    def bank(i):
        return P2[i // 2][:, (i % 2) * 512:(i % 2 + 1) * 512]

    def bank_bf(i):
        return P2[i // 2][:, :].bitcast(BF16)[:, (i % 2) * 1024:(i % 2 + 1) * 1024]

    def pair(i):
        return P2[i][:, :]

    identf = sb("identf", [128, 128])
    identb = sb("identb", [128, 128], BF16)
    mod_bc = sb("mod_bc", [128, 6 * D])
    fg_bc = sb("fg_bc", [128, D])
    yaTd = nc.dram_tensor("yaTd", [4, 128, NOWN * 128], BF16, kind="Internal").ap()
    r_yaT = [Reg() for _ in range(8)]
    ysts = [sb("yst0", [128, 128], BF16), sb("yst1", [128, 128], BF16)]
    ysi = [0]
    A1col = sb("A1col", [128, 8])
    B1col = sb("B1col", [128, 8])
    neglam = sb("neglam", [128, 1])
    hg_bc = sb("hg_bc", [128, 128])
    lng_bc = sb("lng_bc", [128, 512])
    lnb_bc = sb("lnb_bc", [128, 512])
    bs_col = sb("bs_col", [128, 4])
    maskb = sb("maskb", [128, 256], BF16)
    wsT = sb("wsT", [128, 4, 128], BF16)
    skT = sb("skT", [128, 16, 128], BF16)
    ss_r = [sb(f"ss{i}", [128, 4]) for i in range(4)]
    ss_i = [0]

    def dbg_out(buf_ap, reads, dst=None):
        fw.dma("sp", lambda e: e.dma_start(out=(dbg_d if dst is None else dst), in_=buf_ap), reads, [r_none])

    fw.op("pool", lambda e: e.memset(identf[:, :], 1.0), [], [identf])
    fw.op("pool", lambda e: e.affine_select(out=identf[:, :], in_=identf[:, :], pattern=[[-1, 128]],
                                            compare_op=OP.is_equal, fill=0.0, base=0, channel_multiplier=1),
          [identf], [identf])
    fw.op("dve", lambda e: e.tensor_copy(out=identb[:, :], in_=identf[:, :]), [identf], [identb])

    for (dst, src, ncol) in ((winb, w_in, 4608), (wbab, w_ba, D), (wbbb, w_bb, D), (woutb, w_out, D), (wpqb, w_pq, 2048)):
        for c0 in range(0, ncol, 512):
            fw.dma("pool", lambda e, dst=dst, src=src, c0=c0: e.dma_start(out=dst[:, c0:c0 + 512], in_=src[:, c0:c0 + 512]),
                   [], [Reg()])
            wregs.append(fw.last_written)

    with nc.allow_non_contiguous_dma(reason="tiny param loads"):
        n1g_col = sb("n1g_col", [128, 8])
        c_col = sb("c_col", [128, 8])
        fw.dma("sp", lambda e: e.dma_start(out=n1g_col[:, :], in_=norm1_g[0, :].rearrange("(c p) -> p c", p=128)), [], [n1g_col])
        fw.dma("sp", lambda e: e.dma_start(out=c_col[:, :], in_=c_b[0, :].rearrange("(c p) -> p c", p=128)), [], [c_col])
        fw.dma("sp", lambda e: e.dma_start(out=bs_col[:, :], in_=sg_b.rearrange("g p -> p g")), [], [bs_col])
    fw.dma("sp", lambda e: e.dma_start(out=mod_bc[:, :], in_=b_ada[0:1, :].partition_broadcast(128)), [], [mod_bc])
    fw.dma("sp", lambda e: e.dma_start(out=fg_bc[:, :], in_=final_g[0:1, :].partition_broadcast(128)), [], [fg_bc])
    fw.dma("sp", lambda e: e.dma_start(out=hg_bc[:, :], in_=head_g[0:1, :].partition_broadcast(128)), [], [hg_bc])
    fw.dma("sp", lambda e: e.dma_start(out=lng_bc[:, :], in_=ln_g[0:1, :].partition_broadcast(128)), [], [lng_bc])
    fw.dma("sp", lambda e: e.dma_start(out=lnb_bc[:, :], in_=ln_b[0:1, :].partition_broadcast(128)), [], [lnb_bc])
    fw.op("dve", lambda e: e.tensor_scalar(out=hg_bc[:, :], in0=hg_bc[:, :], scalar1=1.0 - LAM_INIT, scalar2=None, op0=OP.mult),
          [hg_bc], [hg_bc])

    with ExitStack() as es:
        def tl(name, shape, dt=F32):
            return es.enter_context(nc.sbuf_tensor(name, list(shape), dt))
        wa0_t = tl("s0a", [128, 8, 512]); wa1_t = tl("s0b", [128, 8, 512]); csbc_t = tl("s0c", [128, 8, 128])
        lam_t = tl("s0d", [128, 4, 64]); n2g_t = tl("s0e", [128, 1024]); mcol_t = tl("s0f", [128, 16])
        tmp0_t = tl("s0g", [128, 512]); lsc_t = tl("s0h", [128, 8])
        wab = [Buf(wa0_t), Buf(wa1_t)]
        csbc = Buf(csbc_t)
        lam4 = Buf(lam_t)
        n2g = Buf(n2g_t)
        mcol = Buf(mcol_t)
        tmp0 = Buf(tmp0_t)
        lsc = Buf(lsc_t)
        for i, src in enumerate((lq1, lk1, lq2, lk2)):
            fw.dma("sp", lambda e, i=i, src=src: e.dma_start(out=lam4[:, i, :], in_=src[0:1, :].partition_broadcast(128)), [], [lam4])
        fw.op("dve", lambda e: e.tensor_tensor(out=lam4[:, 0, :], in0=lam4[:, 0, :], in1=lam4[:, 1, :], op=OP.mult), [lam4], [lam4])
        fw.op("dve", lambda e: e.tensor_tensor(out=lam4[:, 2, :], in0=lam4[:, 2, :], in1=lam4[:, 3, :], op=OP.mult), [lam4], [lam4])
        fw.op("dve", lambda e: e.tensor_reduce(out=lsc[:, 0:1], in_=lam4[:, 0, :], axis=AX.X, op=OP.add), [lam4], [lsc])
        fw.op("dve", lambda e: e.tensor_reduce(out=lsc[:, 1:2], in_=lam4[:, 2, :], axis=AX.X, op=OP.add), [lam4], [lsc])
        fw.op("act", lambda e: e.activation(out=lsc[:, 2:4], in_=lsc[:, 0:2], func=AF.Exp), [lsc], [lsc])
        fw.op("dve", lambda e: e.tensor_tensor(out=lsc[:, 4:5], in0=lsc[:, 3:4], in1=lsc[:, 2:3], op=OP.subtract), [lsc], [lsc])
        fw.op("dve", lambda e: e.tensor_scalar(out=neglam[:, :], in0=lsc[:, 4:5], scalar1=-LAM_INIT, scalar2=None, op0=OP.add),
              [lsc], [neglam])
        fw.dma("sp", lambda e: e.dma_start(out=tmp0[:, 0:256], in_=amask[:, :]), [], [tmp0])
        fw.op("dve", lambda e: e.tensor_copy(out=maskb[:, :], in_=tmp0[:, 0:256]), [tmp0], [maskb])
        for g in range(4):
            fw.dma("sp", lambda e, g=g: e.dma_start(out=tmp0[:, 0:128], in_=sg_w[g, :, :]), [], [tmp0])
            fw.op("dve", lambda e: e.memset(tmp0[0:64, 64:128], 0.0), [tmp0], [tmp0])
            fw.op("pe", lambda e: e.transpose(out=bank(0)[:, 0:128], in_=tmp0[:, 0:128], identity=identf[:, :]), [tmp0, identf], [PR[0]])
            fw.op("act", lambda e, g=g: e.activation(out=wsT[:, g, :], in_=bank(0)[:, 0:128], func=AF.Copy), [PR[0]], [wsT])
        for l in range(16):
            fw.dma("sp", lambda e, l=l: e.dma_start(out=tmp0[:, 0:128], in_=sub_keys[l, :, :]), [], [tmp0])
            fw.op("pe", lambda e: e.transpose(out=bank(1)[:, 0:128], in_=tmp0[:, 0:128], identity=identf[:, :]), [tmp0, identf], [PR[1]])
            fw.op("act", lambda e, l=l: e.activation(out=skT[:, l, :], in_=bank(1)[:, 0:128], func=AF.Copy), [PR[1]], [skT])
        fw.op("act", lambda e: e.activation(out=c_col[:, :], in_=c_col[:, :], func=AF.Silu), [c_col], [c_col])
        fw.op("dve", lambda e: e.tensor_copy(out=csbc[:, :, :], in_=c_col[:, :].unsqueeze(2).to_broadcast([128, 8, 128])), [c_col], [csbc])
        w_ada_v = w_ada.rearrange("(c p) n -> p c n", p=128)
        for ec in range(12):
            wb_ = wab[ec % 2]
            fw.dma("sp" if ec % 2 == 0 else "act", lambda e, ec=ec, wb_=wb_: e.dma_start(out=wb_[:, :, :], in_=w_ada_v[:, :, ec * 512:(ec + 1) * 512]), [], [wb_])
            pb = 2 + (ec % 2)
            for c in range(8):
                fw.op("pe", lambda e, c=c, wb_=wb_, pb=pb: e.matmul(bank(pb), lhsT=csbc[:, c, :], rhs=wb_[:, c, :], start=(c == 0), stop=(c == 7)),
                      [csbc, wb_], [PR[pb]])
            fw.op("dve", lambda e, ec=ec, pb=pb: e.tensor_tensor(out=mod_bc[:, ec * 512:(ec + 1) * 512], in0=bank(pb), in1=mod_bc[:, ec * 512:(ec + 1) * 512], op=OP.add),
                  [PR[pb], mod_bc], [mod_bc])
        fw.dma("sp", lambda e: e.dma_start(out=modscr[0:1, :], in_=mod_bc[0:1, 0:2048]), [mod_bc], [r_modscr])
        with nc.allow_non_contiguous_dma(reason="tiny param loads"):
            fw.dma("sp", lambda e: e.dma_start(out=mcol[:, :], in_=modscr[0, :].rearrange("(c p) -> p c", p=128)), [r_modscr], [mcol])
        fw.op("dve", lambda e: e.tensor_copy(out=B1col[:, :], in_=mcol[:, 0:8]), [mcol], [B1col])
        fw.op("dve", lambda e: e.scalar_tensor_tensor(out=A1col[:, :], in0=mcol[:, 8:16], scalar=1.0, in1=n1g_col[:, :], op0=OP.add, op1=OP.mult),
              [mcol, n1g_col], [A1col])
        fw.dma("sp", lambda e: e.dma_start(out=n2g[:, :], in_=norm2_g[0:1, :].partition_broadcast(128)), [], [n2g])
        fw.op("dve", lambda e: e.scalar_tensor_tensor(out=mod_bc[:, 4 * D:5 * D], in0=mod_bc[:, 4 * D:5 * D], scalar=1.0, in1=n2g[:, :], op0=OP.add, op1=OP.mult),
              [mod_bc, n2g], [mod_bc])
        fw.barrier()
    if debug is not None and debug[0] == "mod":
        dbg_out(mod_bc[:, :], [mod_bc])
        fw.barrier()
        return nc

    A1bc = A1col[:, :].unsqueeze(2).to_broadcast([128, 8, 128])
    B1bc = B1col[:, :].unsqueeze(2).to_broadcast([128, 8, 128])

    def rstd_of(ssbuf, col, n, tmpcol):
        fw.op("dve", lambda e: e.tensor_scalar(out=ssbuf[:, tmpcol:tmpcol + 1], in0=ssbuf[:, col:col + 1], scalar1=1.0 / n, scalar2=EPS, op0=OP.mult, op1=OP.add),
              [ssbuf], [ssbuf])
        fw.op("act", lambda e: e.activation(out=ssbuf[:, tmpcol:tmpcol + 1], in_=ssbuf[:, tmpcol:tmpcol + 1], func=AF.Ln), [ssbuf], [ssbuf])
        fw.op("act", lambda e: e.activation(out=ssbuf[:, tmpcol:tmpcol + 1], in_=ssbuf[:, tmpcol:tmpcol + 1], func=AF.Exp, scale=-0.5), [ssbuf], [ssbuf])

    def norm1_block(src_rows, xin, xs, tmpf, hT, hcols, pb, add_eng="pool"):
        ssb = ss_r[ss_i[0] % 4]
        ss_i[0] += 1
        if src_rows is not None:
            fw.dma("sp", lambda e: e.dma_start(out=xin[:, :], in_=src_rows), [], [xin])
        fw.op("act", lambda e: e.activation(out=xs[:, :], in_=xin[:, :], func=AF.Square, accum_out=ssb[:, 0:1]), [xin], [xs, ssb])
        rstd_of(ssb, 0, D, 1)
        if add_eng == "act":
            fw.op("act", lambda e: e.activation(out=xs[:, :], in_=xin[:, :], func=AF.Copy, scale=ssb[:, 1:2]), [xin, ssb], [xs])
        else:
            fw.op("dve", lambda e: e.tensor_scalar(out=xs[:, :], in0=xin[:, :], scalar1=ssb[:, 1:2], scalar2=None, op0=OP.mult), [xin, ssb], [xs])
        tpv = bank_bf(pb)
        for c in range(8):
            fw.op("pe", lambda e, c=c: e.transpose(out=tpv[:, c * 128:(c + 1) * 128], in_=xs[:, c * 128:(c + 1) * 128], identity=identb[:, :]),
                  [xs, identb], [PR[pb]])
        if add_eng == "act":
            for c in range(8):
                fw.op("act", lambda e, c=c: e.activation(out=hT[:, c, hcols], in_=tpv[:, c * 128:(c + 1) * 128], func=AF.Identity, scale=A1col[:, c:c + 1], bias=B1col[:, c:c + 1]),
                      [PR[pb], A1col, B1col], [hT])
            return
        fw.op("dve", lambda e: e.tensor_tensor(out=tmpf[:, :].rearrange("p (c t) -> p c t", c=8), in0=tpv.rearrange("p (c t) -> p c t", c=8), in1=A1bc, op=OP.mult),
              [PR[pb], A1col], [tmpf])
        fw.op(add_eng, lambda e: e.tensor_tensor(out=hT[:, :, hcols], in0=tmpf[:, :].rearrange("p (c t) -> p c t", c=8), in1=B1bc, op=OP.add),
              [tmpf, B1col], [hT])

    def norm_p1(src_rows, xin, xs):
        ssb = ss_r[ss_i[0] % 4]
        ss_i[0] += 1
        fw.dma("sp", lambda e: e.dma_start(out=xin[:, :], in_=src_rows), [], [xin])
        fw.op("act", lambda e: e.activation(out=xs[:, :], in_=xin[:, :], func=AF.Square, accum_out=ssb[:, 0:1]), [xin], [xs, ssb])
        rstd_of(ssb, 0, D, 1)
        fw.op("dve", lambda e: e.tensor_scalar(out=xs[:, :], in0=xin[:, :], scalar1=ssb[:, 1:2], scalar2=None, op0=OP.mult), [xin, ssb], [xs])

    def norm_p2(xs, tmpf, hT, hcols, pb):
        tpv = bank_bf(pb)
        for c in range(8):
            fw.op("pe", lambda e, c=c: e.transpose(out=tpv[:, c * 128:(c + 1) * 128], in_=xs[:, c * 128:(c + 1) * 128], identity=identb[:, :]),
                  [xs, identb], [PR[pb]])
        fw.op("dve", lambda e: e.tensor_tensor(out=tmpf[:, :].rearrange("p (c t) -> p c t", c=8), in0=tpv.rearrange("p (c t) -> p c t", c=8), in1=A1bc, op=OP.mult),
              [PR[pb], A1col], [tmpf])
        fw.op("pool", lambda e: e.tensor_tensor(out=hT[:, :, hcols], in0=tmpf[:, :].rearrange("p (c t) -> p c t", c=8), in1=B1bc, op=OP.add),
              [tmpf, B1col], [hT])

    winb_v = winb.rearrange("(c p) n -> p c n", p=128)

    for hp in range(2):
        with ExitStack() as es:
            def tl(name, shape, dt=F32):
                return es.enter_context(nc.sbuf_tensor(f"{name}_p{hp}", list(shape), dt))
            KT_t = tl("KT", [128, 2, S], BF16); V_t = tl("VV", [128, NB, 2, 129], BF16)
            wk_t = tl("wk", [128, 8, 256], BF16); wv_t = tl("wv", [128, 8, 256], BF16); wq_t = tl("wq", [128, 8, 256], BF16)
            xin0_t = tl("xin0", [128, D]); xin1_t = tl("xin1", [128, D]); xin2_t = tl("xin2", [128, D])
            xs0_t = tl("xs0", [128, D], BF16); xs1_t = tl("xs1", [128, D], BF16)
            xsB = [Buf(tl(f"xsB{i}", [128, D], BF16)) for i in range(4)]
            tmpf0_t = tl("tmpf0", [128, D]); tmpf1_t = tl("tmpf1", [128, D])
            hT0_t = tl("hT0", [128, 8, 512], BF16); hT1_t = tl("hT1", [128, 8, 512], BF16)
            QT0_t = tl("QT0", [128, 2, 512], BF16); QT1_t = tl("QT1", [128, 2, 512], BF16)
            PT0_t = tl("PT0", [128, 512], BF16); PT1_t = tl("PT1", [128, 512], BF16); PT2_t = tl("PT2", [128, 512], BF16); PT3_t = tl("PT3", [128, 512], BF16)
            at0_t = tl("at0", [128, 128]); at1_t = tl("at1", [128, 128])
            ab0_t = tl("ab0", [128, 128], BF16); ab1_t = tl("ab1", [128, 128], BF16)
            asm_t = tl("asm", [128, 8])
            KT = KT_t
            V = V_t
            rKT = [Reg() for _ in range(16)]
            rV = [Reg() for _ in range(16)]
            wk, wv, wq = Buf(wk_t), Buf(wv_t), Buf(wq_t)
            xins = [Buf(xin0_t), Buf(xin1_t), Buf(xin2_t)]
            xss = [Buf(xs0_t), Buf(xs1_t)]
            tmpfs = [Buf(tmpf0_t), Buf(tmpf1_t)]
            hTs = [Buf(hT0_t), Buf(hT1_t)]
            QTs = [Buf(QT0_t), Buf(QT1_t)]
            PTs = [Buf(PT0_t), Buf(PT1_t), Buf(PT2_t), Buf(PT3_t)]
            ats = [Buf(at0_t), Buf(at1_t)]
            abs_ = [Buf(ab0_t), Buf(ab1_t)]
            asm = Buf(asm_t)
            qc0, kc0, vc0 = hp * 256, 512 + hp * 256, 1024 + hp * 256
            fw.dma("sp", lambda e: e.dma_start(out=wq[:, :, :], in_=winb_v[:, :, qc0:qc0 + 256]), wregs, [wq])
            fw.dma("sp", lambda e: e.dma_start(out=wk[:, :, :], in_=winb_v[:, :, kc0:kc0 + 256]), wregs, [wk])
            fw.dma("sp", lambda e: e.dma_start(out=wv[:, :, :], in_=winb_v[:, :, vc0:vc0 + 256]), wregs, [wv])
            rVall = Reg()
            fw.op("pool", lambda e: e.memset(V[:, :, :, 128:129], 1.0), [], [rVall])
            for r_ in rV:
                r_.w = dict(rVall.w)
            nbc = [0]

            def normS(sbi):
                if True:
                    hT = hTs[sbi % 2]
                    if hp == 0:
                        for dst_, src_, r0_ in tconv[2 * sbi:2 * sbi + 2]:
                            fw.dma("pool", lambda e: e.dma_start(out=dst_[r0_:r0_ + 1024, :], in_=src_[r0_:r0_ + 1024, :]), [], [Reg()])
                            tregs.append(fw.last_written)
                    for bl in range(4):
                        gb = sbi * 4 + bl
                        nb_ = nbc[0]
                        nbc[0] += 1
                        norm1_block(x_b[gb * 128:(gb + 1) * 128, :], xins[nb_ % 3], xss[nb_ % 2], tmpfs[nb_ % 2], hT,
                                    slice(bl * 128, (bl + 1) * 128), nb_ % 2)

            def kvS(sbi):
                if True:
                    hT = hTs[sbi % 2]
                    for hh in range(2):
                        pb = 2 + hh
                        for c in range(8):
                            fw.op("pe", lambda e: e.matmul(bank(pb), lhsT=wk[:, c, hh * 128:(hh + 1) * 128], rhs=hT[:, c, :], start=(c == 0), stop=(c == 7)),
                                  [wk, hT], [PR[pb]])
                        fw.op("act", lambda e: e.activation(out=KT[:, hh, sbi * 512:(sbi + 1) * 512], in_=bank(pb), func=AF.Copy),
                              [PR[pb]], [rKT[sbi]])
                    for bl in range(4):
                        gb = sbi * 4 + bl
                        pb = 4 + (bl % 2)
                        for c in range(8):
                            fw.op("pe", lambda e: e.matmul(bank(pb)[:, 0:256], lhsT=hT[:, c, bl * 128:(bl + 1) * 128], rhs=wv[:, c, :], start=(c == 0), stop=(c == 7)),
                                  [wv, hT], [PR[pb]])
                        fw.op("dve", lambda e: e.tensor_copy(out=V[:, gb, :, 0:128], in_=bank(pb)[:, 0:256].rearrange("p (h v) -> p h v", h=2)),
                              [PR[pb]], [rV[sbi]])

            normS(0)
            for sbi in range(16):
                if sbi + 1 < 16:
                    interleave(fw, [lambda: normS(sbi + 1), lambda: kvS(sbi)], [1, 1])
                else:
                    kvS(sbi)
            nblk = nbc[0]
            if debug is not None and debug[0] == "kv" and hp == 0:
                with nc.sbuf_tensor("dbgt", [128, 2048], F32) as dt_:
                    dtb = Buf(dt_)
                    fw.op("dve", lambda e: e.tensor_copy(out=dtb[:, 0:1024], in_=KT[:, 1, 7168:8192]), rKT, [dtb])
                    fw.op("dve", lambda e: e.tensor_copy(out=dtb[:, 1024:2048].rearrange("p (b v) -> p b v", b=8), in_=V[:, 56:64, 1, 0:128]), rV, [dtb])
                    dbg_out(dtb[:, :], [dtb])
                    fw.barrier()
                    return nc
            def b_p1(g_):
                for qi_ in range(4):
                    j_ = 4 * g_ + qi_
                    norm_p1(x_own[j_ * 128:(j_ + 1) * 128, :], xins[(4 * g_ + qi_) % 3], xsB[qi_])

            b_p1(0)
            for g in range(8):
                hT = hTs[g % 2]
                QT = QTs[g % 2]
                for qi in range(4):
                    norm_p2(xsB[qi], tmpfs[qi % 2], hT, slice(qi * 128, (qi + 1) * 128), qi % 2)
                for hh in range(2):
                    pb = 2 + hh
                    for c in range(8):
                        fw.op("pe", lambda e, c=c, hh=hh, pb=pb: e.matmul(bank(pb), lhsT=wq[:, c, hh * 128:(hh + 1) * 128], rhs=hT[:, c, :], start=(c == 0), stop=(c == 7)),
                              [wq, hT], [PR[pb]])
                    fw.op("act", lambda e, hh=hh, pb=pb: e.activation(out=QT[:, hh, :], in_=bank(pb), func=AF.Copy, scale=0.125),
                          [PR[pb]], [QT])
                def attn_g():
                    for hh in range(2):
                        kmax = 8 * g + 7

                        def emit_S(kb):
                            qmin = max(0, (kb - 8 * g) // 2)
                            c0 = qmin * 128
                            for half in range(2):
                                spb = 4 + 2 * (kb % 2) + half
                                rs = slice(half * 64, (half + 1) * 64)
                                fw.op("pe", lambda e: e.matmul(bank(spb)[:, c0:512], lhsT=KT[rs, hh, kb * 128:(kb + 1) * 128], rhs=QT[rs, hh, c0:512], start=True, stop=True),
                                      [rKT[kb // 4], QT], [PR[spb]])
                            for half in range(2):
                                spb = 4 + 2 * (kb % 2) + half
                                PT = PTs[2 * (kb % 2) + half]
                                fw.op("act", lambda e: e.activation(out=PT[:, c0:512], in_=bank(spb)[:, c0:512], func=AF.Exp),
                                      [PR[spb]], [PT])
                                for qi in range(qmin, 4):
                                    j = 4 * g + qi
                                    m = kb - 2 * j
                                    if m in (0, 1):
                                        fw.op("pool", lambda e: e.tensor_tensor(out=PT[:, qi * 128:(qi + 1) * 128], in0=PT[:, qi * 128:(qi + 1) * 128], in1=maskb[:, m * 128:(m + 1) * 128], op=OP.mult),
                                              [PT, maskb], [PT])

                        def emit_PV(kb):
                            qmin = max(0, (kb - 8 * g) // 2)
                            for half in range(2):
                                PT = PTs[2 * (kb % 2) + half]
                                for qi in range(qmin, 4):
                                    j = 4 * g + qi
                                    ob = qi
                                    ov = bank(ob)[:, half * 129:(half + 1) * 129]
                                    fw.op("pe", lambda e: e.matmul(ov, lhsT=PT[:, qi * 128:(qi + 1) * 128], rhs=V[:, kb, hh, :], start=(kb == 0 and half == 0), stop=(kb == 2 * j + 1 and half == 1)),
                                          [PT, rV[kb // 4]], [PR[ob]])

                        emit_S(0)
                        for kb in range(kmax + 1):
                            if kb + 1 <= kmax:
                                emit_S(kb + 1)
                            emit_PV(kb)
                        for qi in range(4):
                            j = 4 * g + qi
                            ob = qi
                            at = ats[qi % 2]
                            ab = abs_[qi % 2]
                            o3 = bank(ob)[:, 0:258].rearrange("p (a b) -> p a b", a=2)
                            fw.op("dve", lambda e, o3=o3: e.reciprocal(out=asm[:, 0:2], in_=o3[:, :, 128]), [PR[ob]], [asm])
                            fw.op("dve", lambda e: e.tensor_tensor(out=asm[:, 2:3], in0=asm[:, 1:2], in1=neglam[:, :], op=OP.mult), [asm, neglam], [asm])
                            fw.op("dve", lambda e, o3=o3, at=at: e.tensor_scalar(out=at[:, :], in0=o3[:, 0, 0:128], scalar1=asm[:, 0:1], scalar2=None, op0=OP.mult), [PR[ob], asm], [at])
                            fw.op("dve", lambda e, o3=o3, at=at: e.scalar_tensor_tensor(out=at[:, :], in0=o3[:, 1, 0:128], scalar=asm[:, 2:3], in1=at[:, :], op0=OP.mult, op1=OP.add), [PR[ob], asm, at], [at])
                            fw.op("act", lambda e, at=at, ab=ab: e.activation(out=ab[:, :], in_=at[:, :], func=AF.Square, accum_out=asm[:, 3:4]), [at], [ab, asm])
                            rstd_of(asm, 3, 128, 4)
                            fw.op("dve", lambda e, at=at, ab=ab: e.scalar_tensor_tensor(out=ab[:, :], in0=at[:, :], scalar=asm[:, 4:5], in1=hg_bc[:, :], op0=OP.mult, op1=OP.mult), [at, asm, hg_bc], [ab])
                            tb = 6 + (qi % 2)
                            fw.op("pe", lambda e, ab=ab, tb=tb: e.transpose(out=bank_bf(tb)[:, 0:128], in_=ab[:, :], identity=identb[:, :]), [ab, identb], [PR[tb]])
                            yst = ysts[ysi[0] % 2]
                            ysi[0] += 1
                            fw.op("act", lambda e, tb=tb: e.activation(out=yst[:, :], in_=bank_bf(tb)[:, 0:128], func=AF.Copy), [PR[tb]], [yst])
                            rr_ = Reg()
                            fw.dma("sp", lambda e, j=j, hh=hh: e.dma_start(out=yaTd[hp * 2 + hh, :, j * 128:(j + 1) * 128], in_=yst[:, :]), [yst], [rr_])
                            yaregs.append(rr_)
                if g + 1 < 8:
                    interleave(fw, [attn_g, lambda: b_p1(g + 1)], [8, 1])
                else:
                    attn_g()
            fw.barrier()
    def tl(name, shape, dt=F32):
        return Buf(nc.alloc_sbuf_tensor(name, list(shape), dt))

    wbab_v = wbab.rearrange("(c p) n -> p c n", p=128)
    wbbb_v = wbbb.rearrange("(c p) n -> p c n", p=128)
    woutb_v = woutb.rearrange("(c p) n -> p c n", p=128)
    wpqb_v = wpqb.rearrange("(c p) n -> p c n", p=128)
    Wsl = [tl(f"Wsl{i}", [128, 4096], BF16) for i in range(5)]
    wi = [0]

    def load_w(view3, c):
        w = Wsl[wi[0] % 5]
        wi[0] += 1
        fw.dma("sp", lambda e: e.dma_start(out=w[:, :].rearrange("p (c n) -> p c n", c=c), in_=view3), wregs, [w])
        return w, w[:, :].rearrange("p (c n) -> p c n", c=c)

    xgs = [[tl(f"xg{p}{b}", [128, D]) for b in range(2)] for p in range(2)]
    xsC = tl("xsC", [128, D], BF16)
    tmpfC = tl("tmpfC", [128, D])
    hTC = tl("hTC", [128, 8, 256], BF16)
    gTC = tl("gTC", [128, 16, 256], BF16)
    gu = tl("gu", [128, 512])
    gs = tl("gs", [128, 512])
    svn = tl("svn", [128, 512], BF16)
    ybt = tl("ybt", [128, 512], BF16)
    ybT = tl("ybT", [128, 4, 256], BF16)
    t5a = tl("t5a", [128, 256])
    t5b = tl("t5b", [128, 256])
    mT = tl("mT", [128, 8, 256], BF16)
    h2gs = [[tl(f"h2g{p}{b}", [128, D], BF16) for b in range(2)] for p in range(2)]
    st6 = tl("st6", [128, 12])
    scf = tl("scf", [128, 2048])
    scw = tl("scw", [128, 2048])
    tv = tl("tv", [128, 256])
    ti = tl("ti", [128, 256], U32)
    tif = tl("tif", [128, 256])
    cv = tl("cv", [128, 128])
    ci = tl("ci", [128, 128], U32)
    cif = tl("cif", [128, 128])
    gsm = tl("gsm", [128, 128])
    sm8 = tl("sm8", [128, 8])
    ak = tl("ak", [128, 128])
    bk = tl("bk", [128, 128])
    i1s = tl("i1s", [128, 128])
    i2s = tl("i2s", [128, 128])
    eidxf = tl("eidxf", [128, 128])
    eTs = [[tl(f"eT{p}{b}", [128, 128], U32) for b in range(2)] for p in range(2)]
    gTs_ = [[tl(f"gT_{p}{b}", [128, 128]) for b in range(2)] for p in range(2)]
    yaGs = [tl("yaG0", [128, 4, 256], BF16), tl("yaG1", [128, 4, 256], BF16)]
    tmpfL = ViewBuf(mod_bc.t[:, 0:1024])
    junkL = ViewBuf(mod_bc.t[:, 1024:2048].bitcast(BF16)[:, 0:1024])
    aT = tl("aT", [128, 128])
    coefT = tl("coefT", [128, 128])
    NGB = 9
    Gbufs = [tl(f"Gbuf{i}", [128, 2 * D], BF16) for i in range(NGB)]
    gbi = [0]

    gslots = [[f"d_g{i}", nc.alloc_semaphore(f"d_g{i}"), 0] for i in range(NGB)]

    def gather(table, idx_ap, rd):
        k = gbi[0] % NGB
        b_ = Gbufs[k]
        pre = [Gbufs[(k + 1) % NGB]] if k % 2 == 0 else []
        gbi[0] += 1
        fw.dma("pool", lambda e: e.indirect_dma_start(out=b_[:, :], out_offset=None, in_=table[:, :], in_offset=bass.IndirectOffsetOnAxis(ap=idx_ap, axis=0)),
               rd, [b_], slot=gslots[k], pre=pre)
        return b_
    Wc = tl("Wc", [128, 256], BF16)
    Zts = [tl(f"Zt{i}", [128, 128], BF16) for i in range(4)]
    acols = [tl(f"acol{i}", [128, 1]) for i in range(8)]
    ccols = [tl(f"ccol{i}", [128, 1]) for i in range(8)]
    thr16 = tl("thr16", [128, 16])
    io16 = tl("io16", [128, 16])
    ioi = tl("ioi", [128, 16], I32)

    fw.op("pool", lambda e: e.memset(Wc[:, :], 0.0), [], [Wc])
    fw.op("pool", lambda e: e.memset(Wc[:, 127:128], 1.0), [Wc], [Wc])
    fw.op("pool", lambda e: e.iota(ioi[:, :], pattern=[[1, 16]], base=0, channel_multiplier=0), [], [ioi])
    fw.op("dve", lambda e: e.tensor_copy(out=io16[:, :], in_=ioi[:, :]), [ioi], [io16])
    fw.op("dve", lambda e: e.tensor_scalar(out=thr16[:, :], in0=io16[:, :], scalar1=16.0, scalar2=None, op0=OP.mult), [io16], [thr16])
    def top16(src3, wrk3, vals3, idx3, n, regs):
        r_src, r_wrk, r_vals, r_idx = regs
        for l in range(n):
            fw.op("dve", lambda e: e.max(out=vals3[:, l, 0:8], in_=src3[:, l, :]), [r_src], [r_vals])
            fw.op("dve", lambda e: e.max_index(out=idx3[:, l, 0:8], in_max=vals3[:, l, 0:8], in_values=src3[:, l, :]), [r_src, r_vals], [r_idx])
            fw.op("dve", lambda e: e.match_replace(out=wrk3[:, l, :], in_to_replace=vals3[:, l, 0:8], in_values=src3[:, l, :], imm_value=NEG), [r_src, r_vals], [r_wrk])
            fw.op("dve", lambda e: e.max(out=vals3[:, l, 8:16], in_=wrk3[:, l, :]), [r_wrk], [r_vals])
            fw.op("dve", lambda e: e.max_index(out=idx3[:, l, 8:16], in_max=vals3[:, l, 8:16], in_values=wrk3[:, l, :]), [r_wrk, r_vals], [r_idx])

    NGC = 16 if debug is None else debug[2]

    def cmain(g):
        p = g % 2
        xg = xgs[p]
        h2g = h2gs[p]
        for bl in range(2):
            j = 2 * g + bl
            fw.dma("sp", lambda e: e.dma_start(out=xg[bl][:, :], in_=x_own[j * 128:(j + 1) * 128, :]), [], [xg[bl]])
            norm1_block(None, xg[bl], xsC, tmpfC, hTC, slice(bl * 128, (bl + 1) * 128), bl, add_eng="act")
        for u in range(4):
            w, w3 = load_w(winb_v[:, :, 2560 + u * 512:2560 + (u + 1) * 512], 8)
            for cc in range(4):
                ch = u * 4 + cc
                pb = ch % 2
                for c in range(8):
                    fw.op("pe", lambda e: e.matmul(bank(pb)[:, 0:256], lhsT=w3[:, c, cc * 128:(cc + 1) * 128], rhs=hTC[:, c, :], start=(c == 0), stop=(c == 7)),
                          [w, hTC], [PR[pb]])
                fw.op("act", lambda e: e.activation(out=gTC[:, ch, :], in_=bank(pb)[:, 0:256], func=AF.Sigmoid), [PR[pb]], [gTC])
        wu, wu3 = load_w(winb_v[:, :, 1536:2048], 8)
        wsv, wsv3 = load_w(winb_v[:, :, 2048:2560], 8)
        for bl in range(2):
            bs_ = slice(bl * 128, (bl + 1) * 128)
            for c in range(8):
                fw.op("pe", lambda e: e.matmul(bank(0), lhsT=hTC[:, c, bs_], rhs=wu3[:, c, :], start=(c == 0), stop=(c == 7)), [wu, hTC], [PR[0]])
            for c in range(8):
                fw.op("pe", lambda e: e.matmul(bank(1), lhsT=hTC[:, c, bs_], rhs=wsv3[:, c, :], start=(c == 0), stop=(c == 7)), [wsv, hTC], [PR[1]])
            fw.op("act", lambda e: e.activation(out=gu[:, :], in_=bank(0), func=AF.Gelu_apprx_tanh), [PR[0]], [gu])
            fw.op("act", lambda e: e.activation(out=gs[:, :], in_=bank(1), func=AF.Gelu_apprx_tanh), [PR[1]], [gs])
            fw.op("dve", lambda e: e.bn_stats(out=st6[:, 0:6], in_=gs[:, :]), [gs], [st6])
            fw.op("dve", lambda e: e.bn_aggr(out=st6[:, 6:8], in_=st6[:, 0:6]), [st6], [st6])
            rstd_of(st6, 7, 1.0, 8)
            fw.op("dve", lambda e: e.tensor_scalar(out=gs[:, :], in0=gs[:, :], scalar1=st6[:, 6:7], scalar2=st6[:, 8:9], op0=OP.subtract, op1=OP.mult), [gs, st6], [gs])
            fw.op("dve", lambda e: e.tensor_tensor(out=gs[:, :], in0=gs[:, :], in1=lng_bc[:, :], op=OP.mult), [gs, lng_bc], [gs])
            fw.op("dve", lambda e: e.tensor_tensor(out=svn[:, :], in0=gs[:, :], in1=lnb_bc[:, :], op=OP.add), [gs, lnb_bc], [svn])
            for gr in range(4):
                fw.op("pe", lambda e: e.matmul(bank(0)[:, gr * 128:(gr + 1) * 128], lhsT=wsT[:, gr, :], rhs=svn[:, gr * 128:(gr + 1) * 128], start=True, stop=True),
                      [wsT, svn], [PR[0]])
            for gr in range(4):
                fw.op("dve", lambda e: e.scalar_tensor_tensor(out=ybt[:, gr * 128:(gr + 1) * 128], in0=bank(0)[:, gr * 128:(gr + 1) * 128], scalar=bs_col[:, gr:gr + 1], in1=gu[:, gr * 128:(gr + 1) * 128], op0=OP.add, op1=OP.mult),
                      [PR[0], bs_col, gu], [ybt])
            for ch in range(4):
                fw.op("pe", lambda e: e.transpose(out=bank_bf(1)[:, ch * 128:(ch + 1) * 128], in_=ybt[:, ch * 128:(ch + 1) * 128], identity=identb[:, :]), [ybt, identb], [PR[1]])
            fw.op("act", lambda e: e.activation(out=ybT[:, :, bs_], in_=bank_bf(1)[:, 0:512].rearrange("p (c t) -> p c t", c=4), func=AF.Copy), [PR[1]], [ybT])
        wa, wa3 = load_w(wbab_v, 4)
        wb, wb3 = load_w(wbbb_v, 4)
        yaG = yaGs[p]
        fw.dma("sp", lambda e: e.dma_start(out=yaG[:, :, :], in_=yaTd[:, :, g * 256:(g + 1) * 256].rearrange("c p t -> p c t")), yaregs, [yaG])
        for dc in range(8):
            ba, bb = 0, 1
            for cc in range(4):
                fw.op("pe", lambda e: e.matmul(bank(ba)[:, 0:256], lhsT=wa3[:, cc, dc * 128:(dc + 1) * 128], rhs=yaG[:, cc, :], start=(cc == 0), stop=(cc == 3)),
                      [wa, yaG], [PR[ba]])
            for cc in range(4):
                fw.op("pe", lambda e: e.matmul(bank(bb)[:, 0:256], lhsT=wb3[:, cc, dc * 128:(dc + 1) * 128], rhs=ybT[:, cc, :], start=(cc == 0), stop=(cc == 3)),
                      [wb, ybT], [PR[bb]])
            fw.op("dve", lambda e: e.tensor_tensor(out=t5a[:, :], in0=bank(ba)[:, 0:256], in1=gTC[:, dc, :], op=OP.mult), [PR[ba], gTC], [t5a])
            fw.op("dve", lambda e: e.tensor_tensor(out=t5b[:, :], in0=bank(bb)[:, 0:256], in1=gTC[:, 8 + dc, :], op=OP.mult), [PR[bb], gTC], [t5b])
            fw.op("dve", lambda e: e.tensor_tensor(out=mT[:, dc, :], in0=t5a[:, :], in1=t5b[:, :], op=OP.add), [t5a, t5b], [mT])
        wo0, wo03 = load_w(woutb_v[:, :, 0:512], 8)
        wo1, wo13 = load_w(woutb_v[:, :, 512:1024], 8)
        for bl in range(2):
            bs_ = slice(bl * 128, (bl + 1) * 128)
            for half, (wo, wo3) in enumerate(((wo0, wo03), (wo1, wo13))):
                for c in range(8):
                    fw.op("pe", lambda e: e.matmul(bank(half), lhsT=mT[:, c, bs_], rhs=wo3[:, c, :], start=(c == 0), stop=(c == 7)), [wo, mT], [PR[half]])
            fw.op("dve", lambda e: e.tensor_tensor(out=tmpfC[:, :], in0=pair(0), in1=mod_bc[:, 2 * D:3 * D], op=OP.mult), [PR[0], PR[1], mod_bc], [tmpfC])
            fw.op("dve", lambda e: e.tensor_tensor(out=xg[bl][:, :], in0=xg[bl][:, :], in1=tmpfC[:, :], op=OP.add), [xg[bl], tmpfC], [xg[bl]])
        if debug is not None and debug[0] == "x2":
            dbg_out(xg[0][:, :], [xg[0]], dbg_d[:, 0:D])
            dbg_out(xg[1][:, :], [xg[1]], dbg_d[:, D:2 * D])
            return
        for bl in range(2):
            ssb = ss_r[ss_i[0] % 4]
            ss_i[0] += 1
            fw.op("act", lambda e: e.activation(out=xsC[:, :], in_=xg[bl][:, :], func=AF.Square, accum_out=ssb[:, 0:1]), [xg[bl]], [xsC, ssb])
            rstd_of(ssb, 0, D, 1)
            fw.op("dve", lambda e: e.scalar_tensor_tensor(out=tmpfC[:, :], in0=xg[bl][:, :], scalar=ssb[:, 1:2], in1=mod_bc[:, 4 * D:5 * D], op0=OP.mult, op1=OP.mult), [xg[bl], ssb, mod_bc], [tmpfC])
            fw.op("dve", lambda e: e.tensor_tensor(out=h2g[bl][:, :], in0=tmpfC[:, :], in1=mod_bc[:, 3 * D:4 * D], op=OP.add), [tmpfC, mod_bc], [h2g[bl]])
            for c in range(8):
                fw.op("pe", lambda e: e.transpose(out=bank_bf(bl)[:, c * 128:(c + 1) * 128], in_=h2g[bl][:, c * 128:(c + 1) * 128], identity=identb[:, :]), [h2g[bl], identb], [PR[bl]])
            fw.op("act", lambda e: e.activation(out=hTC[:, :, bl * 128:(bl + 1) * 128], in_=bank_bf(bl)[:, 0:1024].rearrange("p (c t) -> p c t", c=8), func=AF.Copy), [PR[bl]], [hTC])
        for u in range(4):
            w, w3 = load_w(wpqb_v[:, :, u * 512:(u + 1) * 512], 8)
            for cc in range(4):
                l = u * 4 + cc
                pb = l % 2
                for c in range(8):
                    fw.op("pe", lambda e: e.matmul(bank(pb)[:, 0:256], lhsT=w3[:, c, cc * 128:(cc + 1) * 128], rhs=hTC[:, c, :], start=(c == 0), stop=(c == 7)), [w, hTC], [PR[pb]])
                fw.op("act", lambda e: e.activation(out=gTC[:, l, :], in_=bank(pb)[:, 0:256], func=AF.Copy), [PR[pb]], [gTC])
        for bl in range(2):
            eT = eTs[p][bl]
            gT_ = gTs_[p][bl]
            bs_ = slice(bl * 128, (bl + 1) * 128)
            for rnd in range(2):
                for l8 in range(8):
                    l = rnd * 8 + l8
                    fw.op("pe", lambda e: e.matmul(bank(l8 // 4)[:, (l8 % 4) * 128:(l8 % 4 + 1) * 128], lhsT=gTC[:, l, bs_], rhs=skT[:, l, :], start=True, stop=True), [gTC, skT], [PR[l8 // 4]])
                if rnd == 0:
                    fw.op("act", lambda e: e.activation(out=scf[:, 0:1024], in_=pair(0), func=AF.Copy), [PR[0], PR[1]], [scf])
                else:
                    fw.op("act", lambda e: e.activation(out=scf[:, 1024:2048], in_=pair(0), func=AF.Copy), [PR[0], PR[1]], [scf])
            r_src, r_wrk, r_vals, r_idx = scf, scw, tv, ti
            top16(scf[:, :].rearrange("p (l n) -> p l n", l=16), scw[:, :].rearrange("p (l n) -> p l n", l=16),
                  tv[:, :].rearrange("p (l k) -> p l k", l=16), ti[:, :].rearrange("p (l k) -> p l k", l=16), 16, (scf, scw, tv, ti))
            fw.op("dve", lambda e: e.tensor_copy(out=tif[:, :], in_=ti[:, :]), [ti], [tif])
            tv4 = tv[:, :].rearrange("p (h i k) -> p h i k", h=8, i=2)
            tif4 = tif[:, :].rearrange("p (h i k) -> p h i k", h=8, i=2)
            cand4 = scf[:, :].rearrange("p (h a b) -> p h a b", h=8, a=16)
            fw.op("dve", lambda e: e.tensor_tensor(out=cand4, in0=tv4[:, :, 0, :].unsqueeze(3).to_broadcast([128, 8, 16, 16]), in1=tv4[:, :, 1, :].unsqueeze(2).to_broadcast([128, 8, 16, 16]), op=OP.add), [tv], [scf])
            top16(scf[:, :].rearrange("p (h n) -> p h n", h=8), scw[:, :].rearrange("p (h n) -> p h n", h=8),
                  cv[:, :].rearrange("p (h k) -> p h k", h=8), ci[:, :].rearrange("p (h k) -> p h k", h=8), 8, (scf, scw, cv, ci))
            cv3 = cv[:, :].rearrange("p (h k) -> p h k", h=8)
            gsm3 = gsm[:, :].rearrange("p (h k) -> p h k", h=8)
            fw.op("dve", lambda e: e.tensor_tensor(out=gsm3, in0=cv3, in1=cv3[:, :, 0:1].to_broadcast([128, 8, 16]), op=OP.subtract), [cv], [gsm])
            fw.op("act", lambda e: e.activation(out=gsm[:, :], in_=gsm[:, :], func=AF.Exp), [gsm], [gsm])
            fw.op("dve", lambda e: e.tensor_reduce(out=sm8[:, :], in_=gsm3, axis=AX.X, op=OP.add), [gsm], [sm8])
            fw.op("dve", lambda e: e.reciprocal(out=sm8[:, :], in_=sm8[:, :]), [sm8], [sm8])
            fw.op("dve", lambda e: e.tensor_tensor(out=gsm3, in0=gsm3, in1=sm8[:, :].unsqueeze(2).to_broadcast([128, 8, 16]), op=OP.mult), [gsm, sm8], [gsm])
            fw.op("dve", lambda e: e.tensor_copy(out=cif[:, :], in_=ci[:, :]), [ci], [cif])
            oh4 = scw[:, :].rearrange("p (h k a) -> p h k a", h=8, k=16)
            cif3 = cif[:, :].rearrange("p (h k) -> p h k", h=8)
            ak3 = ak[:, :].rearrange("p (h k) -> p h k", h=8)
            bk3 = bk[:, :].rearrange("p (h k) -> p h k", h=8)
            thr_b = thr16[:, :].unsqueeze(1).unsqueeze(1).to_broadcast([128, 8, 16, 16])
            io_b = io16[:, :].unsqueeze(1).unsqueeze(1).to_broadcast([128, 8, 16, 16])
            fw.op("dve", lambda e: e.tensor_tensor(out=oh4, in0=cif3.unsqueeze(3).to_broadcast([128, 8, 16, 16]), in1=thr_b, op=OP.is_ge), [cif, thr16], [scw])
            fw.op("dve", lambda e: e.tensor_reduce(out=ak3, in_=oh4, axis=AX.X, op=OP.add), [scw], [ak])
            fw.op("dve", lambda e: e.tensor_scalar(out=ak[:, :], in0=ak[:, :], scalar1=-1.0, scalar2=None, op0=OP.add), [ak], [ak])
            fw.op("dve", lambda e: e.scalar_tensor_tensor(out=bk[:, :], in0=ak[:, :], scalar=-16.0, in1=cif[:, :], op0=OP.mult, op1=OP.add), [ak, cif], [bk])
            for (kk3, pidx, dst) in ((ak3, 0, i1s), (bk3, 1, i2s)):
                fw.op("dve", lambda e: e.tensor_tensor(out=oh4, in0=kk3.unsqueeze(3).to_broadcast([128, 8, 16, 16]), in1=io_b, op=OP.is_equal), [ak, bk, io16], [scw])
                fw.op("dve", lambda e: e.tensor_tensor(out=oh4, in0=oh4, in1=tif4[:, :, pidx, :].unsqueeze(2).to_broadcast([128, 8, 16, 16]), op=OP.mult), [scw, tif], [scw])
                fw.op("dve", lambda e: e.tensor_reduce(out=dst[:, :].rearrange("p (h k) -> p h k", h=8), in_=oh4, axis=AX.X, op=OP.add), [scw], [dst])
            fw.op("dve", lambda e: e.scalar_tensor_tensor(out=eidxf[:, :], in0=i1s[:, :], scalar=128.0, in1=i2s[:, :], op0=OP.mult, op1=OP.add), [i1s, i2s], [eidxf])
            fw.op("pe", lambda e: e.transpose(out=bank(0)[:, 0:128], in_=eidxf[:, :], identity=identf[:, :]), [eidxf, identf], [PR[0]])
            fw.op("dve", lambda e: e.tensor_copy(out=eT[:, :], in_=bank(0)[:, 0:128]), [PR[0]], [eT])
            fw.op("pe", lambda e: e.transpose(out=bank(1)[:, 0:128], in_=gsm[:, :], identity=identf[:, :]), [gsm, identf], [PR[1]])
            fw.op("act", lambda e: e.activation(out=gT_[:, :], in_=bank(1)[:, 0:128], func=AF.Copy), [PR[1]], [gT_])

    def loops(g):
        p = g % 2
        xg = xgs[p]
        h2g = h2gs[p]
        for bl in range(2):
            j = 2 * g + bl
            eT = eTs[p][bl]
            gT_ = gTs_[p][bl]
            gbs = {}

            def s_gather(t):
                gbs[t] = gather(pdu, eT[:, t:t + 1], [eT] + tregs)

            def s_bcast(t):
                hbp = 2 + (t % 2)
                for half in range(2):
                    fw.op("pe", lambda e: e.matmul(bank(2 * hbp + half), lhsT=identb[:, t:t + 1].to_broadcast([128, 128]), rhs=h2g[bl][:, half * 512:(half + 1) * 512], start=True, stop=True),
                          [identb, h2g[bl]], [PR[2 * hbp + half]])

            def s_dot(t):
                hbp = 2 + (t % 2)
                ac = acols[t % 8]
                cc_ = ccols[t % 8]
                gb_ = gbs[t]
                fw.op("dve", lambda e: e.scalar_tensor_tensor(out=junkL[:, :], in0=gb_[:, 0:D], scalar=1.0, in1=pair(hbp), op0=OP.mult, op1=OP.mult, accum_out=ac[:, 0:1]),
                      [gb_, PR[2 * hbp], PR[2 * hbp + 1]], [junkL, ac])
                fw.op("act", lambda e: e.activation(out=cc_[:, 0:1], in_=ac[:, 0:1], func=AF.Gelu_apprx_tanh), [ac], [cc_])

            def s_up(t):
                cc_ = ccols[t % 8]
                zt = Zts[t % 4]
                gb_ = gbs.pop(t)
                fw.op("dve", lambda e: e.tensor_scalar(out=zt[:, :], in0=Wc[:, 127 - t:255 - t], scalar1=cc_[:, 0:1], scalar2=gT_[:, t:t + 1], op0=OP.mult, op1=OP.mult),
                      [Wc, cc_, gT_], [zt])
                for half in range(2):
                    fw.op("pe", lambda e: e.matmul(bank(2 + half), lhsT=zt[:, :], rhs=gb_[:, D + half * 512:D + (half + 1) * 512], start=(t == 0), stop=(t == 127)),
                          [zt, gb_], [PR[2 + half]])

            AH = NGB - 2
            for t in range(min(AH, 128)):
                s_gather(t)
            s_bcast(0)
            for t in range(128):
                if t + AH < 128:
                    s_gather(t + AH)
                if t + 1 < 128:
                    s_bcast(t + 1)
                s_dot(t)
                if t >= 1:
                    s_up(t - 1)
            s_up(127)
            fw.op("dve", lambda e: e.tensor_tensor(out=tmpfL[:, :], in0=pair(1), in1=mod_bc[:, 5 * D:6 * D], op=OP.mult), [PR[2], PR[3], mod_bc], [tmpfL])
            fw.op("dve", lambda e: e.tensor_tensor(out=xg[bl][:, :], in0=xg[bl][:, :], in1=tmpfL[:, :], op=OP.add), [xg[bl], tmpfL], [xg[bl]])
            ssb = ss_r[ss_i[0] % 4]
            ss_i[0] += 1
            fw.op("act", lambda e: e.activation(out=junkL[:, :], in_=xg[bl][:, :], func=AF.Square, accum_out=ssb[:, 0:1]), [xg[bl]], [junkL, ssb])
            rstd_of(ssb, 0, D, 1)
            fw.op("dve", lambda e: e.scalar_tensor_tensor(out=tmpfL[:, :], in0=xg[bl][:, :], scalar=ssb[:, 1:2], in1=fg_bc[:, :], op0=OP.mult, op1=OP.mult), [xg[bl], ssb, fg_bc], [tmpfL])
            fw.dma("sp", lambda e: e.dma_start(out=out_d[j * 128:(j + 1) * 128, :], in_=tmpfL[:, :]), [tmpfL], [r_none])

    cmain(0)
    if debug is not None and debug[0] == "x2":
        fw.barrier()
        return nc
    for g in range(NGC):
        if g + 1 < NGC:
            interleave(fw, [lambda: loops(g), lambda: cmain(g + 1)], [2, 1])
        else:
            loops(g)
    fw.barrier()
    return nc


def _prep_inputs(inputs):
    g = {k: np.asarray(v) for k, v in inputs.items()}
    x = np.ascontiguousarray(g["x"], dtype=np.float32)
    shared = {
        "w_ada": g["w_ada"][0], "b_ada": g["b_ada"], "norm1_g": g["norm1_g"], "w_in": g["w_in"][0],
        "da_lambda_q1": g["da_lambda_q1"], "da_lambda_k1": g["da_lambda_k1"],
        "da_lambda_q2": g["da_lambda_q2"], "da_lambda_k2": g["da_lambda_k2"],
        "da_head_g": g["da_head_g"], "sg_ln_g": g["sg_ln_g"], "sg_ln_b": g["sg_ln_b"],
        "sg_w": g["sg_w"][0], "sg_b": g["sg_b"][0], "w_branch_a": g["w_branch_a"][0],
        "w_branch_b": g["w_branch_b"][0], "w_out": g["w_out"][0], "norm2_g": g["norm2_g"],
        "peer_w_query": g["peer_w_query"][0], "peer_sub_keys": g["peer_sub_keys"][0].reshape(16, 128, 128),
        "peer_down": g["peer_down"][0], "peer_up": g["peer_up"][0], "final_g": g["final_g"].reshape(1, D),
    }
    shared = {k: np.ascontiguousarray(v, dtype=np.float32) for k, v in shared.items()}
    kk = np.arange(128)[:, None] // 64
    qq = np.arange(128)[None, :] // 64
    diag = (kk <= qq).astype(np.float32)
    in_maps = []
    for core in range(8):
        b, par = core // 2, core % 2
        xb = x[b]
        xo = np.ascontiguousarray(xb.reshape(NB, 128, D)[par::2].reshape(NOWN * 128, D))
        if par == 0:
            am = np.concatenate([diag, np.zeros((128, 128), np.float32)], axis=1)
        else:
            am = np.concatenate([np.ones((128, 128), np.float32), diag], axis=1)
        m = dict(shared)
        m["x_b"] = xb
        m["x_own"] = xo
        m["c_b"] = np.ascontiguousarray(g["c"][b:b + 1], dtype=np.float32)
        m["amask"] = np.ascontiguousarray(am)
        in_maps.append(m)
    return in_maps


def kernel(**inputs):
    in_maps = _prep_inputs(inputs)
    nc = build()
    res = run_bass_kernel_spmd(nc, in_maps, core_ids=list(range(8)))
    out = np.zeros((4, S, D), np.float32)
    for core in range(8):
        b, par = core // 2, core % 2
        o = np.asarray(res.results[core]["out"]).reshape(NOWN, 128, D)
        out[b].reshape(NB, 128, D)[par::2] = o
    return out
```

```python
import numpy as np
from contextlib import ExitStack
import concourse.bass as bass
import concourse.mybir as mybir
from concourse.bass_utils import run_bass_kernel_spmd

F32 = mybir.dt.float32
BF16 = mybir.dt.bfloat16
I32 = mybir.dt.int32
U32 = mybir.dt.uint32
AF = mybir.ActivationFunctionType
OP = mybir.AluOpType
AX = mybir.AxisListType

D = 1024
S = 8192
NB = 64
NOWN = 32
EPS = 1e-6
LAM_INIT = 0.8 - 0.6 * 1.0
NEG = -1.0e30
SEM_LIMIT = 30000


class Reg:
    def __init__(self):
        self.w = {}
        self.r = {}


class Buf(Reg):
    def __init__(self, t):
        Reg.__init__(self)
        self.t = t

    def __getitem__(self, k):
        return self.t[k]


class FW:
    def __init__(self, nc):
        self.nc = nc
        self.engs = dict(pe=nc.tensor, act=nc.scalar, dve=nc.vector, pool=nc.gpsimd, sp=nc.sync)
        self.cur = {}
        self.nsem = 0
        self.known = {k: {} for k in self.engs}
        for k in self.engs:
            self._newsem(k)
        self.dsem = {}
        for q, n in (("sp", 12), ("act", 4), ("pool", 12)):
            self.dsem[q] = [[f"d_{q}{i}", nc.alloc_semaphore(f"d_{q}{i}"), 0] for i in range(n)]
        self.drr = {q: 0 for q in self.dsem}
        self.ninst = 0
        self.hook = None

    def _newsem(self, k):
        name = f"e_{k}{self.nsem}"
        self.cur[k] = [name, self.nc.alloc_semaphore(name), 0]
        self.nsem += 1

    def _deps(self, reads, writes):
        deps = {}

        def add(d):
            for name, (s, v) in d.items():
                if name not in deps or deps[name][1] < v:
                    deps[name] = (s, v)
        for b in reads:
            add(b.w)
        for b in writes:
            add(b.w)
            add(b.r)
        return deps

    def _wait(self, k, deps):
        eng = self.engs[k]
        for name, (s, v) in deps.items():
            if k == "pe" and name.startswith("e_pe"):
                continue
            if self.known[k].get(name, 0) >= v:
                continue
            eng.wait_ge(s, v)
            self.known[k][name] = v
            self.ninst += 1

    def _record(self, ev, reads, writes):
        name, s, v = ev
        for b in reads:
            b.r[name] = (s, v)
        for b in writes:
            b.w = {name: (s, v)}
            b.r = {}

    def op(self, k, fn, reads, writes):
        self._wait(k, self._deps(reads, writes))
        ins = fn(self.engs[k])
        c = self.cur[k]
        if c[2] >= SEM_LIMIT:
            self._newsem(k)
            c = self.cur[k]
        c[2] += 1
        ins.then_inc(c[1], 1)
        self.ninst += 1
        self._record((c[0], c[1], c[2]), reads, writes)
        if self.hook is not None:
            self.hook()

    def dma(self, q, fn, reads, writes, slot=None, pre=()):
        if slot is None:
            slots = self.dsem[q]
            i = self.drr[q]
            self.drr[q] = (i + 1) % len(slots)
            sl = slots[i]
        else:
            sl = slot
        deps = self._deps(reads, list(writes) + list(pre))
        if slot is None and sl[2] > 0:
            deps[sl[0]] = (sl[1], sl[2])
        self._wait(q, deps)
        ins = fn(self.engs[q])
        sl[2] += 16
        ins.then_inc(sl[1], 16)
        self.ninst += 1
        self._record((sl[0], sl[1], sl[2]), reads, writes)
        self.last_written = writes[0] if writes else None
        if self.hook is not None:
            self.hook()

    def barrier(self):
        evs = {}
        for k, c in self.cur.items():
            if c[2] > 0:
                evs[c[0]] = (c[1], c[2])
        for q, slots in self.dsem.items():
            for sl in slots:
                if sl[2] > 0:
                    evs[sl[0]] = (sl[1], sl[2])
        for k in self.engs:
            self._wait(k, dict(evs))


def interleave(fw, fns, weights):
    import threading
    n = len(fns)
    sems = [threading.Semaphore(0) for _ in fns]
    done = [False] * n
    st = {"cur": 0, "cnt": 0}
    errs = []
    fin = threading.Semaphore(0)

    def nxt(i):
        for k in range(1, n + 1):
            j = (i + k) % n
            if not done[j]:
                return j
        return None

    def hook():
        i = st["cur"]
        st["cnt"] += 1
        if st["cnt"] >= weights[i]:
            st["cnt"] = 0
            j = nxt(i)
            if j is not None and j != i:
                st["cur"] = j
                sems[j].release()
                sems[i].acquire()

    def runner(i):
        sems[i].acquire()
        try:
            fns[i]()
        except BaseException as e:
            errs.append(e)
        done[i] = True
        j = nxt(i)
        if j is not None:
            st["cur"] = j
            st["cnt"] = 0
            sems[j].release()
        else:
            fin.release()

    ths = [threading.Thread(target=runner, args=(i,)) for i in range(n)]
    old = fw.hook
    fw.hook = hook
    for t in ths:
        t.start()
    st["cur"] = 0
    sems[0].release()
    fin.acquire()
    for t in ths:
        t.join()
    fw.hook = old
    if errs:
        raise errs[0]


class ViewBuf(Reg):
    def __init__(self, ap):
        Reg.__init__(self)
        self.ap_ = ap

    def __getitem__(self, k):
        return self.ap_[k]


def build(debug=None):
    nc = bass.Bass("TRN2", target_bir_lowering=False)
    fw = FW(nc)

    def din(name, shape, dt=F32):
        return nc.dram_tensor(name, list(shape), dt, kind="ExternalInput").ap()

    x_b = din("x_b", [S, D])
    x_own = din("x_own", [NOWN * 128, D])
    c_b = din("c_b", [1, D])
    w_ada = din("w_ada", [D, 6 * D])
    b_ada = din("b_ada", [1, 6 * D])
    norm1_g = din("norm1_g", [1, D])
    w_in = din("w_in", [D, 4608])
    lq1 = din("da_lambda_q1", [1, 64])
    lk1 = din("da_lambda_k1", [1, 64])
    lq2 = din("da_lambda_q2", [1, 64])
    lk2 = din("da_lambda_k2", [1, 64])
    head_g = din("da_head_g", [1, 128])
    ln_g = din("sg_ln_g", [1, 512])
    ln_b = din("sg_ln_b", [1, 512])
    sg_w = din("sg_w", [4, 128, 128])
    sg_b = din("sg_b", [4, 128])
    w_ba = din("w_branch_a", [512, D])
    w_bb = din("w_branch_b", [512, D])
    w_out = din("w_out", [D, D])
    norm2_g = din("norm2_g", [1, D])
    w_pq = din("peer_w_query", [D, 2048])
    sub_keys = din("peer_sub_keys", [16, 128, 128])
    p_down = din("peer_down", [16384, D])
    p_up = din("peer_up", [16384, D])
    final_g = din("final_g", [1, D])
    amask = din("amask", [128, 256])
    out_d = nc.dram_tensor("out", [NOWN * 128, D], F32, kind="ExternalOutput").ap()
    dbg_d = None
    if debug is not None:
        dbg_d = nc.dram_tensor("dbg", list(debug[1]), F32, kind="ExternalOutput").ap()

    winb = nc.dram_tensor("winb", [D, 4608], BF16, kind="Internal").ap()
    wbab = nc.dram_tensor("wbab", [512, D], BF16, kind="Internal").ap()
    wbbb = nc.dram_tensor("wbbb", [512, D], BF16, kind="Internal").ap()
    woutb = nc.dram_tensor("woutb", [D, D], BF16, kind="Internal").ap()
    wpqb = nc.dram_tensor("wpqb", [D, 2048], BF16, kind="Internal").ap()
    modscr = nc.dram_tensor("modscr", [1, 2048], F32, kind="Internal").ap()
    pdu = nc.dram_tensor("pdu", [16384, 2 * D], BF16, kind="Internal").ap()
    tregs = []
    tconv = [(pdu[:, hf * D:(hf + 1) * D], src, r0) for hf, src in enumerate((p_down, p_up)) for r0 in range(0, 16384, 1024)]
    wregs = []
    r_modscr = Reg()
    r_none = Reg()
    yaregs = []

    def sb(name, shape, dt=F32):
        return Buf(nc.alloc_sbuf_tensor(name, list(shape), dt))

    P2 = [nc.alloc_psum_tensor(f"ps{i}", [128, 1024], F32) for i in range(4)]
    PR = [Reg() for _ in range(8)]

    def bank(i):
        return P2[i // 2][:, (i % 2) * 512:(i % 2 + 1) * 512]

    def bank_bf(i):
        return P2[i // 2][:, :].bitcast(BF16)[:, (i % 2) * 1024:(i % 2 + 1) * 1024]

    def pair(i):
        return P2[i][:, :]

    identf = sb("identf", [128, 128])
    identb = sb("identb", [128, 128], BF16)
    mod_bc = sb("mod_bc", [128, 6 * D])
    fg_bc = sb("fg_bc", [128, D])
    yaTd = nc.dram_tensor("yaTd", [4, 128, NOWN * 128], BF16, kind="Internal").ap()
    r_yaT = [Reg() for _ in range(8)]
    ysts = [sb("yst0", [128, 128], BF16), sb("yst1", [128, 128], BF16)]
    ysi = [0]
    A1col = sb("A1col", [128, 8])
    B1col = sb("B1col", [128, 8])
    neglam = sb("neglam", [128, 1])
    hg_bc = sb("hg_bc", [128, 128])
    lng_bc = sb("lng_bc", [128, 512])
    lnb_bc = sb("lnb_bc", [128, 512])
    bs_col = sb("bs_col", [128, 4])
    maskb = sb("maskb", [128, 256], BF16)
    wsT = sb("wsT", [128, 4, 128], BF16)
    skT = sb("skT", [128, 16, 128], BF16)
    ss_r = [sb(f"ss{i}", [128, 4]) for i in range(4)]
    ss_i = [0]

    def dbg_out(buf_ap, reads, dst=None):
        fw.dma("sp", lambda e: e.dma_start(out=(dbg_d if dst is None else dst), in_=buf_ap), reads, [r_none])

    fw.op("pool", lambda e: e.memset(identf[:, :], 1.0), [], [identf])
    fw.op("pool", lambda e: e.affine_select(out=identf[:, :], in_=identf[:, :], pattern=[[-1, 128]],
                                            compare_op=OP.is_equal, fill=0.0, base=0, channel_multiplier=1),
          [identf], [identf])
    fw.op("dve", lambda e: e.tensor_copy(out=identb[:, :], in_=identf[:, :]), [identf], [identb])

    for (dst, src, ncol) in ((winb, w_in, 4608), (wbab, w_ba, D), (wbbb, w_bb, D), (woutb, w_out, D), (wpqb, w_pq, 2048)):
        for c0 in range(0, ncol, 512):
            fw.dma("pool", lambda e, dst=dst, src=src, c0=c0: e.dma_start(out=dst[:, c0:c0 + 512], in_=src[:, c0:c0 + 512]),
                   [], [Reg()])
            wregs.append(fw.last_written)

    with nc.allow_non_contiguous_dma(reason="tiny param loads"):
        n1g_col = sb("n1g_col", [128, 8])
        c_col = sb("c_col", [128, 8])
        fw.dma("sp", lambda e: e.dma_start(out=n1g_col[:, :], in_=norm1_g[0, :].rearrange("(c p) -> p c", p=128)), [], [n1g_col])
        fw.dma("sp", lambda e: e.dma_start(out=c_col[:, :], in_=c_b[0, :].rearrange("(c p) -> p c", p=128)), [], [c_col])
        fw.dma("sp", lambda e: e.dma_start(out=bs_col[:, :], in_=sg_b.rearrange("g p -> p g")), [], [bs_col])
    fw.dma("sp", lambda e: e.dma_start(out=mod_bc[:, :], in_=b_ada[0:1, :].partition_broadcast(128)), [], [mod_bc])
    fw.dma("sp", lambda e: e.dma_start(out=fg_bc[:, :], in_=final_g[0:1, :].partition_broadcast(128)), [], [fg_bc])
    fw.dma("sp", lambda e: e.dma_start(out=hg_bc[:, :], in_=head_g[0:1, :].partition_broadcast(128)), [], [hg_bc])
    fw.dma("sp", lambda e: e.dma_start(out=lng_bc[:, :], in_=ln_g[0:1, :].partition_broadcast(128)), [], [lng_bc])
    fw.dma("sp", lambda e: e.dma_start(out=lnb_bc[:, :], in_=ln_b[0:1, :].partition_broadcast(128)), [], [lnb_bc])
    fw.op("dve", lambda e: e.tensor_scalar(out=hg_bc[:, :], in0=hg_bc[:, :], scalar1=1.0 - LAM_INIT, scalar2=None, op0=OP.mult),
          [hg_bc], [hg_bc])

    with ExitStack() as es:
        def tl(name, shape, dt=F32):
            return es.enter_context(nc.sbuf_tensor(name, list(shape), dt))
        wa0_t = tl("s0a", [128, 8, 512]); wa1_t = tl("s0b", [128, 8, 512]); csbc_t = tl("s0c", [128, 8, 128])
        lam_t = tl("s0d", [128, 4, 64]); n2g_t = tl("s0e", [128, 1024]); mcol_t = tl("s0f", [128, 16])
        tmp0_t = tl("s0g", [128, 512]); lsc_t = tl("s0h", [128, 8])
        wab = [Buf(wa0_t), Buf(wa1_t)]
        csbc = Buf(csbc_t)
        lam4 = Buf(lam_t)
        n2g = Buf(n2g_t)
        mcol = Buf(mcol_t)
        tmp0 = Buf(tmp0_t)
        lsc = Buf(lsc_t)
        for i, src in enumerate((lq1, lk1, lq2, lk2)):
            fw.dma("sp", lambda e, i=i, src=src: e.dma_start(out=lam4[:, i, :], in_=src[0:1, :].partition_broadcast(128)), [], [lam4])
        fw.op("dve", lambda e: e.tensor_tensor(out=lam4[:, 0, :], in0=lam4[:, 0, :], in1=lam4[:, 1, :], op=OP.mult), [lam4], [lam4])
        fw.op("dve", lambda e: e.tensor_tensor(out=lam4[:, 2, :], in0=lam4[:, 2, :], in1=lam4[:, 3, :], op=OP.mult), [lam4], [lam4])
        fw.op("dve", lambda e: e.tensor_reduce(out=lsc[:, 0:1], in_=lam4[:, 0, :], axis=AX.X, op=OP.add), [lam4], [lsc])
        fw.op("dve", lambda e: e.tensor_reduce(out=lsc[:, 1:2], in_=lam4[:, 2, :], axis=AX.X, op=OP.add), [lam4], [lsc])
        fw.op("act", lambda e: e.activation(out=lsc[:, 2:4], in_=lsc[:, 0:2], func=AF.Exp), [lsc], [lsc])
        fw.op("dve", lambda e: e.tensor_tensor(out=lsc[:, 4:5], in0=lsc[:, 3:4], in1=lsc[:, 2:3], op=OP.subtract), [lsc], [lsc])
        fw.op("dve", lambda e: e.tensor_scalar(out=neglam[:, :], in0=lsc[:, 4:5], scalar1=-LAM_INIT, scalar2=None, op0=OP.add),
              [lsc], [neglam])
        fw.dma("sp", lambda e: e.dma_start(out=tmp0[:, 0:256], in_=amask[:, :]), [], [tmp0])
        fw.op("dve", lambda e: e.tensor_copy(out=maskb[:, :], in_=tmp0[:, 0:256]), [tmp0], [maskb])
        for g in range(4):
            fw.dma("sp", lambda e, g=g: e.dma_start(out=tmp0[:, 0:128], in_=sg_w[g, :, :]), [], [tmp0])
            fw.op("dve", lambda e: e.memset(tmp0[0:64, 64:128], 0.0), [tmp0], [tmp0])
            fw.op("pe", lambda e: e.transpose(out=bank(0)[:, 0:128], in_=tmp0[:, 0:128], identity=identf[:, :]), [tmp0, identf], [PR[0]])
            fw.op("act", lambda e, g=g: e.activation(out=wsT[:, g, :], in_=bank(0)[:, 0:128], func=AF.Copy), [PR[0]], [wsT])
        for l in range(16):
            fw.dma("sp", lambda e, l=l: e.dma_start(out=tmp0[:, 0:128], in_=sub_keys[l, :, :]), [], [tmp0])
            fw.op("pe", lambda e: e.transpose(out=bank(1)[:, 0:128], in_=tmp0[:, 0:128], identity=identf[:, :]), [tmp0, identf], [PR[1]])
            fw.op("act", lambda e, l=l: e.activation(out=skT[:, l, :], in_=bank(1)[:, 0:128], func=AF.Copy), [PR[1]], [skT])
        fw.op("act", lambda e: e.activation(out=c_col[:, :], in_=c_col[:, :], func=AF.Silu), [c_col], [c_col])
        fw.op("dve", lambda e: e.tensor_copy(out=csbc[:, :, :], in_=c_col[:, :].unsqueeze(2).to_broadcast([128, 8, 128])), [c_col], [csbc])
        w_ada_v = w_ada.rearrange("(c p) n -> p c n", p=128)
        for ec in range(12):
            wb_ = wab[ec % 2]
            fw.dma("sp" if ec % 2 == 0 else "act", lambda e, ec=ec, wb_=wb_: e.dma_start(out=wb_[:, :, :], in_=w_ada_v[:, :, ec * 512:(ec + 1) * 512]), [], [wb_])
            pb = 2 + (ec % 2)
            for c in range(8):
                fw.op("pe", lambda e, c=c, wb_=wb_, pb=pb: e.matmul(bank(pb), lhsT=csbc[:, c, :], rhs=wb_[:, c, :], start=(c == 0), stop=(c == 7)),
                      [csbc, wb_], [PR[pb]])
            fw.op("dve", lambda e, ec=ec, pb=pb: e.tensor_tensor(out=mod_bc[:, ec * 512:(ec + 1) * 512], in0=bank(pb), in1=mod_bc[:, ec * 512:(ec + 1) * 512], op=OP.add),
                  [PR[pb], mod_bc], [mod_bc])
        fw.dma("sp", lambda e: e.dma_start(out=modscr[0:1, :], in_=mod_bc[0:1, 0:2048]), [mod_bc], [r_modscr])
        with nc.allow_non_contiguous_dma(reason="tiny param loads"):
            fw.dma("sp", lambda e: e.dma_start(out=mcol[:, :], in_=modscr[0, :].rearrange("(c p) -> p c", p=128)), [r_modscr], [mcol])
        fw.op("dve", lambda e: e.tensor_copy(out=B1col[:, :], in_=mcol[:, 0:8]), [mcol], [B1col])
        fw.op("dve", lambda e: e.scalar_tensor_tensor(out=A1col[:, :], in0=mcol[:, 8:16], scalar=1.0, in1=n1g_col[:, :], op0=OP.add, op1=OP.mult),
              [mcol, n1g_col], [A1col])
        fw.dma("sp", lambda e: e.dma_start(out=n2g[:, :], in_=norm2_g[0:1, :].partition_broadcast(128)), [], [n2g])
        fw.op("dve", lambda e: e.scalar_tensor_tensor(out=mod_bc[:, 4 * D:5 * D], in0=mod_bc[:, 4 * D:5 * D], scalar=1.0, in1=n2g[:, :], op0=OP.add, op1=OP.mult),
              [mod_bc, n2g], [mod_bc])
        fw.barrier()
    if debug is not None and debug[0] == "mod":
        dbg_out(mod_bc[:, :], [mod_bc])
        fw.barrier()
        return nc

    A1bc = A1col[:, :].unsqueeze(2).to_broadcast([128, 8, 128])
    B1bc = B1col[:, :].unsqueeze(2).to_broadcast([128, 8, 128])

    def rstd_of(ssbuf, col, n, tmpcol):
        fw.op("dve", lambda e: e.tensor_scalar(out=ssbuf[:, tmpcol:tmpcol + 1], in0=ssbuf[:, col:col + 1], scalar1=1.0 / n, scalar2=EPS, op0=OP.mult, op1=OP.add),
              [ssbuf], [ssbuf])
        fw.op("act", lambda e: e.activation(out=ssbuf[:, tmpcol:tmpcol + 1], in_=ssbuf[:, tmpcol:tmpcol + 1], func=AF.Ln), [ssbuf], [ssbuf])
        fw.op("act", lambda e: e.activation(out=ssbuf[:, tmpcol:tmpcol + 1], in_=ssbuf[:, tmpcol:tmpcol + 1], func=AF.Exp, scale=-0.5), [ssbuf], [ssbuf])

    def norm1_block(src_rows, xin, xs, tmpf, hT, hcols, pb, add_eng="pool"):
        ssb = ss_r[ss_i[0] % 4]
        ss_i[0] += 1
        if src_rows is not None:
            fw.dma("sp", lambda e: e.dma_start(out=xin[:, :], in_=src_rows), [], [xin])
        fw.op("act", lambda e: e.activation(out=xs[:, :], in_=xin[:, :], func=AF.Square, accum_out=ssb[:, 0:1]), [xin], [xs, ssb])
        rstd_of(ssb, 0, D, 1)
        if add_eng == "act":
            fw.op("act", lambda e: e.activation(out=xs[:, :], in_=xin[:, :], func=AF.Copy, scale=ssb[:, 1:2]), [xin, ssb], [xs])
        else:
            fw.op("dve", lambda e: e.tensor_scalar(out=xs[:, :], in0=xin[:, :], scalar1=ssb[:, 1:2], scalar2=None, op0=OP.mult), [xin, ssb], [xs])
        tpv = bank_bf(pb)
        for c in range(8):
            fw.op("pe", lambda e, c=c: e.transpose(out=tpv[:, c * 128:(c + 1) * 128], in_=xs[:, c * 128:(c + 1) * 128], identity=identb[:, :]),
                  [xs, identb], [PR[pb]])
        if add_eng == "act":
            for c in range(8):
                fw.op("act", lambda e, c=c: e.activation(out=hT[:, c, hcols], in_=tpv[:, c * 128:(c + 1) * 128], func=AF.Identity, scale=A1col[:, c:c + 1], bias=B1col[:, c:c + 1]),
                      [PR[pb], A1col, B1col], [hT])
            return
        fw.op("dve", lambda e: e.tensor_tensor(out=tmpf[:, :].rearrange("p (c t) -> p c t", c=8), in0=tpv.rearrange("p (c t) -> p c t", c=8), in1=A1bc, op=OP.mult),
              [PR[pb], A1col], [tmpf])
        fw.op(add_eng, lambda e: e.tensor_tensor(out=hT[:, :, hcols], in0=tmpf[:, :].rearrange("p (c t) -> p c t", c=8), in1=B1bc, op=OP.add),
              [tmpf, B1col], [hT])

    def norm_p1(src_rows, xin, xs):
        ssb = ss_r[ss_i[0] % 4]
        ss_i[0] += 1
        fw.dma("sp", lambda e: e.dma_start(out=xin[:, :], in_=src_rows), [], [xin])
        fw.op("act", lambda e: e.activation(out=xs[:, :], in_=xin[:, :], func=AF.Square, accum_out=ssb[:, 0:1]), [xin], [xs, ssb])
        rstd_of(ssb, 0, D, 1)
        fw.op("dve", lambda e: e.tensor_scalar(out=xs[:, :], in0=xin[:, :], scalar1=ssb[:, 1:2], scalar2=None, op0=OP.mult), [xin, ssb], [xs])

    def norm_p2(xs, tmpf, hT, hcols, pb):
        tpv = bank_bf(pb)
        for c in range(8):
            fw.op("pe", lambda e, c=c: e.transpose(out=tpv[:, c * 128:(c + 1) * 128], in_=xs[:, c * 128:(c + 1) * 128], identity=identb[:, :]),
                  [xs, identb], [PR[pb]])
        fw.op("dve", lambda e: e.tensor_tensor(out=tmpf[:, :].rearrange("p (c t) -> p c t", c=8), in0=tpv.rearrange("p (c t) -> p c t", c=8), in1=A1bc, op=OP.mult),
              [PR[pb], A1col], [tmpf])
        fw.op("pool", lambda e: e.tensor_tensor(out=hT[:, :, hcols], in0=tmpf[:, :].rearrange("p (c t) -> p c t", c=8), in1=B1bc, op=OP.add),
              [tmpf, B1col], [hT])

    winb_v = winb.rearrange("(c p) n -> p c n", p=128)

    for hp in range(2):
        with ExitStack() as es:
            def tl(name, shape, dt=F32):
                return es.enter_context(nc.sbuf_tensor(f"{name}_p{hp}", list(shape), dt))
            KT_t = tl("KT", [128, 2, S], BF16); V_t = tl("VV", [128, NB, 2, 129], BF16)
            wk_t = tl("wk", [128, 8, 256], BF16); wv_t = tl("wv", [128, 8, 256], BF16); wq_t = tl("wq", [128, 8, 256], BF16)
            xin0_t = tl("xin0", [128, D]); xin1_t = tl("xin1", [128, D]); xin2_t = tl("xin2", [128, D])
            xs0_t = tl("xs0", [128, D], BF16); xs1_t = tl("xs1", [128, D], BF16)
            xsB = [Buf(tl(f"xsB{i}", [128, D], BF16)) for i in range(4)]
            tmpf0_t = tl("tmpf0", [128, D]); tmpf1_t = tl("tmpf1", [128, D])
            hT0_t = tl("hT0", [128, 8, 512], BF16); hT1_t = tl("hT1", [128, 8, 512], BF16)
            QT0_t = tl("QT0", [128, 2, 512], BF16); QT1_t = tl("QT1", [128, 2, 512], BF16)
            PT0_t = tl("PT0", [128, 512], BF16); PT1_t = tl("PT1", [128, 512], BF16); PT2_t = tl("PT2", [128, 512], BF16); PT3_t = tl("PT3", [128, 512], BF16)
            at0_t = tl("at0", [128, 128]); at1_t = tl("at1", [128, 128])
            ab0_t = tl("ab0", [128, 128], BF16); ab1_t = tl("ab1", [128, 128], BF16)
            asm_t = tl("asm", [128, 8])
            KT = KT_t
            V = V_t
            rKT = [Reg() for _ in range(16)]
            rV = [Reg() for _ in range(16)]
            wk, wv, wq = Buf(wk_t), Buf(wv_t), Buf(wq_t)
            xins = [Buf(xin0_t), Buf(xin1_t), Buf(xin2_t)]
            xss = [Buf(xs0_t), Buf(xs1_t)]
            tmpfs = [Buf(tmpf0_t), Buf(tmpf1_t)]
            hTs = [Buf(hT0_t), Buf(hT1_t)]
            QTs = [Buf(QT0_t), Buf(QT1_t)]
            PTs = [Buf(PT0_t), Buf(PT1_t), Buf(PT2_t), Buf(PT3_t)]
            ats = [Buf(at0_t), Buf(at1_t)]
            abs_ = [Buf(ab0_t), Buf(ab1_t)]
            asm = Buf(asm_t)
            qc0, kc0, vc0 = hp * 256, 512 + hp * 256, 1024 + hp * 256
            fw.dma("sp", lambda e: e.dma_start(out=wq[:, :, :], in_=winb_v[:, :, qc0:qc0 + 256]), wregs, [wq])
            fw.dma("sp", lambda e: e.dma_start(out=wk[:, :, :], in_=winb_v[:, :, kc0:kc0 + 256]), wregs, [wk])
            fw.dma("sp", lambda e: e.dma_start(out=wv[:, :, :], in_=winb_v[:, :, vc0:vc0 + 256]), wregs, [wv])
            rVall = Reg()
            fw.op("pool", lambda e: e.memset(V[:, :, :, 128:129], 1.0), [], [rVall])
            for r_ in rV:
                r_.w = dict(rVall.w)
            nbc = [0]

            def normS(sbi):
                if True:
                    hT = hTs[sbi % 2]
                    if hp == 0:
                        for dst_, src_, r0_ in tconv[2 * sbi:2 * sbi + 2]:
                            fw.dma("pool", lambda e: e.dma_start(out=dst_[r0_:r0_ + 1024, :], in_=src_[r0_:r0_ + 1024, :]), [], [Reg()])
                            tregs.append(fw.last_written)
                    for bl in range(4):
                        gb = sbi * 4 + bl
                        nb_ = nbc[0]
                        nbc[0] += 1
                        norm1_block(x_b[gb * 128:(gb + 1) * 128, :], xins[nb_ % 3], xss[nb_ % 2], tmpfs[nb_ % 2], hT,
                                    slice(bl * 128, (bl + 1) * 128), nb_ % 2)

            def kvS(sbi):
                if True:
                    hT = hTs[sbi % 2]
                    for hh in range(2):
                        pb = 2 + hh
                        for c in range(8):
                            fw.op("pe", lambda e: e.matmul(bank(pb), lhsT=wk[:, c, hh * 128:(hh + 1) * 128], rhs=hT[:, c, :], start=(c == 0), stop=(c == 7)),
                                  [wk, hT], [PR[pb]])
                        fw.op("act", lambda e: e.activation(out=KT[:, hh, sbi * 512:(sbi + 1) * 512], in_=bank(pb), func=AF.Copy),
                              [PR[pb]], [rKT[sbi]])
                    for bl in range(4):
                        gb = sbi * 4 + bl
                        pb = 4 + (bl % 2)
                        for c in range(8):
                            fw.op("pe", lambda e: e.matmul(bank(pb)[:, 0:256], lhsT=hT[:, c, bl * 128:(bl + 1) * 128], rhs=wv[:, c, :], start=(c == 0), stop=(c == 7)),
                                  [wv, hT], [PR[pb]])
                        fw.op("dve", lambda e: e.tensor_copy(out=V[:, gb, :, 0:128], in_=bank(pb)[:, 0:256].rearrange("p (h v) -> p h v", h=2)),
                              [PR[pb]], [rV[sbi]])

            normS(0)
            for sbi in range(16):
                if sbi + 1 < 16:
                    interleave(fw, [lambda: normS(sbi + 1), lambda: kvS(sbi)], [1, 1])
                else:
                    kvS(sbi)
            nblk = nbc[0]
            if debug is not None and debug[0] == "kv" and hp == 0:
                with nc.sbuf_tensor("dbgt", [128, 2048], F32) as dt_:
                    dtb = Buf(dt_)
                    fw.op("dve", lambda e: e.tensor_copy(out=dtb[:, 0:1024], in_=KT[:, 1, 7168:8192]), rKT, [dtb])
                    fw.op("dve", lambda e: e.tensor_copy(out=dtb[:, 1024:2048].rearrange("p (b v) -> p b v", b=8), in_=V[:, 56:64, 1, 0:128]), rV, [dtb])
                    dbg_out(dtb[:, :], [dtb])
                    fw.barrier()
                    return nc
            def b_p1(g_):
                for qi_ in range(4):
                    j_ = 4 * g_ + qi_
                    norm_p1(x_own[j_ * 128:(j_ + 1) * 128, :], xins[(4 * g_ + qi_) % 3], xsB[qi_])

            b_p1(0)
            for g in range(8):
                hT = hTs[g % 2]
                QT = QTs[g % 2]
                for qi in range(4):
                    norm_p2(xsB[qi], tmpfs[qi % 2], hT, slice(qi * 128, (qi + 1) * 128), qi % 2)
                for hh in range(2):
                    pb = 2 + hh
                    for c in range(8):
                        fw.op("pe", lambda e, c=c, hh=hh, pb=pb: e.matmul(bank(pb), lhsT=wq[:, c, hh * 128:(hh + 1) * 128], rhs=hT[:, c, :], start=(c == 0), stop=(c == 7)),
                              [wq, hT], [PR[pb]])
                    fw.op("act", lambda e, hh=hh, pb=pb: e.activation(out=QT[:, hh, :], in_=bank(pb), func=AF.Copy, scale=0.125),
                          [PR[pb]], [QT])
                def attn_g():
                    for hh in range(2):
                        kmax = 8 * g + 7

                        def emit_S(kb):
                            qmin = max(0, (kb - 8 * g) // 2)
                            c0 = qmin * 128
                            for half in range(2):
                                spb = 4 + 2 * (kb % 2) + half
                                rs = slice(half * 64, (half + 1) * 64)
                                fw.op("pe", lambda e: e.matmul(bank(spb)[:, c0:512], lhsT=KT[rs, hh, kb * 128:(kb + 1) * 128], rhs=QT[rs, hh, c0:512], start=True, stop=True),
                                      [rKT[kb // 4], QT], [PR[spb]])
                            for half in range(2):
                                spb = 4 + 2 * (kb % 2) + half
                                PT = PTs[2 * (kb % 2) + half]
                                fw.op("act", lambda e: e.activation(out=PT[:, c0:512], in_=bank(spb)[:, c0:512], func=AF.Exp),
                                      [PR[spb]], [PT])
                                for qi in range(qmin, 4):
                                    j = 4 * g + qi
                                    m = kb - 2 * j
                                    if m in (0, 1):
                                        fw.op("pool", lambda e: e.tensor_tensor(out=PT[:, qi * 128:(qi + 1) * 128], in0=PT[:, qi * 128:(qi + 1) * 128], in1=maskb[:, m * 128:(m + 1) * 128], op=OP.mult),
                                              [PT, maskb], [PT])

                        def emit_PV(kb):
                            qmin = max(0, (kb - 8 * g) // 2)
                            for half in range(2):
                                PT = PTs[2 * (kb % 2) + half]
                                for qi in range(qmin, 4):
                                    j = 4 * g + qi
                                    ob = qi
                                    ov = bank(ob)[:, half * 129:(half + 1) * 129]
                                    fw.op("pe", lambda e: e.matmul(ov, lhsT=PT[:, qi * 128:(qi + 1) * 128], rhs=V[:, kb, hh, :], start=(kb == 0 and half == 0), stop=(kb == 2 * j + 1 and half == 1)),
                                          [PT, rV[kb // 4]], [PR[ob]])

                        emit_S(0)
                        for kb in range(kmax + 1):
                            if kb + 1 <= kmax:
                                emit_S(kb + 1)
                            emit_PV(kb)
                        for qi in range(4):
                            j = 4 * g + qi
                            ob = qi
                            at = ats[qi % 2]
                            ab = abs_[qi % 2]
                            o3 = bank(ob)[:, 0:258].rearrange("p (a b) -> p a b", a=2)
                            fw.op("dve", lambda e, o3=o3: e.reciprocal(out=asm[:, 0:2], in_=o3[:, :, 128]), [PR[ob]], [asm])
                            fw.op("dve", lambda e: e.tensor_tensor(out=asm[:, 2:3], in0=asm[:, 1:2], in1=neglam[:, :], op=OP.mult), [asm, neglam], [asm])
                            fw.op("dve", lambda e, o3=o3, at=at: e.tensor_scalar(out=at[:, :], in0=o3[:, 0, 0:128], scalar1=asm[:, 0:1], scalar2=None, op0=OP.mult), [PR[ob], asm], [at])
                            fw.op("dve", lambda e, o3=o3, at=at: e.scalar_tensor_tensor(out=at[:, :], in0=o3[:, 1, 0:128], scalar=asm[:, 2:3], in1=at[:, :], op0=OP.mult, op1=OP.add), [PR[ob], asm, at], [at])
                            fw.op("act", lambda e, at=at, ab=ab: e.activation(out=ab[:, :], in_=at[:, :], func=AF.Square, accum_out=asm[:, 3:4]), [at], [ab, asm])
                            rstd_of(asm, 3, 128, 4)
                            fw.op("dve", lambda e, at=at, ab=ab: e.scalar_tensor_tensor(out=ab[:, :], in0=at[:, :], scalar=asm[:, 4:5], in1=hg_bc[:, :], op0=OP.mult, op1=OP.mult), [at, asm, hg_bc], [ab])
                            tb = 6 + (qi % 2)
                            fw.op("pe", lambda e, ab=ab, tb=tb: e.transpose(out=bank_bf(tb)[:, 0:128], in_=ab[:, :], identity=identb[:, :]), [ab, identb], [PR[tb]])
                            yst = ysts[ysi[0] % 2]
                            ysi[0] += 1
                            fw.op("act", lambda e, tb=tb: e.activation(out=yst[:, :], in_=bank_bf(tb)[:, 0:128], func=AF.Copy), [PR[tb]], [yst])
                            rr_ = Reg()
                            fw.dma("sp", lambda e, j=j, hh=hh: e.dma_start(out=yaTd[hp * 2 + hh, :, j * 128:(j + 1) * 128], in_=yst[:, :]), [yst], [rr_])
                            yaregs.append(rr_)
                if g + 1 < 8:
                    interleave(fw, [attn_g, lambda: b_p1(g + 1)], [8, 1])
                else:
                    attn_g()
            fw.barrier()
    def tl(name, shape, dt=F32):
        return Buf(nc.alloc_sbuf_tensor(name, list(shape), dt))

    wbab_v = wbab.rearrange("(c p) n -> p c n", p=128)
    wbbb_v = wbbb.rearrange("(c p) n -> p c n", p=128)
    woutb_v = woutb.rearrange("(c p) n -> p c n", p=128)
    wpqb_v = wpqb.rearrange("(c p) n -> p c n", p=128)
    Wsl = [tl(f"Wsl{i}", [128, 4096], BF16) for i in range(5)]
    wi = [0]

    def load_w(view3, c):
        w = Wsl[wi[0] % 5]
        wi[0] += 1
        fw.dma("sp", lambda e: e.dma_start(out=w[:, :].rearrange("p (c n) -> p c n", c=c), in_=view3), wregs, [w])
        return w, w[:, :].rearrange("p (c n) -> p c n", c=c)

    xgs = [[tl(f"xg{p}{b}", [128, D]) for b in range(2)] for p in range(2)]
    xsC = tl("xsC", [128, D], BF16)
    tmpfC = tl("tmpfC", [128, D])
    hTC = tl("hTC", [128, 8, 256], BF16)
    gTC = tl("gTC", [128, 16, 256], BF16)
    gu = tl("gu", [128, 512])
    gs = tl("gs", [128, 512])
    svn = tl("svn", [128, 512], BF16)
    ybt = tl("ybt", [128, 512], BF16)
    ybT = tl("ybT", [128, 4, 256], BF16)
    t5a = tl("t5a", [128, 256])
    t5b = tl("t5b", [128, 256])
    mT = tl("mT", [128, 8, 256], BF16)
    h2gs = [[tl(f"h2g{p}{b}", [128, D], BF16) for b in range(2)] for p in range(2)]
    st6 = tl("st6", [128, 12])
    scf = tl("scf", [128, 2048])
    scw = tl("scw", [128, 2048])
    tv = tl("tv", [128, 256])
    ti = tl("ti", [128, 256], U32)
    tif = tl("tif", [128, 256])
    cv = tl("cv", [128, 128])
    ci = tl("ci", [128, 128], U32)
    cif = tl("cif", [128, 128])
    gsm = tl("gsm", [128, 128])
    sm8 = tl("sm8", [128, 8])
    ak = tl("ak", [128, 128])
    bk = tl("bk", [128, 128])
    i1s = tl("i1s", [128, 128])
    i2s = tl("i2s", [128, 128])
    eidxf = tl("eidxf", [128, 128])
    eTs = [[tl(f"eT{p}{b}", [128, 128], U32) for b in range(2)] for p in range(2)]
    gTs_ = [[tl(f"gT_{p}{b}", [128, 128]) for b in range(2)] for p in range(2)]
    yaGs = [tl("yaG0", [128, 4, 256], BF16), tl("yaG1", [128, 4, 256], BF16)]
    tmpfL = ViewBuf(mod_bc.t[:, 0:1024])
    junkL = ViewBuf(mod_bc.t[:, 1024:2048].bitcast(BF16)[:, 0:1024])
    aT = tl("aT", [128, 128])
    coefT = tl("coefT", [128, 128])
    NGB = 9
    Gbufs = [tl(f"Gbuf{i}", [128, 2 * D], BF16) for i in range(NGB)]
    gbi = [0]

    gslots = [[f"d_g{i}", nc.alloc_semaphore(f"d_g{i}"), 0] for i in range(NGB)]

    def gather(table, idx_ap, rd):
        k = gbi[0] % NGB
        b_ = Gbufs[k]
        pre = [Gbufs[(k + 1) % NGB]] if k % 2 == 0 else []
        gbi[0] += 1
        fw.dma("pool", lambda e: e.indirect_dma_start(out=b_[:, :], out_offset=None, in_=table[:, :], in_offset=bass.IndirectOffsetOnAxis(ap=idx_ap, axis=0)),
               rd, [b_], slot=gslots[k], pre=pre)
        return b_
    Wc = tl("Wc", [128, 256], BF16)
    Zts = [tl(f"Zt{i}", [128, 128], BF16) for i in range(4)]
    acols = [tl(f"acol{i}", [128, 1]) for i in range(8)]
    ccols = [tl(f"ccol{i}", [128, 1]) for i in range(8)]
    thr16 = tl("thr16", [128, 16])
    io16 = tl("io16", [128, 16])
    ioi = tl("ioi", [128, 16], I32)

    fw.op("pool", lambda e: e.memset(Wc[:, :], 0.0), [], [Wc])
    fw.op("pool", lambda e: e.memset(Wc[:, 127:128], 1.0), [Wc], [Wc])
    fw.op("pool", lambda e: e.iota(ioi[:, :], pattern=[[1, 16]], base=0, channel_multiplier=0), [], [ioi])
    fw.op("dve", lambda e: e.tensor_copy(out=io16[:, :], in_=ioi[:, :]), [ioi], [io16])
    fw.op("dve", lambda e: e.tensor_scalar(out=thr16[:, :], in0=io16[:, :], scalar1=16.0, scalar2=None, op0=OP.mult), [io16], [thr16])
    def top16(src3, wrk3, vals3, idx3, n, regs):
        r_src, r_wrk, r_vals, r_idx = regs
        for l in range(n):
            fw.op("dve", lambda e: e.max(out=vals3[:, l, 0:8], in_=src3[:, l, :]), [r_src], [r_vals])
            fw.op("dve", lambda e: e.max_index(out=idx3[:, l, 0:8], in_max=vals3[:, l, 0:8], in_values=src3[:, l, :]), [r_src, r_vals], [r_idx])
            fw.op("dve", lambda e: e.match_replace(out=wrk3[:, l, :], in_to_replace=vals3[:, l, 0:8], in_values=src3[:, l, :], imm_value=NEG), [r_src, r_vals], [r_wrk])
            fw.op("dve", lambda e: e.max(out=vals3[:, l, 8:16], in_=wrk3[:, l, :]), [r_wrk], [r_vals])
            fw.op("dve", lambda e: e.max_index(out=idx3[:, l, 8:16], in_max=vals3[:, l, 8:16], in_values=wrk3[:, l, :]), [r_wrk, r_vals], [r_idx])

    NGC = 16 if debug is None else debug[2]

    def cmain(g):
        p = g % 2
        xg = xgs[p]
        h2g = h2gs[p]
        for bl in range(2):
            j = 2 * g + bl
            fw.dma("sp", lambda e: e.dma_start(out=xg[bl][:, :], in_=x_own[j * 128:(j + 1) * 128, :]), [], [xg[bl]])
            norm1_block(None, xg[bl], xsC, tmpfC, hTC, slice(bl * 128, (bl + 1) * 128), bl, add_eng="act")
        for u in range(4):
            w, w3 = load_w(winb_v[:, :, 2560 + u * 512:2560 + (u + 1) * 512], 8)
            for cc in range(4):
                ch = u * 4 + cc
                pb = ch % 2
                for c in range(8):
                    fw.op("pe", lambda e: e.matmul(bank(pb)[:, 0:256], lhsT=w3[:, c, cc * 128:(cc + 1) * 128], rhs=hTC[:, c, :], start=(c == 0), stop=(c == 7)),
                          [w, hTC], [PR[pb]])
                fw.op("act", lambda e: e.activation(out=gTC[:, ch, :], in_=bank(pb)[:, 0:256], func=AF.Sigmoid), [PR[pb]], [gTC])
        wu, wu3 = load_w(winb_v[:, :, 1536:2048], 8)
        wsv, wsv3 = load_w(winb_v[:, :, 2048:2560], 8)
        for bl in range(2):
            bs_ = slice(bl * 128, (bl + 1) * 128)
            for c in range(8):
                fw.op("pe", lambda e: e.matmul(bank(0), lhsT=hTC[:, c, bs_], rhs=wu3[:, c, :], start=(c == 0), stop=(c == 7)), [wu, hTC], [PR[0]])
            for c in range(8):
                fw.op("pe", lambda e: e.matmul(bank(1), lhsT=hTC[:, c, bs_], rhs=wsv3[:, c, :], start=(c == 0), stop=(c == 7)), [wsv, hTC], [PR[1]])
            fw.op("act", lambda e: e.activation(out=gu[:, :], in_=bank(0), func=AF.Gelu_apprx_tanh), [PR[0]], [gu])
            fw.op("act", lambda e: e.activation(out=gs[:, :], in_=bank(1), func=AF.Gelu_apprx_tanh), [PR[1]], [gs])
            fw.op("dve", lambda e: e.bn_stats(out=st6[:, 0:6], in_=gs[:, :]), [gs], [st6])
            fw.op("dve", lambda e: e.bn_aggr(out=st6[:, 6:8], in_=st6[:, 0:6]), [st6], [st6])
            rstd_of(st6, 7, 1.0, 8)
            fw.op("dve", lambda e: e.tensor_scalar(out=gs[:, :], in0=gs[:, :], scalar1=st6[:, 6:7], scalar2=st6[:, 8:9], op0=OP.subtract, op1=OP.mult), [gs, st6], [gs])
            fw.op("dve", lambda e: e.tensor_tensor(out=gs[:, :], in0=gs[:, :], in1=lng_bc[:, :], op=OP.mult), [gs, lng_bc], [gs])
            fw.op("dve", lambda e: e.tensor_tensor(out=svn[:, :], in0=gs[:, :], in1=lnb_bc[:, :], op=OP.add), [gs, lnb_bc], [svn])
            for gr in range(4):
                fw.op("pe", lambda e: e.matmul(bank(0)[:, gr * 128:(gr + 1) * 128], lhsT=wsT[:, gr, :], rhs=svn[:, gr * 128:(gr + 1) * 128], start=True, stop=True),
                      [wsT, svn], [PR[0]])
            for gr in range(4):
                fw.op("dve", lambda e: e.scalar_tensor_tensor(out=ybt[:, gr * 128:(gr + 1) * 128], in0=bank(0)[:, gr * 128:(gr + 1) * 128], scalar=bs_col[:, gr:gr + 1], in1=gu[:, gr * 128:(gr + 1) * 128], op0=OP.add, op1=OP.mult),
                      [PR[0], bs_col, gu], [ybt])
            for ch in range(4):
                fw.op("pe", lambda e: e.transpose(out=bank_bf(1)[:, ch * 128:(ch + 1) * 128], in_=ybt[:, ch * 128:(ch + 1) * 128], identity=identb[:, :]), [ybt, identb], [PR[1]])
            fw.op("act", lambda e: e.activation(out=ybT[:, :, bs_], in_=bank_bf(1)[:, 0:512].rearrange("p (c t) -> p c t", c=4), func=AF.Copy), [PR[1]], [ybT])
        wa, wa3 = load_w(wbab_v, 4)
        wb, wb3 = load_w(wbbb_v, 4)
        yaG = yaGs[p]
        fw.dma("sp", lambda e: e.dma_start(out=yaG[:, :, :], in_=yaTd[:, :, g * 256:(g + 1) * 256].rearrange("c p t -> p c t")), yaregs, [yaG])
        for dc in range(8):
            ba, bb = 0, 1
            for cc in range(4):
                fw.op("pe", lambda e: e.matmul(bank(ba)[:, 0:256], lhsT=wa3[:, cc, dc * 128:(dc + 1) * 128], rhs=yaG[:, cc, :], start=(cc == 0), stop=(cc == 3)),
                      [wa, yaG], [PR[ba]])
            for cc in range(4):
                fw.op("pe", lambda e: e.matmul(bank(bb)[:, 0:256], lhsT=wb3[:, cc, dc * 128:(dc + 1) * 128], rhs=ybT[:, cc, :], start=(cc == 0), stop=(cc == 3)),
                      [wb, ybT], [PR[bb]])
            fw.op("dve", lambda e: e.tensor_tensor(out=t5a[:, :], in0=bank(ba)[:, 0:256], in1=gTC[:, dc, :], op=OP.mult), [PR[ba], gTC], [t5a])
            fw.op("dve", lambda e: e.tensor_tensor(out=t5b[:, :], in0=bank(bb)[:, 0:256], in1=gTC[:, 8 + dc, :], op=OP.mult), [PR[bb], gTC], [t5b])
            fw.op("dve", lambda e: e.tensor_tensor(out=mT[:, dc, :], in0=t5a[:, :], in1=t5b[:, :], op=OP.add), [t5a, t5b], [mT])
        wo0, wo03 = load_w(woutb_v[:, :, 0:512], 8)
        wo1, wo13 = load_w(woutb_v[:, :, 512:1024], 8)
        for bl in range(2):
            bs_ = slice(bl * 128, (bl + 1) * 128)
            for half, (wo, wo3) in enumerate(((wo0, wo03), (wo1, wo13))):
                for c in range(8):
                    fw.op("pe", lambda e: e.matmul(bank(half), lhsT=mT[:, c, bs_], rhs=wo3[:, c, :], start=(c == 0), stop=(c == 7)), [wo, mT], [PR[half]])
            fw.op("dve", lambda e: e.tensor_tensor(out=tmpfC[:, :], in0=pair(0), in1=mod_bc[:, 2 * D:3 * D], op=OP.mult), [PR[0], PR[1], mod_bc], [tmpfC])
            fw.op("dve", lambda e: e.tensor_tensor(out=xg[bl][:, :], in0=xg[bl][:, :], in1=tmpfC[:, :], op=OP.add), [xg[bl], tmpfC], [xg[bl]])
        if debug is not None and debug[0] == "x2":
            dbg_out(xg[0][:, :], [xg[0]], dbg_d[:, 0:D])
            dbg_out(xg[1][:, :], [xg[1]], dbg_d[:, D:2 * D])
            return
        for bl in range(2):
            ssb = ss_r[ss_i[0] % 4]
            ss_i[0] += 1
            fw.op("act", lambda e: e.activation(out=xsC[:, :], in_=xg[bl][:, :], func=AF.Square, accum_out=ssb[:, 0:1]), [xg[bl]], [xsC, ssb])
            rstd_of(ssb, 0, D, 1)
            fw.op("dve", lambda e: e.scalar_tensor_tensor(out=tmpfC[:, :], in0=xg[bl][:, :], scalar=ssb[:, 1:2], in1=mod_bc[:, 4 * D:5 * D], op0=OP.mult, op1=OP.mult), [xg[bl], ssb, mod_bc], [tmpfC])
            fw.op("dve", lambda e: e.tensor_tensor(out=h2g[bl][:, :], in0=tmpfC[:, :], in1=mod_bc[:, 3 * D:4 * D], op=OP.add), [tmpfC, mod_bc], [h2g[bl]])
            for c in range(8):
                fw.op("pe", lambda e: e.transpose(out=bank_bf(bl)[:, c * 128:(c + 1) * 128], in_=h2g[bl][:, c * 128:(c + 1) * 128], identity=identb[:, :]), [h2g[bl], identb], [PR[bl]])
            fw.op("act", lambda e: e.activation(out=hTC[:, :, bl * 128:(bl + 1) * 128], in_=bank_bf(bl)[:, 0:1024].rearrange("p (c t) -> p c t", c=8), func=AF.Copy), [PR[bl]], [hTC])
        for u in range(4):
            w, w3 = load_w(wpqb_v[:, :, u * 512:(u + 1) * 512], 8)
            for cc in range(4):
                l = u * 4 + cc
                pb = l % 2
                for c in range(8):
                    fw.op("pe", lambda e: e.matmul(bank(pb)[:, 0:256], lhsT=w3[:, c, cc * 128:(cc + 1) * 128], rhs=hTC[:, c, :], start=(c == 0), stop=(c == 7)), [w, hTC], [PR[pb]])
                fw.op("act", lambda e: e.activation(out=gTC[:, l, :], in_=bank(pb)[:, 0:256], func=AF.Copy), [PR[pb]], [gTC])
        for bl in range(2):
            eT = eTs[p][bl]
            gT_ = gTs_[p][bl]
            bs_ = slice(bl * 128, (bl + 1) * 128)
            for rnd in range(2):
                for l8 in range(8):
                    l = rnd * 8 + l8
                    fw.op("pe", lambda e: e.matmul(bank(l8 // 4)[:, (l8 % 4) * 128:(l8 % 4 + 1) * 128], lhsT=gTC[:, l, bs_], rhs=skT[:, l, :], start=True, stop=True), [gTC, skT], [PR[l8 // 4]])
                if rnd == 0:
                    fw.op("act", lambda e: e.activation(out=scf[:, 0:1024], in_=pair(0), func=AF.Copy), [PR[0], PR[1]], [scf])
                else:
                    fw.op("act", lambda e: e.activation(out=scf[:, 1024:2048], in_=pair(0), func=AF.Copy), [PR[0], PR[1]], [scf])
            r_src, r_wrk, r_vals, r_idx = scf, scw, tv, ti
            top16(scf[:, :].rearrange("p (l n) -> p l n", l=16), scw[:, :].rearrange("p (l n) -> p l n", l=16),
                  tv[:, :].rearrange("p (l k) -> p l k", l=16), ti[:, :].rearrange("p (l k) -> p l k", l=16), 16, (scf, scw, tv, ti))
            fw.op("dve", lambda e: e.tensor_copy(out=tif[:, :], in_=ti[:, :]), [ti], [tif])
            tv4 = tv[:, :].rearrange("p (h i k) -> p h i k", h=8, i=2)
            tif4 = tif[:, :].rearrange("p (h i k) -> p h i k", h=8, i=2)
            cand4 = scf[:, :].rearrange("p (h a b) -> p h a b", h=8, a=16)
            fw.op("dve", lambda e: e.tensor_tensor(out=cand4, in0=tv4[:, :, 0, :].unsqueeze(3).to_broadcast([128, 8, 16, 16]), in1=tv4[:, :, 1, :].unsqueeze(2).to_broadcast([128, 8, 16, 16]), op=OP.add), [tv], [scf])
            top16(scf[:, :].rearrange("p (h n) -> p h n", h=8), scw[:, :].rearrange("p (h n) -> p h n", h=8),
                  cv[:, :].rearrange("p (h k) -> p h k", h=8), ci[:, :].rearrange("p (h k) -> p h k", h=8), 8, (scf, scw, cv, ci))
            cv3 = cv[:, :].rearrange("p (h k) -> p h k", h=8)
            gsm3 = gsm[:, :].rearrange("p (h k) -> p h k", h=8)
            fw.op("dve", lambda e: e.tensor_tensor(out=gsm3, in0=cv3, in1=cv3[:, :, 0:1].to_broadcast([128, 8, 16]), op=OP.subtract), [cv], [gsm])
            fw.op("act", lambda e: e.activation(out=gsm[:, :], in_=gsm[:, :], func=AF.Exp), [gsm], [gsm])
            fw.op("dve", lambda e: e.tensor_reduce(out=sm8[:, :], in_=gsm3, axis=AX.X, op=OP.add), [gsm], [sm8])
            fw.op("dve", lambda e: e.reciprocal(out=sm8[:, :], in_=sm8[:, :]), [sm8], [sm8])
            fw.op("dve", lambda e: e.tensor_tensor(out=gsm3, in0=gsm3, in1=sm8[:, :].unsqueeze(2).to_broadcast([128, 8, 16]), op=OP.mult), [gsm, sm8], [gsm])
            fw.op("dve", lambda e: e.tensor_copy(out=cif[:, :], in_=ci[:, :]), [ci], [cif])
            oh4 = scw[:, :].rearrange("p (h k a) -> p h k a", h=8, k=16)
            cif3 = cif[:, :].rearrange("p (h k) -> p h k", h=8)
            ak3 = ak[:, :].rearrange("p (h k) -> p h k", h=8)
            bk3 = bk[:, :].rearrange("p (h k) -> p h k", h=8)
            thr_b = thr16[:, :].unsqueeze(1).unsqueeze(1).to_broadcast([128, 8, 16, 16])
            io_b = io16[:, :].unsqueeze(1).unsqueeze(1).to_broadcast([128, 8, 16, 16])
            fw.op("dve", lambda e: e.tensor_tensor(out=oh4, in0=cif3.unsqueeze(3).to_broadcast([128, 8, 16, 16]), in1=thr_b, op=OP.is_ge), [cif, thr16], [scw])
            fw.op("dve", lambda e: e.tensor_reduce(out=ak3, in_=oh4, axis=AX.X, op=OP.add), [scw], [ak])
            fw.op("dve", lambda e: e.tensor_scalar(out=ak[:, :], in0=ak[:, :], scalar1=-1.0, scalar2=None, op0=OP.add), [ak], [ak])
            fw.op("dve", lambda e: e.scalar_tensor_tensor(out=bk[:, :], in0=ak[:, :], scalar=-16.0, in1=cif[:, :], op0=OP.mult, op1=OP.add), [ak, cif], [bk])
            for (kk3, pidx, dst) in ((ak3, 0, i1s), (bk3, 1, i2s)):
                fw.op("dve", lambda e: e.tensor_tensor(out=oh4, in0=kk3.unsqueeze(3).to_broadcast([128, 8, 16, 16]), in1=io_b, op=OP.is_equal), [ak, bk, io16], [scw])
                fw.op("dve", lambda e: e.tensor_tensor(out=oh4, in0=oh4, in1=tif4[:, :, pidx, :].unsqueeze(2).to_broadcast([128, 8, 16, 16]), op=OP.mult), [scw, tif], [scw])
                fw.op("dve", lambda e: e.tensor_reduce(out=dst[:, :].rearrange("p (h k) -> p h k", h=8), in_=oh4, axis=AX.X, op=OP.add), [scw], [dst])
            fw.op("dve", lambda e: e.scalar_tensor_tensor(out=eidxf[:, :], in0=i1s[:, :], scalar=128.0, in1=i2s[:, :], op0=OP.mult, op1=OP.add), [i1s, i2s], [eidxf])
            fw.op("pe", lambda e: e.transpose(out=bank(0)[:, 0:128], in_=eidxf[:, :], identity=identf[:, :]), [eidxf, identf], [PR[0]])
            fw.op("dve", lambda e: e.tensor_copy(out=eT[:, :], in_=bank(0)[:, 0:128]), [PR[0]], [eT])
            fw.op("pe", lambda e: e.transpose(out=bank(1)[:, 0:128], in_=gsm[:, :], identity=identf[:, :]), [gsm, identf], [PR[1]])
            fw.op("act", lambda e: e.activation(out=gT_[:, :], in_=bank(1)[:, 0:128], func=AF.Copy), [PR[1]], [gT_])

    def loops(g):
        p = g % 2
        xg = xgs[p]
        h2g = h2gs[p]
        for bl in range(2):
            j = 2 * g + bl
            eT = eTs[p][bl]
            gT_ = gTs_[p][bl]
            gbs = {}

            def s_gather(t):
                gbs[t] = gather(pdu, eT[:, t:t + 1], [eT] + tregs)

            def s_bcast(t):
                hbp = 2 + (t % 2)
                for half in range(2):
                    fw.op("pe", lambda e: e.matmul(bank(2 * hbp + half), lhsT=identb[:, t:t + 1].to_broadcast([128, 128]), rhs=h2g[bl][:, half * 512:(half + 1) * 512], start=True, stop=True),
                          [identb, h2g[bl]], [PR[2 * hbp + half]])

            def s_dot(t):
                hbp = 2 + (t % 2)
                ac = acols[t % 8]
                cc_ = ccols[t % 8]
                gb_ = gbs[t]
                fw.op("dve", lambda e: e.scalar_tensor_tensor(out=junkL[:, :], in0=gb_[:, 0:D], scalar=1.0, in1=pair(hbp), op0=OP.mult, op1=OP.mult, accum_out=ac[:, 0:1]),
                      [gb_, PR[2 * hbp], PR[2 * hbp + 1]], [junkL, ac])
                fw.op("act", lambda e: e.activation(out=cc_[:, 0:1], in_=ac[:, 0:1], func=AF.Gelu_apprx_tanh), [ac], [cc_])

            def s_up(t):
                cc_ = ccols[t % 8]
                zt = Zts[t % 4]
                gb_ = gbs.pop(t)
                fw.op("dve", lambda e: e.tensor_scalar(out=zt[:, :], in0=Wc[:, 127 - t:255 - t], scalar1=cc_[:, 0:1], scalar2=gT_[:, t:t + 1], op0=OP.mult, op1=OP.mult),
                      [Wc, cc_, gT_], [zt])
                for half in range(2):
                    fw.op("pe", lambda e: e.matmul(bank(2 + half), lhsT=zt[:, :], rhs=gb_[:, D + half * 512:D + (half + 1) * 512], start=(t == 0), stop=(t == 127)),
                          [zt, gb_], [PR[2 + half]])

            AH = NGB - 2
            for t in range(min(AH, 128)):
                s_gather(t)
            s_bcast(0)
            for t in range(128):
                if t + AH < 128:
                    s_gather(t + AH)
                if t + 1 < 128:
                    s_bcast(t + 1)
                s_dot(t)
                if t >= 1:
                    s_up(t - 1)
            s_up(127)
            fw.op("dve", lambda e: e.tensor_tensor(out=tmpfL[:, :], in0=pair(1), in1=mod_bc[:, 5 * D:6 * D], op=OP.mult), [PR[2], PR[3], mod_bc], [tmpfL])
            fw.op("dve", lambda e: e.tensor_tensor(out=xg[bl][:, :], in0=xg[bl][:, :], in1=tmpfL[:, :], op=OP.add), [xg[bl], tmpfL], [xg[bl]])
            ssb = ss_r[ss_i[0] % 4]
            ss_i[0] += 1
            fw.op("act", lambda e: e.activation(out=junkL[:, :], in_=xg[bl][:, :], func=AF.Square, accum_out=ssb[:, 0:1]), [xg[bl]], [junkL, ssb])
            rstd_of(ssb, 0, D, 1)
            fw.op("dve", lambda e: e.scalar_tensor_tensor(out=tmpfL[:, :], in0=xg[bl][:, :], scalar=ssb[:, 1:2], in1=fg_bc[:, :], op0=OP.mult, op1=OP.mult), [xg[bl], ssb, fg_bc], [tmpfL])
            fw.dma("act", lambda e: e.dma_start(out=out_d[j * 128:(j + 1) * 128, :], in_=tmpfL[:, :]), [tmpfL], [r_none])

    cmain(0)
    if debug is not None and debug[0] == "x2":
        fw.barrier()
        return nc
    for g in range(NGC):
        if g + 1 < NGC:
            interleave(fw, [lambda: loops(g), lambda: cmain(g + 1)], [2, 1])
        else:
            loops(g)
    fw.barrier()
    return nc


def _prep_inputs(inputs):
    g = {k: np.asarray(v) for k, v in inputs.items()}
    x = np.ascontiguousarray(g["x"], dtype=np.float32)
    shared = {
        "w_ada": g["w_ada"][0], "b_ada": g["b_ada"], "norm1_g": g["norm1_g"], "w_in": g["w_in"][0],
        "da_lambda_q1": g["da_lambda_q1"], "da_lambda_k1": g["da_lambda_k1"],
        "da_lambda_q2": g["da_lambda_q2"], "da_lambda_k2": g["da_lambda_k2"],
        "da_head_g": g["da_head_g"], "sg_ln_g": g["sg_ln_g"], "sg_ln_b": g["sg_ln_b"],
        "sg_w": g["sg_w"][0], "sg_b": g["sg_b"][0], "w_branch_a": g["w_branch_a"][0],
        "w_branch_b": g["w_branch_b"][0], "w_out": g["w_out"][0], "norm2_g": g["norm2_g"],
        "peer_w_query": g["peer_w_query"][0], "peer_sub_keys": g["peer_sub_keys"][0].reshape(16, 128, 128),
        "peer_down": g["peer_down"][0], "peer_up": g["peer_up"][0], "final_g": g["final_g"].reshape(1, D),
    }
    shared = {k: np.ascontiguousarray(v, dtype=np.float32) for k, v in shared.items()}
    kk = np.arange(128)[:, None] // 64
    qq = np.arange(128)[None, :] // 64
    diag = (kk <= qq).astype(np.float32)
    in_maps = []
    for core in range(8):
        b, par = core // 2, core % 2
        xb = x[b]
        xo = np.ascontiguousarray(xb.reshape(NB, 128, D)[par::2].reshape(NOWN * 128, D))
        if par == 0:
            am = np.concatenate([diag, np.zeros((128, 128), np.float32)], axis=1)
        else:
            am = np.concatenate([np.ones((128, 128), np.float32), diag], axis=1)
        m = dict(shared)
        m["x_b"] = xb
        m["x_own"] = xo
        m["c_b"] = np.ascontiguousarray(g["c"][b:b + 1], dtype=np.float32)
        m["amask"] = np.ascontiguousarray(am)
        in_maps.append(m)
    return in_maps


def kernel(**inputs):
    in_maps = _prep_inputs(inputs)
    nc = build()
    res = run_bass_kernel_spmd(nc, in_maps, core_ids=list(range(8)))
    out = np.zeros((4, S, D), np.float32)
    for core in range(8):
        b, par = core // 2, core % 2
        o = np.asarray(res.results[core]["out"]).reshape(NOWN, 128, D)
        out[b].reshape(NB, 128, D)[par::2] = o
    return out
```

```python
import numpy as np
from contextlib import ExitStack
import concourse.bass as bass
import concourse.mybir as mybir
from concourse.bass_utils import run_bass_kernel_spmd

F32 = mybir.dt.float32
BF16 = mybir.dt.bfloat16
I32 = mybir.dt.int32
U32 = mybir.dt.uint32
AF = mybir.ActivationFunctionType
OP = mybir.AluOpType
AX = mybir.AxisListType

D = 1024
S = 8192
NB = 64
NOWN = 32
EPS = 1e-6
LAM_INIT = 0.8 - 0.6 * 1.0
NEG = -1.0e30
SEM_LIMIT = 30000


class Reg:
    def __init__(self):
        self.w = {}
        self.r = {}


class Buf(Reg):
    def __init__(self, t):
        Reg.__init__(self)
        self.t = t

    def __getitem__(self, k):
        return self.t[k]


class FW:
    def __init__(self, nc):
        self.nc = nc
        self.engs = dict(pe=nc.tensor, act=nc.scalar, dve=nc.vector, pool=nc.gpsimd, sp=nc.sync)
        self.cur = {}
        self.nsem = 0
        self.known = {k: {} for k in self.engs}
        for k in self.engs:
            self._newsem(k)
        self.dsem = {}
        for q, n in (("sp", 12), ("act", 4), ("pool", 12)):
            self.dsem[q] = [[f"d_{q}{i}", nc.alloc_semaphore(f"d_{q}{i}"), 0] for i in range(n)]
        self.drr = {q: 0 for q in self.dsem}
        self.ninst = 0
        self.hook = None

    def _newsem(self, k):
        name = f"e_{k}{self.nsem}"
        self.cur[k] = [name, self.nc.alloc_semaphore(name), 0]
        self.nsem += 1

    def _deps(self, reads, writes):
        deps = {}

        def add(d):
            for name, (s, v) in d.items():
                if name not in deps or deps[name][1] < v:
                    deps[name] = (s, v)
        for b in reads:
            add(b.w)
        for b in writes:
            add(b.w)
            add(b.r)
        return deps

    def _wait(self, k, deps):
        eng = self.engs[k]
        for name, (s, v) in deps.items():
            if k == "pe" and name.startswith("e_pe"):
                continue
            if self.known[k].get(name, 0) >= v:
                continue
            eng.wait_ge(s, v)
            self.known[k][name] = v
            self.ninst += 1

    def _record(self, ev, reads, writes):
        name, s, v = ev
        for b in reads:
            b.r[name] = (s, v)
        for b in writes:
            b.w = {name: (s, v)}
            b.r = {}

    def op(self, k, fn, reads, writes):
        self._wait(k, self._deps(reads, writes))
        ins = fn(self.engs[k])
        c = self.cur[k]
        if c[2] >= SEM_LIMIT:
            self._newsem(k)
            c = self.cur[k]
        c[2] += 1
        ins.then_inc(c[1], 1)
        self.ninst += 1
        self._record((c[0], c[1], c[2]), reads, writes)
        if self.hook is not None:
            self.hook()

    def dma(self, q, fn, reads, writes, slot=None, pre=()):
        if slot is None:
            slots = self.dsem[q]
            i = self.drr[q]
            self.drr[q] = (i + 1) % len(slots)
            sl = slots[i]
        else:
            sl = slot
        deps = self._deps(reads, list(writes) + list(pre))
        if slot is None and sl[2] > 0:
            deps[sl[0]] = (sl[1], sl[2])
        self._wait(q, deps)
        ins = fn(self.engs[q])
        sl[2] += 16
        ins.then_inc(sl[1], 16)
        self.ninst += 1
        self._record((sl[0], sl[1], sl[2]), reads, writes)
        self.last_written = writes[0] if writes else None
        if self.hook is not None:
            self.hook()

    def barrier(self):
        evs = {}
        for k, c in self.cur.items():
            if c[2] > 0:
                evs[c[0]] = (c[1], c[2])
        for q, slots in self.dsem.items():
            for sl in slots:
                if sl[2] > 0:
                    evs[sl[0]] = (sl[1], sl[2])
        for k in self.engs:
            self._wait(k, dict(evs))


def interleave(fw, fns, weights):
    import threading
    n = len(fns)
    sems = [threading.Semaphore(0) for _ in fns]
    done = [False] * n
    st = {"cur": 0, "cnt": 0}
    errs = []
    fin = threading.Semaphore(0)

    def nxt(i):
        for k in range(1, n + 1):
            j = (i + k) % n
            if not done[j]:
                return j
        return None

    def hook():
        i = st["cur"]
        st["cnt"] += 1
        if st["cnt"] >= weights[i]:
            st["cnt"] = 0
            j = nxt(i)
            if j is not None and j != i:
                st["cur"] = j
                sems[j].release()
                sems[i].acquire()

    def runner(i):
        sems[i].acquire()
        try:
            fns[i]()
        except BaseException as e:
            errs.append(e)
        done[i] = True
        j = nxt(i)
        if j is not None:
            st["cur"] = j
            st["cnt"] = 0
            sems[j].release()
        else:
            fin.release()

    ths = [threading.Thread(target=runner, args=(i,)) for i in range(n)]
    old = fw.hook
    fw.hook = hook
    for t in ths:
        t.start()
    st["cur"] = 0
    sems[0].release()
    fin.acquire()
    for t in ths:
        t.join()
    fw.hook = old
    if errs:
        raise errs[0]


class ViewBuf(Reg):
    def __init__(self, ap):
        Reg.__init__(self)
        self.ap_ = ap

    def __getitem__(self, k):
        return self.ap_[k]


def build(debug=None):
    nc = bass.Bass("TRN2", target_bir_lowering=False)
    fw = FW(nc)

    def din(name, shape, dt=F32):
        return nc.dram_tensor(name, list(shape), dt, kind="ExternalInput").ap()

    x_b = din("x_b", [S, D])
    x_own = din("x_own", [NOWN * 128, D])
    c_b = din("c_b", [1, D])
    w_ada = din("w_ada", [D, 6 * D])
    b_ada = din("b_ada", [1, 6 * D])
    norm1_g = din("norm1_g", [1, D])
    w_in = din("w_in", [D, 4608])
    lq1 = din("da_lambda_q1", [1, 64])
    lk1 = din("da_lambda_k1", [1, 64])
    lq2 = din("da_lambda_q2", [1, 64])
    lk2 = din("da_lambda_k2", [1, 64])
    head_g = din("da_head_g", [1, 128])
    ln_g = din("sg_ln_g", [1, 512])
    ln_b = din("sg_ln_b", [1, 512])
    sg_w = din("sg_w", [4, 128, 128])
    sg_b = din("sg_b", [4, 128])
    w_ba = din("w_branch_a", [512, D])
    w_bb = din("w_branch_b", [512, D])
    w_out = din("w_out", [D, D])
    norm2_g = din("norm2_g", [1, D])
    w_pq = din("peer_w_query", [D, 2048])
    sub_keys = din("peer_sub_keys", [16, 128, 128])
    p_down = din("peer_down", [16384, D])
    p_up = din("peer_up", [16384, D])
    final_g = din("final_g", [1, D])
    amask = din("amask", [128, 256])
    out_d = nc.dram_tensor("out", [NOWN * 128, D], F32, kind="ExternalOutput").ap()
    dbg_d = None
    if debug is not None:
        dbg_d = nc.dram_tensor("dbg", list(debug[1]), F32, kind="ExternalOutput").ap()

    winb = nc.dram_tensor("winb", [D, 4608], BF16, kind="Internal").ap()
    wbab = nc.dram_tensor("wbab", [512, D], BF16, kind="Internal").ap()
    wbbb = nc.dram_tensor("wbbb", [512, D], BF16, kind="Internal").ap()
    woutb = nc.dram_tensor("woutb", [D, D], BF16, kind="Internal").ap()
    wpqb = nc.dram_tensor("wpqb", [D, 2048], BF16, kind="Internal").ap()
    modscr = nc.dram_tensor("modscr", [1, 2048], F32, kind="Internal").ap()
    pdu = nc.dram_tensor("pdu", [16384, 2 * D], BF16, kind="Internal").ap()
    tregs = []
    tconv = [(pdu[:, hf * D:(hf + 1) * D], src, r0) for hf, src in enumerate((p_down, p_up)) for r0 in range(0, 16384, 1024)]
    wregs = []
    r_modscr = Reg()
    r_none = Reg()
    yaregs = []

    def sb(name, shape, dt=F32):
        return Buf(nc.alloc_sbuf_tensor(name, list(shape), dt))

    P2 = [nc.alloc_psum_tensor(f"ps{i}", [128, 1024], F32) for i in range(4)]
    PR = [Reg() for _ in range(8)]

    def bank(i):
        return P2[i // 2][:, (i % 2) * 512:(i % 2 + 1) * 512]

    def bank_bf(i):
        return P2[i // 2][:, :].bitcast(BF16)[:, (i % 2) * 1024:(i % 2 + 1) * 1024]

    def pair(i):
        return P2[i][:, :]

    identf = sb("identf", [128, 128])
    identb = sb("identb", [128, 128], BF16)
    mod_bc = sb("mod_bc", [128, 6 * D])
    fg_bc = sb("fg_bc", [128, D])
    yaTd = nc.dram_tensor("yaTd", [4, 128, NOWN * 128], BF16, kind="Internal").ap()
    r_yaT = [Reg() for _ in range(8)]
    ysts = [sb("yst0", [128, 128], BF16), sb("yst1", [128, 128], BF16)]
    ysi = [0]
    A1col = sb("A1col", [128, 8])
    B1col = sb("B1col", [128, 8])
    neglam = sb("neglam", [128, 1])
    hg_bc = sb("hg_bc", [128, 128])
    lng_bc = sb("lng_bc", [128, 512])
    lnb_bc = sb("lnb_bc", [128, 512])
    bs_col = sb("bs_col", [128, 4])
    maskb = sb("maskb", [128, 256], BF16)
    wsT = sb("wsT", [128, 4, 128], BF16)
    skT = sb("skT", [128, 16, 128], BF16)
    ss_r = [sb(f"ss{i}", [128, 4]) for i in range(4)]
    ss_i = [0]

    def dbg_out(buf_ap, reads, dst=None):
        fw.dma("sp", lambda e: e.dma_start(out=(dbg_d if dst is None else dst), in_=buf_ap), reads, [r_none])

    fw.op("pool", lambda e: e.memset(identf[:, :], 1.0), [], [identf])
    fw.op("pool", lambda e: e.affine_select(out=identf[:, :], in_=identf[:, :], pattern=[[-1, 128]],
                                            compare_op=OP.is_equal, fill=0.0, base=0, channel_multiplier=1),
          [identf], [identf])
    fw.op("dve", lambda e: e.tensor_copy(out=identb[:, :], in_=identf[:, :]), [identf], [identb])

    for (dst, src, ncol) in ((winb, w_in, 4608), (wbab, w_ba, D), (wbbb, w_bb, D), (woutb, w_out, D), (wpqb, w_pq, 2048)):
        for c0 in range(0, ncol, 512):
            fw.dma("pool", lambda e, dst=dst, src=src, c0=c0: e.dma_start(out=dst[:, c0:c0 + 512], in_=src[:, c0:c0 + 512]),
                   [], [Reg()])
            wregs.append(fw.last_written)

    with nc.allow_non_contiguous_dma(reason="tiny param loads"):
        n1g_col = sb("n1g_col", [128, 8])
        c_col = sb("c_col", [128, 8])
        fw.dma("sp", lambda e: e.dma_start(out=n1g_col[:, :], in_=norm1_g[0, :].rearrange("(c p) -> p c", p=128)), [], [n1g_col])
        fw.dma("sp", lambda e: e.dma_start(out=c_col[:, :], in_=c_b[0, :].rearrange("(c p) -> p c", p=128)), [], [c_col])
        fw.dma("sp", lambda e: e.dma_start(out=bs_col[:, :], in_=sg_b.rearrange("g p -> p g")), [], [bs_col])
    fw.dma("sp", lambda e: e.dma_start(out=mod_bc[:, :], in_=b_ada[0:1, :].partition_broadcast(128)), [], [mod_bc])
    fw.dma("sp", lambda e: e.dma_start(out=fg_bc[:, :], in_=final_g[0:1, :].partition_broadcast(128)), [], [fg_bc])
    fw.dma("sp", lambda e: e.dma_start(out=hg_bc[:, :], in_=head_g[0:1, :].partition_broadcast(128)), [], [hg_bc])
    fw.dma("sp", lambda e: e.dma_start(out=lng_bc[:, :], in_=ln_g[0:1, :].partition_broadcast(128)), [], [lng_bc])
    fw.dma("sp", lambda e: e.dma_start(out=lnb_bc[:, :], in_=ln_b[0:1, :].partition_broadcast(128)), [], [lnb_bc])
    fw.op("dve", lambda e: e.tensor_scalar(out=hg_bc[:, :], in0=hg_bc[:, :], scalar1=1.0 - LAM_INIT, scalar2=None, op0=OP.mult),
          [hg_bc], [hg_bc])

    with ExitStack() as es:
        def tl(name, shape, dt=F32):
            return es.enter_context(nc.sbuf_tensor(name, list(shape), dt))
        wa0_t = tl("s0a", [128, 8, 512]); wa1_t = tl("s0b", [128, 8, 512]); csbc_t = tl("s0c", [128, 8, 128])
        lam_t = tl("s0d", [128, 4, 64]); n2g_t = tl("s0e", [128, 1024]); mcol_t = tl("s0f", [128, 16])
        tmp0_t = tl("s0g", [128, 512]); lsc_t = tl("s0h", [128, 8])
        wab = [Buf(wa0_t), Buf(wa1_t)]
        csbc = Buf(csbc_t)
        lam4 = Buf(lam_t)
        n2g = Buf(n2g_t)
        mcol = Buf(mcol_t)
        tmp0 = Buf(tmp0_t)
        lsc = Buf(lsc_t)
        for i, src in enumerate((lq1, lk1, lq2, lk2)):
            fw.dma("sp", lambda e, i=i, src=src: e.dma_start(out=lam4[:, i, :], in_=src[0:1, :].partition_broadcast(128)), [], [lam4])
        fw.op("dve", lambda e: e.tensor_tensor(out=lam4[:, 0, :], in0=lam4[:, 0, :], in1=lam4[:, 1, :], op=OP.mult), [lam4], [lam4])
        fw.op("dve", lambda e: e.tensor_tensor(out=lam4[:, 2, :], in0=lam4[:, 2, :], in1=lam4[:, 3, :], op=OP.mult), [lam4], [lam4])
        fw.op("dve", lambda e: e.tensor_reduce(out=lsc[:, 0:1], in_=lam4[:, 0, :], axis=AX.X, op=OP.add), [lam4], [lsc])
        fw.op("dve", lambda e: e.tensor_reduce(out=lsc[:, 1:2], in_=lam4[:, 2, :], axis=AX.X, op=OP.add), [lam4], [lsc])
        fw.op("act", lambda e: e.activation(out=lsc[:, 2:4], in_=lsc[:, 0:2], func=AF.Exp), [lsc], [lsc])
        fw.op("dve", lambda e: e.tensor_tensor(out=lsc[:, 4:5], in0=lsc[:, 3:4], in1=lsc[:, 2:3], op=OP.subtract), [lsc], [lsc])
        fw.op("dve", lambda e: e.tensor_scalar(out=neglam[:, :], in0=lsc[:, 4:5], scalar1=-LAM_INIT, scalar2=None, op0=OP.add),
              [lsc], [neglam])
        fw.dma("sp", lambda e: e.dma_start(out=tmp0[:, 0:256], in_=amask[:, :]), [], [tmp0])
        fw.op("dve", lambda e: e.tensor_copy(out=maskb[:, :], in_=tmp0[:, 0:256]), [tmp0], [maskb])
        for g in range(4):
            fw.dma("sp", lambda e, g=g: e.dma_start(out=tmp0[:, 0:128], in_=sg_w[g, :, :]), [], [tmp0])
            fw.op("dve", lambda e: e.memset(tmp0[0:64, 64:128], 0.0), [tmp0], [tmp0])
            fw.op("pe", lambda e: e.transpose(out=bank(0)[:, 0:128], in_=tmp0[:, 0:128], identity=identf[:, :]), [tmp0, identf], [PR[0]])
            fw.op("act", lambda e, g=g: e.activation(out=wsT[:, g, :], in_=bank(0)[:, 0:128], func=AF.Copy), [PR[0]], [wsT])
        for l in range(16):
            fw.dma("sp", lambda e, l=l: e.dma_start(out=tmp0[:, 0:128], in_=sub_keys[l, :, :]), [], [tmp0])
            fw.op("pe", lambda e: e.transpose(out=bank(1)[:, 0:128], in_=tmp0[:, 0:128], identity=identf[:, :]), [tmp0, identf], [PR[1]])
            fw.op("act", lambda e, l=l: e.activation(out=skT[:, l, :], in_=bank(1)[:, 0:128], func=AF.Copy), [PR[1]], [skT])
        fw.op("act", lambda e: e.activation(out=c_col[:, :], in_=c_col[:, :], func=AF.Silu), [c_col], [c_col])
        fw.op("dve", lambda e: e.tensor_copy(out=csbc[:, :, :], in_=c_col[:, :].unsqueeze(2).to_broadcast([128, 8, 128])), [c_col], [csbc])
        w_ada_v = w_ada.rearrange("(c p) n -> p c n", p=128)
        for ec in range(12):
            wb_ = wab[ec % 2]
            fw.dma("sp" if ec % 2 == 0 else "act", lambda e, ec=ec, wb_=wb_: e.dma_start(out=wb_[:, :, :], in_=w_ada_v[:, :, ec * 512:(ec + 1) * 512]), [], [wb_])
            pb = 2 + (ec % 2)
            for c in range(8):
                fw.op("pe", lambda e, c=c, wb_=wb_, pb=pb: e.matmul(bank(pb), lhsT=csbc[:, c, :], rhs=wb_[:, c, :], start=(c == 0), stop=(c == 7)),
                      [csbc, wb_], [PR[pb]])
            fw.op("dve", lambda e, ec=ec, pb=pb: e.tensor_tensor(out=mod_bc[:, ec * 512:(ec + 1) * 512], in0=bank(pb), in1=mod_bc[:, ec * 512:(ec + 1) * 512], op=OP.add),
                  [PR[pb], mod_bc], [mod_bc])
        fw.dma("sp", lambda e: e.dma_start(out=modscr[0:1, :], in_=mod_bc[0:1, 0:2048]), [mod_bc], [r_modscr])
        with nc.allow_non_contiguous_dma(reason="tiny param loads"):
            fw.dma("sp", lambda e: e.dma_start(out=mcol[:, :], in_=modscr[0, :].rearrange("(c p) -> p c", p=128)), [r_modscr], [mcol])
        fw.op("dve", lambda e: e.tensor_copy(out=B1col[:, :], in_=mcol[:, 0:8]), [mcol], [B1col])
        fw.op("dve", lambda e: e.scalar_tensor_tensor(out=A1col[:, :], in0=mcol[:, 8:16], scalar=1.0, in1=n1g_col[:, :], op0=OP.add, op1=OP.mult),
              [mcol, n1g_col], [A1col])
        fw.dma("sp", lambda e: e.dma_start(out=n2g[:, :], in_=norm2_g[0:1, :].partition_broadcast(128)), [], [n2g])
        fw.op("dve", lambda e: e.scalar_tensor_tensor(out=mod_bc[:, 4 * D:5 * D], in0=mod_bc[:, 4 * D:5 * D], scalar=1.0, in1=n2g[:, :], op0=OP.add, op1=OP.mult),
              [mod_bc, n2g], [mod_bc])
        fw.barrier()
    if debug is not None and debug[0] == "mod":
        dbg_out(mod_bc[:, :], [mod_bc])
        fw.barrier()
        return nc

    A1bc = A1col[:, :].unsqueeze(2).to_broadcast([128, 8, 128])
    B1bc = B1col[:, :].unsqueeze(2).to_broadcast([128, 8, 128])

    def rstd_of(ssbuf, col, n, tmpcol):
        fw.op("dve", lambda e: e.tensor_scalar(out=ssbuf[:, tmpcol:tmpcol + 1], in0=ssbuf[:, col:col + 1], scalar1=1.0 / n, scalar2=EPS, op0=OP.mult, op1=OP.add),
              [ssbuf], [ssbuf])
        fw.op("act", lambda e: e.activation(out=ssbuf[:, tmpcol:tmpcol + 1], in_=ssbuf[:, tmpcol:tmpcol + 1], func=AF.Ln), [ssbuf], [ssbuf])
        fw.op("act", lambda e: e.activation(out=ssbuf[:, tmpcol:tmpcol + 1], in_=ssbuf[:, tmpcol:tmpcol + 1], func=AF.Exp, scale=-0.5), [ssbuf], [ssbuf])

    def norm1_block(src_rows, xin, xs, tmpf, hT, hcols, pb, add_eng="pool"):
        ssb = ss_r[ss_i[0] % 4]
        ss_i[0] += 1
        if src_rows is not None:
            fw.dma("sp", lambda e: e.dma_start(out=xin[:, :], in_=src_rows), [], [xin])
        fw.op("act", lambda e: e.activation(out=xs[:, :], in_=xin[:, :], func=AF.Square, accum_out=ssb[:, 0:1]), [xin], [xs, ssb])
        rstd_of(ssb, 0, D, 1)
        if add_eng == "act":
            fw.op("act", lambda e: e.activation(out=xs[:, :], in_=xin[:, :], func=AF.Copy, scale=ssb[:, 1:2]), [xin, ssb], [xs])
        else:
            fw.op("dve", lambda e: e.tensor_scalar(out=xs[:, :], in0=xin[:, :], scalar1=ssb[:, 1:2], scalar2=None, op0=OP.mult), [xin, ssb], [xs])
        tpv = bank_bf(pb)
        for c in range(8):
            fw.op("pe", lambda e, c=c: e.transpose(out=tpv[:, c * 128:(c + 1) * 128], in_=xs[:, c * 128:(c + 1) * 128], identity=identb[:, :]),
                  [xs, identb], [PR[pb]])
        if add_eng == "act":
            for c in range(8):
                fw.op("act", lambda e, c=c: e.activation(out=hT[:, c, hcols], in_=tpv[:, c * 128:(c + 1) * 128], func=AF.Identity, scale=A1col[:, c:c + 1], bias=B1col[:, c:c + 1]),
                      [PR[pb], A1col, B1col], [hT])
            return
        fw.op("dve", lambda e: e.tensor_tensor(out=tmpf[:, :].rearrange("p (c t) -> p c t", c=8), in0=tpv.rearrange("p (c t) -> p c t", c=8), in1=A1bc, op=OP.mult),
              [PR[pb], A1col], [tmpf])
        fw.op(add_eng, lambda e: e.tensor_tensor(out=hT[:, :, hcols], in0=tmpf[:, :].rearrange("p (c t) -> p c t", c=8), in1=B1bc, op=OP.add),
              [tmpf, B1col], [hT])

    def norm_p1(src_rows, xin, xs):
        ssb = ss_r[ss_i[0] % 4]
        ss_i[0] += 1
        fw.dma("sp", lambda e: e.dma_start(out=xin[:, :], in_=src_rows), [], [xin])
        fw.op("act", lambda e: e.activation(out=xs[:, :], in_=xin[:, :], func=AF.Square, accum_out=ssb[:, 0:1]), [xin], [xs, ssb])
        rstd_of(ssb, 0, D, 1)
        fw.op("dve", lambda e: e.tensor_scalar(out=xs[:, :], in0=xin[:, :], scalar1=ssb[:, 1:2], scalar2=None, op0=OP.mult), [xin, ssb], [xs])

    def norm_p2(xs, tmpf, hT, hcols, pb):
        tpv = bank_bf(pb)
        for c in range(8):
            fw.op("pe", lambda e, c=c: e.transpose(out=tpv[:, c * 128:(c + 1) * 128], in_=xs[:, c * 128:(c + 1) * 128], identity=identb[:, :]),
                  [xs, identb], [PR[pb]])
        fw.op("dve", lambda e: e.tensor_tensor(out=tmpf[:, :].rearrange("p (c t) -> p c t", c=8), in0=tpv.rearrange("p (c t) -> p c t", c=8), in1=A1bc, op=OP.mult),
              [PR[pb], A1col], [tmpf])
        fw.op("pool", lambda e: e.tensor_tensor(out=hT[:, :, hcols], in0=tmpf[:, :].rearrange("p (c t) -> p c t", c=8), in1=B1bc, op=OP.add),
              [tmpf, B1col], [hT])

    winb_v = winb.rearrange("(c p) n -> p c n", p=128)

    for hp in range(2):
        with ExitStack() as es:
            def tl(name, shape, dt=F32):
                return es.enter_context(nc.sbuf_tensor(f"{name}_p{hp}", list(shape), dt))
            KT_t = tl("KT", [128, 2, S], BF16); V_t = tl("VV", [128, NB, 2, 129], BF16)
            wk_t = tl("wk", [128, 8, 256], BF16); wv_t = tl("wv", [128, 8, 256], BF16); wq_t = tl("wq", [128, 8, 256], BF16)
            xin0_t = tl("xin0", [128, D]); xin1_t = tl("xin1", [128, D]); xin2_t = tl("xin2", [128, D])
            xs0_t = tl("xs0", [128, D], BF16); xs1_t = tl("xs1", [128, D], BF16)
            xsB = [Buf(tl(f"xsB{i}", [128, D], BF16)) for i in range(4)]
            tmpf0_t = tl("tmpf0", [128, D]); tmpf1_t = tl("tmpf1", [128, D])
            hT0_t = tl("hT0", [128, 8, 512], BF16); hT1_t = tl("hT1", [128, 8, 512], BF16)
            QT0_t = tl("QT0", [128, 2, 512], BF16); QT1_t = tl("QT1", [128, 2, 512], BF16)
            PT0_t = tl("PT0", [128, 512], BF16); PT1_t = tl("PT1", [128, 512], BF16); PT2_t = tl("PT2", [128, 512], BF16); PT3_t = tl("PT3", [128, 512], BF16)
            at0_t = tl("at0", [128, 128]); at1_t = tl("at1", [128, 128])
            ab0_t = tl("ab0", [128, 128], BF16); ab1_t = tl("ab1", [128, 128], BF16)
            asm_t = tl("asm", [128, 8])
            KT = KT_t
            V = V_t
            rKT = [Reg() for _ in range(16)]
            rV = [Reg() for _ in range(16)]
            wk, wv, wq = Buf(wk_t), Buf(wv_t), Buf(wq_t)
            xins = [Buf(xin0_t), Buf(xin1_t), Buf(xin2_t)]
            xss = [Buf(xs0_t), Buf(xs1_t)]
            tmpfs = [Buf(tmpf0_t), Buf(tmpf1_t)]
            hTs = [Buf(hT0_t), Buf(hT1_t)]
            QTs = [Buf(QT0_t), Buf(QT1_t)]
            PTs = [Buf(PT0_t), Buf(PT1_t), Buf(PT2_t), Buf(PT3_t)]
            ats = [Buf(at0_t), Buf(at1_t)]
            abs_ = [Buf(ab0_t), Buf(ab1_t)]
            asm = Buf(asm_t)
            qc0, kc0, vc0 = hp * 256, 512 + hp * 256, 1024 + hp * 256
            fw.dma("sp", lambda e: e.dma_start(out=wq[:, :, :], in_=winb_v[:, :, qc0:qc0 + 256]), wregs, [wq])
            fw.dma("sp", lambda e: e.dma_start(out=wk[:, :, :], in_=winb_v[:, :, kc0:kc0 + 256]), wregs, [wk])
            fw.dma("sp", lambda e: e.dma_start(out=wv[:, :, :], in_=winb_v[:, :, vc0:vc0 + 256]), wregs, [wv])
            rVall = Reg()
            fw.op("pool", lambda e: e.memset(V[:, :, :, 128:129], 1.0), [], [rVall])
            for r_ in rV:
                r_.w = dict(rVall.w)
            nbc = [0]

            def normS(sbi):
                if True:
                    hT = hTs[sbi % 2]
                    if hp == 0:
                        for dst_, src_, r0_ in tconv[2 * sbi:2 * sbi + 2]:
                            fw.dma("pool", lambda e: e.dma_start(out=dst_[r0_:r0_ + 1024, :], in_=src_[r0_:r0_ + 1024, :]), [], [Reg()])
                            tregs.append(fw.last_written)
                    for bl in range(4):
                        gb = sbi * 4 + bl
                        nb_ = nbc[0]
                        nbc[0] += 1
                        norm1_block(x_b[gb * 128:(gb + 1) * 128, :], xins[nb_ % 3], xss[nb_ % 2], tmpfs[nb_ % 2], hT,
                                    slice(bl * 128, (bl + 1) * 128), nb_ % 2)

            def kvS(sbi):
                if True:
                    hT = hTs[sbi % 2]
                    for hh in range(2):
                        pb = 2 + hh
                        for c in range(8):
                            fw.op("pe", lambda e: e.matmul(bank(pb), lhsT=wk[:, c, hh * 128:(hh + 1) * 128], rhs=hT[:, c, :], start=(c == 0), stop=(c == 7)),
                                  [wk, hT], [PR[pb]])
                        fw.op("act", lambda e: e.activation(out=KT[:, hh, sbi * 512:(sbi + 1) * 512], in_=bank(pb), func=AF.Copy),
                              [PR[pb]], [rKT[sbi]])
                    for bl in range(4):
                        gb = sbi * 4 + bl
                        pb = 4 + (bl % 2)
                        for c in range(8):
                            fw.op("pe", lambda e: e.matmul(bank(pb)[:, 0:256], lhsT=hT[:, c, bl * 128:(bl + 1) * 128], rhs=wv[:, c, :], start=(c == 0), stop=(c == 7)),
                                  [wv, hT], [PR[pb]])
                        fw.op("dve", lambda e: e.tensor_copy(out=V[:, gb, :, 0:128], in_=bank(pb)[:, 0:256].rearrange("p (h v) -> p h v", h=2)),
                              [PR[pb]], [rV[sbi]])

            normS(0)
            for sbi in range(16):
                if sbi + 1 < 16:
                    interleave(fw, [lambda: normS(sbi + 1), lambda: kvS(sbi)], [1, 1])
                else:
                    kvS(sbi)
            nblk = nbc[0]
            if debug is not None and debug[0] == "kv" and hp == 0:
                with nc.sbuf_tensor("dbgt", [128, 2048], F32) as dt_:
                    dtb = Buf(dt_)
                    fw.op("dve", lambda e: e.tensor_copy(out=dtb[:, 0:1024], in_=KT[:, 1, 7168:8192]), rKT, [dtb])
                    fw.op("dve", lambda e: e.tensor_copy(out=dtb[:, 1024:2048].rearrange("p (b v) -> p b v", b=8), in_=V[:, 56:64, 1, 0:128]), rV, [dtb])
                    dbg_out(dtb[:, :], [dtb])
                    fw.barrier()
                    return nc
            def b_p1(g_):
                for qi_ in range(4):
                    j_ = 4 * g_ + qi_
                    norm_p1(x_own[j_ * 128:(j_ + 1) * 128, :], xins[(4 * g_ + qi_) % 3], xsB[qi_])

            b_p1(0)
            for g in range(8):
                hT = hTs[g % 2]
                QT = QTs[g % 2]
                for qi in range(4):
                    norm_p2(xsB[qi], tmpfs[qi % 2], hT, slice(qi * 128, (qi + 1) * 128), qi % 2)
                for hh in range(2):
                    pb = 2 + hh
                    for c in range(8):
                        fw.op("pe", lambda e, c=c, hh=hh, pb=pb: e.matmul(bank(pb), lhsT=wq[:, c, hh * 128:(hh + 1) * 128], rhs=hT[:, c, :], start=(c == 0), stop=(c == 7)),
                              [wq, hT], [PR[pb]])
                    fw.op("act", lambda e, hh=hh, pb=pb: e.activation(out=QT[:, hh, :], in_=bank(pb), func=AF.Copy, scale=0.125),
                          [PR[pb]], [QT])
                def attn_g():
                    for hh in range(2):
                        kmax = 8 * g + 7

                        def emit_S(kb):
                            qmin = max(0, (kb - 8 * g) // 2)
                            c0 = qmin * 128
                            for half in range(2):
                                spb = 4 + 2 * (kb % 2) + half
                                rs = slice(half * 64, (half + 1) * 64)
                                fw.op("pe", lambda e: e.matmul(bank(spb)[:, c0:512], lhsT=KT[rs, hh, kb * 128:(kb + 1) * 128], rhs=QT[rs, hh, c0:512], start=True, stop=True),
                                      [rKT[kb // 4], QT], [PR[spb]])
                            for half in range(2):
                                spb = 4 + 2 * (kb % 2) + half
                                PT = PTs[2 * (kb % 2) + half]
                                fw.op("act", lambda e: e.activation(out=PT[:, c0:512], in_=bank(spb)[:, c0:512], func=AF.Exp),
                                      [PR[spb]], [PT])
                                for qi in range(qmin, 4):
                                    j = 4 * g + qi
                                    m = kb - 2 * j
                                    if m in (0, 1):
                                        fw.op("pool", lambda e: e.tensor_tensor(out=PT[:, qi * 128:(qi + 1) * 128], in0=PT[:, qi * 128:(qi + 1) * 128], in1=maskb[:, m * 128:(m + 1) * 128], op=OP.mult),
                                              [PT, maskb], [PT])

                        def emit_PV(kb):
                            qmin = max(0, (kb - 8 * g) // 2)
                            for half in range(2):
                                PT = PTs[2 * (kb % 2) + half]
                                for qi in range(qmin, 4):
                                    j = 4 * g + qi
                                    ob = qi
                                    ov = bank(ob)[:, half * 129:(half + 1) * 129]
                                    fw.op("pe", lambda e: e.matmul(ov, lhsT=PT[:, qi * 128:(qi + 1) * 128], rhs=V[:, kb, hh, :], start=(kb == 0 and half == 0), stop=(kb == 2 * j + 1 and half == 1)),
                                          [PT, rV[kb // 4]], [PR[ob]])

                        emit_S(0)
                        for kb in range(kmax + 1):
                            if kb + 1 <= kmax:
                                emit_S(kb + 1)
                            emit_PV(kb)
                        for qi in range(4):
                            j = 4 * g + qi
                            ob = qi
                            at = ats[qi % 2]
                            ab = abs_[qi % 2]
                            o3 = bank(ob)[:, 0:258].rearrange("p (a b) -> p a b", a=2)
                            fw.op("dve", lambda e, o3=o3: e.reciprocal(out=asm[:, 0:2], in_=o3[:, :, 128]), [PR[ob]], [asm])
                            fw.op("dve", lambda e: e.tensor_tensor(out=asm[:, 2:3], in0=asm[:, 1:2], in1=neglam[:, :], op=OP.mult), [asm, neglam], [asm])
                            fw.op("dve", lambda e, o3=o3, at=at: e.tensor_scalar(out=at[:, :], in0=o3[:, 0, 0:128], scalar1=asm[:, 0:1], scalar2=None, op0=OP.mult), [PR[ob], asm], [at])
                            fw.op("dve", lambda e, o3=o3, at=at: e.scalar_tensor_tensor(out=at[:, :], in0=o3[:, 1, 0:128], scalar=asm[:, 2:3], in1=at[:, :], op0=OP.mult, op1=OP.add), [PR[ob], asm, at], [at])
                            fw.op("act", lambda e, at=at, ab=ab: e.activation(out=ab[:, :], in_=at[:, :], func=AF.Square, accum_out=asm[:, 3:4]), [at], [ab, asm])
                            rstd_of(asm, 3, 128, 4)
                            fw.op("dve", lambda e, at=at, ab=ab: e.scalar_tensor_tensor(out=ab[:, :], in0=at[:, :], scalar=asm[:, 4:5], in1=hg_bc[:, :], op0=OP.mult, op1=OP.mult), [at, asm, hg_bc], [ab])
                            tb = 6 + (qi % 2)
                            fw.op("pe", lambda e, ab=ab, tb=tb: e.transpose(out=bank_bf(tb)[:, 0:128], in_=ab[:, :], identity=identb[:, :]), [ab, identb], [PR[tb]])
                            yst = ysts[ysi[0] % 2]
                            ysi[0] += 1
                            fw.op("act", lambda e, tb=tb: e.activation(out=yst[:, :], in_=bank_bf(tb)[:, 0:128], func=AF.Copy), [PR[tb]], [yst])
                            rr_ = Reg()
                            fw.dma("sp", lambda e, j=j, hh=hh: e.dma_start(out=yaTd[hp * 2 + hh, :, j * 128:(j + 1) * 128], in_=yst[:, :]), [yst], [rr_])
                            yaregs.append(rr_)
                if g + 1 < 8:
                    interleave(fw, [attn_g, lambda: b_p1(g + 1)], [8, 1])
                else:
                    attn_g()
            fw.barrier()
    def tl(name, shape, dt=F32):
        return Buf(nc.alloc_sbuf_tensor(name, list(shape), dt))

    wbab_v = wbab.rearrange("(c p) n -> p c n", p=128)
    wbbb_v = wbbb.rearrange("(c p) n -> p c n", p=128)
    woutb_v = woutb.rearrange("(c p) n -> p c n", p=128)
    wpqb_v = wpqb.rearrange("(c p) n -> p c n", p=128)
    Wsl = [tl(f"Wsl{i}", [128, 4096], BF16) for i in range(5)]
    wi = [0]

    def load_w(view3, c):
        w = Wsl[wi[0] % 5]
        wi[0] += 1
        fw.dma("sp", lambda e: e.dma_start(out=w[:, :].rearrange("p (c n) -> p c n", c=c), in_=view3), wregs, [w])
        return w, w[:, :].rearrange("p (c n) -> p c n", c=c)

    xgs = [[tl(f"xg{p}{b}", [128, D]) for b in range(2)] for p in range(2)]
    xsC = tl("xsC", [128, D], BF16)
    tmpfC = tl("tmpfC", [128, D])
    hTC = tl("hTC", [128, 8, 256], BF16)
    gTC = tl("gTC", [128, 16, 256], BF16)
    gu = tl("gu", [128, 512])
    gs = tl("gs", [128, 512])
    svn = tl("svn", [128, 512], BF16)
    ybt = tl("ybt", [128, 512], BF16)
    ybT = tl("ybT", [128, 4, 256], BF16)
    t5a = tl("t5a", [128, 256])
    t5b = tl("t5b", [128, 256])
    mT = tl("mT", [128, 8, 256], BF16)
    h2gs = [[tl(f"h2g{p}{b}", [128, D], BF16) for b in range(2)] for p in range(2)]
    st6 = tl("st6", [128, 12])
    scf = tl("scf", [128, 2048])
    scw = tl("scw", [128, 2048])
    tv = tl("tv", [128, 256])
    ti = tl("ti", [128, 256], U32)
    tif = tl("tif", [128, 256])
    cv = tl("cv", [128, 128])
    ci = tl("ci", [128, 128], U32)
    cif = tl("cif", [128, 128])
    gsm = tl("gsm", [128, 128])
    sm8 = tl("sm8", [128, 8])
    ak = tl("ak", [128, 128])
    bk = tl("bk", [128, 128])
    i1s = tl("i1s", [128, 128])
    i2s = tl("i2s", [128, 128])
    eidxf = tl("eidxf", [128, 128])
    eTs = [[tl(f"eT{p}{b}", [128, 128], U32) for b in range(2)] for p in range(2)]
    gTs_ = [[tl(f"gT_{p}{b}", [128, 128]) for b in range(2)] for p in range(2)]
    yaGs = [tl("yaG0", [128, 4, 256], BF16), tl("yaG1", [128, 4, 256], BF16)]
    tmpfL = ViewBuf(mod_bc.t[:, 0:1024])
    junkL = ViewBuf(mod_bc.t[:, 1024:2048].bitcast(BF16)[:, 0:1024])
    junkL2 = ViewBuf(mod_bc.t[:, 1024:2048].bitcast(BF16)[:, 1024:2048])
    junkR = [junkL, junkL2]
    aT = tl("aT", [128, 128])
    coefT = tl("coefT", [128, 128])
    NGB = 9
    Gbufs = [tl(f"Gbuf{i}", [128, 2 * D], BF16) for i in range(NGB)]
    gbi = [0]

    gslots = [[f"d_g{i}", nc.alloc_semaphore(f"d_g{i}"), 0] for i in range(NGB)]

    def gather(table, idx_ap, rd):
        k = gbi[0] % NGB
        b_ = Gbufs[k]
        pre = [Gbufs[(k + 1) % NGB]] if k % 2 == 0 else []
        gbi[0] += 1
        fw.dma("pool", lambda e: e.indirect_dma_start(out=b_[:, :], out_offset=None, in_=table[:, :], in_offset=bass.IndirectOffsetOnAxis(ap=idx_ap, axis=0)),
               rd, [b_], slot=gslots[k], pre=pre)
        return b_
    Wc = tl("Wc", [128, 256], BF16)
    Zts = [tl(f"Zt{i}", [128, 128], BF16) for i in range(4)]
    acols = [tl(f"acol{i}", [128, 1]) for i in range(8)]
    ccols = [tl(f"ccol{i}", [128, 1]) for i in range(8)]
    thr16 = tl("thr16", [128, 16])
    io16 = tl("io16", [128, 16])
    ioi = tl("ioi", [128, 16], I32)

    fw.op("pool", lambda e: e.memset(Wc[:, :], 0.0), [], [Wc])
    fw.op("pool", lambda e: e.memset(Wc[:, 127:128], 1.0), [Wc], [Wc])
    fw.op("pool", lambda e: e.iota(ioi[:, :], pattern=[[1, 16]], base=0, channel_multiplier=0), [], [ioi])
    fw.op("dve", lambda e: e.tensor_copy(out=io16[:, :], in_=ioi[:, :]), [ioi], [io16])
    fw.op("dve", lambda e: e.tensor_scalar(out=thr16[:, :], in0=io16[:, :], scalar1=16.0, scalar2=None, op0=OP.mult), [io16], [thr16])
    def top16(src3, wrk3, vals3, idx3, n, regs):
        r_src, r_wrk, r_vals, r_idx = regs
        for l in range(n):
            fw.op("dve", lambda e: e.max(out=vals3[:, l, 0:8], in_=src3[:, l, :]), [r_src], [r_vals])
            fw.op("dve", lambda e: e.max_index(out=idx3[:, l, 0:8], in_max=vals3[:, l, 0:8], in_values=src3[:, l, :]), [r_src, r_vals], [r_idx])
            fw.op("dve", lambda e: e.match_replace(out=wrk3[:, l, :], in_to_replace=vals3[:, l, 0:8], in_values=src3[:, l, :], imm_value=NEG), [r_src, r_vals], [r_wrk])
            fw.op("dve", lambda e: e.max(out=vals3[:, l, 8:16], in_=wrk3[:, l, :]), [r_wrk], [r_vals])
            fw.op("dve", lambda e: e.max_index(out=idx3[:, l, 8:16], in_max=vals3[:, l, 8:16], in_values=wrk3[:, l, :]), [r_wrk, r_vals], [r_idx])

    NGC = 16 if debug is None else debug[2]

    def cmain(g):
        p = g % 2
        xg = xgs[p]
        h2g = h2gs[p]
        for bl in range(2):
            j = 2 * g + bl
            fw.dma("sp", lambda e: e.dma_start(out=xg[bl][:, :], in_=x_own[j * 128:(j + 1) * 128, :]), [], [xg[bl]])
            norm1_block(None, xg[bl], xsC, tmpfC, hTC, slice(bl * 128, (bl + 1) * 128), bl, add_eng="act")
        for u in range(4):
            w, w3 = load_w(winb_v[:, :, 2560 + u * 512:2560 + (u + 1) * 512], 8)
            for cc in range(4):
                ch = u * 4 + cc
                pb = ch % 2
                for c in range(8):
                    fw.op("pe", lambda e: e.matmul(bank(pb)[:, 0:256], lhsT=w3[:, c, cc * 128:(cc + 1) * 128], rhs=hTC[:, c, :], start=(c == 0), stop=(c == 7)),
                          [w, hTC], [PR[pb]])
                fw.op("act", lambda e: e.activation(out=gTC[:, ch, :], in_=bank(pb)[:, 0:256], func=AF.Sigmoid), [PR[pb]], [gTC])
        wu, wu3 = load_w(winb_v[:, :, 1536:2048], 8)
        wsv, wsv3 = load_w(winb_v[:, :, 2048:2560], 8)
        for bl in range(2):
            bs_ = slice(bl * 128, (bl + 1) * 128)
            for c in range(8):
                fw.op("pe", lambda e: e.matmul(bank(0), lhsT=hTC[:, c, bs_], rhs=wu3[:, c, :], start=(c == 0), stop=(c == 7)), [wu, hTC], [PR[0]])
            for c in range(8):
                fw.op("pe", lambda e: e.matmul(bank(1), lhsT=hTC[:, c, bs_], rhs=wsv3[:, c, :], start=(c == 0), stop=(c == 7)), [wsv, hTC], [PR[1]])
            fw.op("act", lambda e: e.activation(out=gu[:, :], in_=bank(0), func=AF.Gelu_apprx_tanh), [PR[0]], [gu])
            fw.op("act", lambda e: e.activation(out=gs[:, :], in_=bank(1), func=AF.Gelu_apprx_tanh), [PR[1]], [gs])
            fw.op("dve", lambda e: e.bn_stats(out=st6[:, 0:6], in_=gs[:, :]), [gs], [st6])
            fw.op("dve", lambda e: e.bn_aggr(out=st6[:, 6:8], in_=st6[:, 0:6]), [st6], [st6])
            rstd_of(st6, 7, 1.0, 8)
            fw.op("dve", lambda e: e.tensor_scalar(out=gs[:, :], in0=gs[:, :], scalar1=st6[:, 6:7], scalar2=st6[:, 8:9], op0=OP.subtract, op1=OP.mult), [gs, st6], [gs])
            fw.op("dve", lambda e: e.tensor_tensor(out=gs[:, :], in0=gs[:, :], in1=lng_bc[:, :], op=OP.mult), [gs, lng_bc], [gs])
            fw.op("dve", lambda e: e.tensor_tensor(out=svn[:, :], in0=gs[:, :], in1=lnb_bc[:, :], op=OP.add), [gs, lnb_bc], [svn])
            for gr in range(4):
                fw.op("pe", lambda e: e.matmul(bank(0)[:, gr * 128:(gr + 1) * 128], lhsT=wsT[:, gr, :], rhs=svn[:, gr * 128:(gr + 1) * 128], start=True, stop=True),
                      [wsT, svn], [PR[0]])
            for gr in range(4):
                fw.op("dve", lambda e: e.scalar_tensor_tensor(out=ybt[:, gr * 128:(gr + 1) * 128], in0=bank(0)[:, gr * 128:(gr + 1) * 128], scalar=bs_col[:, gr:gr + 1], in1=gu[:, gr * 128:(gr + 1) * 128], op0=OP.add, op1=OP.mult),
                      [PR[0], bs_col, gu], [ybt])
            for ch in range(4):
                fw.op("pe", lambda e: e.transpose(out=bank_bf(1)[:, ch * 128:(ch + 1) * 128], in_=ybt[:, ch * 128:(ch + 1) * 128], identity=identb[:, :]), [ybt, identb], [PR[1]])
            fw.op("act", lambda e: e.activation(out=ybT[:, :, bs_], in_=bank_bf(1)[:, 0:512].rearrange("p (c t) -> p c t", c=4), func=AF.Copy), [PR[1]], [ybT])
        wa, wa3 = load_w(wbab_v, 4)
        wb, wb3 = load_w(wbbb_v, 4)
        yaG = yaGs[p]
        fw.dma("sp", lambda e: e.dma_start(out=yaG[:, :, :], in_=yaTd[:, :, g * 256:(g + 1) * 256].rearrange("c p t -> p c t")), yaregs, [yaG])
        for dc in range(8):
            ba, bb = 0, 1
            for cc in range(4):
                fw.op("pe", lambda e: e.matmul(bank(ba)[:, 0:256], lhsT=wa3[:, cc, dc * 128:(dc + 1) * 128], rhs=yaG[:, cc, :], start=(cc == 0), stop=(cc == 3)),
                      [wa, yaG], [PR[ba]])
            for cc in range(4):
                fw.op("pe", lambda e: e.matmul(bank(bb)[:, 0:256], lhsT=wb3[:, cc, dc * 128:(dc + 1) * 128], rhs=ybT[:, cc, :], start=(cc == 0), stop=(cc == 3)),
                      [wb, ybT], [PR[bb]])
            fw.op("dve", lambda e: e.tensor_tensor(out=t5a[:, :], in0=bank(ba)[:, 0:256], in1=gTC[:, dc, :], op=OP.mult), [PR[ba], gTC], [t5a])
            fw.op("dve", lambda e: e.tensor_tensor(out=t5b[:, :], in0=bank(bb)[:, 0:256], in1=gTC[:, 8 + dc, :], op=OP.mult), [PR[bb], gTC], [t5b])
            fw.op("dve", lambda e: e.tensor_tensor(out=mT[:, dc, :], in0=t5a[:, :], in1=t5b[:, :], op=OP.add), [t5a, t5b], [mT])
        wo0, wo03 = load_w(woutb_v[:, :, 0:512], 8)
        wo1, wo13 = load_w(woutb_v[:, :, 512:1024], 8)
        for bl in range(2):
            bs_ = slice(bl * 128, (bl + 1) * 128)
            for half, (wo, wo3) in enumerate(((wo0, wo03), (wo1, wo13))):
                for c in range(8):
                    fw.op("pe", lambda e: e.matmul(bank(half), lhsT=mT[:, c, bs_], rhs=wo3[:, c, :], start=(c == 0), stop=(c == 7)), [wo, mT], [PR[half]])
            fw.op("dve", lambda e: e.tensor_tensor(out=tmpfC[:, :], in0=pair(0), in1=mod_bc[:, 2 * D:3 * D], op=OP.mult), [PR[0], PR[1], mod_bc], [tmpfC])
            fw.op("dve", lambda e: e.tensor_tensor(out=xg[bl][:, :], in0=xg[bl][:, :], in1=tmpfC[:, :], op=OP.add), [xg[bl], tmpfC], [xg[bl]])
        if debug is not None and debug[0] == "x2":
            dbg_out(xg[0][:, :], [xg[0]], dbg_d[:, 0:D])
            dbg_out(xg[1][:, :], [xg[1]], dbg_d[:, D:2 * D])
            return
        for bl in range(2):
            ssb = ss_r[ss_i[0] % 4]
            ss_i[0] += 1
            fw.op("act", lambda e: e.activation(out=xsC[:, :], in_=xg[bl][:, :], func=AF.Square, accum_out=ssb[:, 0:1]), [xg[bl]], [xsC, ssb])
            rstd_of(ssb, 0, D, 1)
            fw.op("dve", lambda e: e.scalar_tensor_tensor(out=tmpfC[:, :], in0=xg[bl][:, :], scalar=ssb[:, 1:2], in1=mod_bc[:, 4 * D:5 * D], op0=OP.mult, op1=OP.mult), [xg[bl], ssb, mod_bc], [tmpfC])
            fw.op("dve", lambda e: e.tensor_tensor(out=h2g[bl][:, :], in0=tmpfC[:, :], in1=mod_bc[:, 3 * D:4 * D], op=OP.add), [tmpfC, mod_bc], [h2g[bl]])
            for c in range(8):
                fw.op("pe", lambda e: e.transpose(out=bank_bf(bl)[:, c * 128:(c + 1) * 128], in_=h2g[bl][:, c * 128:(c + 1) * 128], identity=identb[:, :]), [h2g[bl], identb], [PR[bl]])
            fw.op("act", lambda e: e.activation(out=hTC[:, :, bl * 128:(bl + 1) * 128], in_=bank_bf(bl)[:, 0:1024].rearrange("p (c t) -> p c t", c=8), func=AF.Copy), [PR[bl]], [hTC])
        for u in range(4):
            w, w3 = load_w(wpqb_v[:, :, u * 512:(u + 1) * 512], 8)
            for cc in range(4):
                l = u * 4 + cc
                pb = l % 2
                for c in range(8):
                    fw.op("pe", lambda e: e.matmul(bank(pb)[:, 0:256], lhsT=w3[:, c, cc * 128:(cc + 1) * 128], rhs=hTC[:, c, :], start=(c == 0), stop=(c == 7)), [w, hTC], [PR[pb]])
                fw.op("act", lambda e: e.activation(out=gTC[:, l, :], in_=bank(pb)[:, 0:256], func=AF.Copy), [PR[pb]], [gTC])
        for bl in range(2):
            eT = eTs[p][bl]
            gT_ = gTs_[p][bl]
            bs_ = slice(bl * 128, (bl + 1) * 128)
            for rnd in range(2):
                for l8 in range(8):
                    l = rnd * 8 + l8
                    fw.op("pe", lambda e: e.matmul(bank(l8 // 4)[:, (l8 % 4) * 128:(l8 % 4 + 1) * 128], lhsT=gTC[:, l, bs_], rhs=skT[:, l, :], start=True, stop=True), [gTC, skT], [PR[l8 // 4]])
                if rnd == 0:
                    fw.op("act", lambda e: e.activation(out=scf[:, 0:1024], in_=pair(0), func=AF.Copy), [PR[0], PR[1]], [scf])
                else:
                    fw.op("act", lambda e: e.activation(out=scf[:, 1024:2048], in_=pair(0), func=AF.Copy), [PR[0], PR[1]], [scf])
            r_src, r_wrk, r_vals, r_idx = scf, scw, tv, ti
            top16(scf[:, :].rearrange("p (l n) -> p l n", l=16), scw[:, :].rearrange("p (l n) -> p l n", l=16),
                  tv[:, :].rearrange("p (l k) -> p l k", l=16), ti[:, :].rearrange("p (l k) -> p l k", l=16), 16, (scf, scw, tv, ti))
            fw.op("dve", lambda e: e.tensor_copy(out=tif[:, :], in_=ti[:, :]), [ti], [tif])
            tv4 = tv[:, :].rearrange("p (h i k) -> p h i k", h=8, i=2)
            tif4 = tif[:, :].rearrange("p (h i k) -> p h i k", h=8, i=2)
            cand4 = scf[:, :].rearrange("p (h a b) -> p h a b", h=8, a=16)
            fw.op("dve", lambda e: e.tensor_tensor(out=cand4, in0=tv4[:, :, 0, :].unsqueeze(3).to_broadcast([128, 8, 16, 16]), in1=tv4[:, :, 1, :].unsqueeze(2).to_broadcast([128, 8, 16, 16]), op=OP.add), [tv], [scf])
            top16(scf[:, :].rearrange("p (h n) -> p h n", h=8), scw[:, :].rearrange("p (h n) -> p h n", h=8),
                  cv[:, :].rearrange("p (h k) -> p h k", h=8), ci[:, :].rearrange("p (h k) -> p h k", h=8), 8, (scf, scw, cv, ci))
            cv3 = cv[:, :].rearrange("p (h k) -> p h k", h=8)
            gsm3 = gsm[:, :].rearrange("p (h k) -> p h k", h=8)
            fw.op("dve", lambda e: e.tensor_tensor(out=gsm3, in0=cv3, in1=cv3[:, :, 0:1].to_broadcast([128, 8, 16]), op=OP.subtract), [cv], [gsm])
            fw.op("act", lambda e: e.activation(out=gsm[:, :], in_=gsm[:, :], func=AF.Exp), [gsm], [gsm])
            fw.op("dve", lambda e: e.tensor_reduce(out=sm8[:, :], in_=gsm3, axis=AX.X, op=OP.add), [gsm], [sm8])
            fw.op("dve", lambda e: e.reciprocal(out=sm8[:, :], in_=sm8[:, :]), [sm8], [sm8])
            fw.op("dve", lambda e: e.tensor_tensor(out=gsm3, in0=gsm3, in1=sm8[:, :].unsqueeze(2).to_broadcast([128, 8, 16]), op=OP.mult), [gsm, sm8], [gsm])
            fw.op("dve", lambda e: e.tensor_copy(out=cif[:, :], in_=ci[:, :]), [ci], [cif])
            oh4 = scw[:, :].rearrange("p (h k a) -> p h k a", h=8, k=16)
            cif3 = cif[:, :].rearrange("p (h k) -> p h k", h=8)
            ak3 = ak[:, :].rearrange("p (h k) -> p h k", h=8)
            bk3 = bk[:, :].rearrange("p (h k) -> p h k", h=8)
            thr_b = thr16[:, :].unsqueeze(1).unsqueeze(1).to_broadcast([128, 8, 16, 16])
            io_b = io16[:, :].unsqueeze(1).unsqueeze(1).to_broadcast([128, 8, 16, 16])
            fw.op("dve", lambda e: e.tensor_tensor(out=oh4, in0=cif3.unsqueeze(3).to_broadcast([128, 8, 16, 16]), in1=thr_b, op=OP.is_ge), [cif, thr16], [scw])
            fw.op("dve", lambda e: e.tensor_reduce(out=ak3, in_=oh4, axis=AX.X, op=OP.add), [scw], [ak])
            fw.op("dve", lambda e: e.tensor_scalar(out=ak[:, :], in0=ak[:, :], scalar1=-1.0, scalar2=None, op0=OP.add), [ak], [ak])
            fw.op("dve", lambda e: e.scalar_tensor_tensor(out=bk[:, :], in0=ak[:, :], scalar=-16.0, in1=cif[:, :], op0=OP.mult, op1=OP.add), [ak, cif], [bk])
            for (kk3, pidx, dst) in ((ak3, 0, i1s), (bk3, 1, i2s)):
                fw.op("dve", lambda e: e.tensor_tensor(out=oh4, in0=kk3.unsqueeze(3).to_broadcast([128, 8, 16, 16]), in1=io_b, op=OP.is_equal), [ak, bk, io16], [scw])
                fw.op("dve", lambda e: e.tensor_tensor(out=oh4, in0=oh4, in1=tif4[:, :, pidx, :].unsqueeze(2).to_broadcast([128, 8, 16, 16]), op=OP.mult), [scw, tif], [scw])
                fw.op("dve", lambda e: e.tensor_reduce(out=dst[:, :].rearrange("p (h k) -> p h k", h=8), in_=oh4, axis=AX.X, op=OP.add), [scw], [dst])
            fw.op("dve", lambda e: e.scalar_tensor_tensor(out=eidxf[:, :], in0=i1s[:, :], scalar=128.0, in1=i2s[:, :], op0=OP.mult, op1=OP.add), [i1s, i2s], [eidxf])
            fw.op("pe", lambda e: e.transpose(out=bank(0)[:, 0:128], in_=eidxf[:, :], identity=identf[:, :]), [eidxf, identf], [PR[0]])
            fw.op("dve", lambda e: e.tensor_copy(out=eT[:, :], in_=bank(0)[:, 0:128]), [PR[0]], [eT])
            fw.op("pe", lambda e: e.transpose(out=bank(1)[:, 0:128], in_=gsm[:, :], identity=identf[:, :]), [gsm, identf], [PR[1]])
            fw.op("act", lambda e: e.activation(out=gT_[:, :], in_=bank(1)[:, 0:128], func=AF.Copy), [PR[1]], [gT_])

    def loops(g):
        p = g % 2
        xg = xgs[p]
        h2g = h2gs[p]
        for bl in range(2):
            j = 2 * g + bl
            eT = eTs[p][bl]
            gT_ = gTs_[p][bl]
            gbs = {}

            def s_gather(t):
                gbs[t] = gather(pdu, eT[:, t:t + 1], [eT] + tregs)

            def s_bcast(t):
                hbp = 2 + (t % 2)
                for half in range(2):
                    fw.op("pe", lambda e: e.matmul(bank(2 * hbp + half), lhsT=identb[:, t:t + 1].to_broadcast([128, 128]), rhs=h2g[bl][:, half * 512:(half + 1) * 512], start=True, stop=True),
                          [identb, h2g[bl]], [PR[2 * hbp + half]])

            def s_dot(t):
                hbp = 2 + (t % 2)
                ac = acols[t % 8]
                cc_ = ccols[t % 8]
                gb_ = gbs[t]
                jk = junkR[t % 2]
                fw.op("dve", lambda e: e.scalar_tensor_tensor(out=jk[:, :], in0=gb_[:, 0:D], scalar=1.0, in1=pair(hbp), op0=OP.mult, op1=OP.mult, accum_out=ac[:, 0:1]),
                      [gb_, PR[2 * hbp], PR[2 * hbp + 1]], [jk, ac])
                fw.op("act", lambda e: e.activation(out=cc_[:, 0:1], in_=ac[:, 0:1], func=AF.Gelu_apprx_tanh), [ac], [cc_])

            def s_up(t):
                cc_ = ccols[t % 8]
                zt = Zts[t % 4]
                gb_ = gbs.pop(t)
                fw.op("dve", lambda e: e.tensor_scalar(out=zt[:, :], in0=Wc[:, 127 - t:255 - t], scalar1=cc_[:, 0:1], scalar2=gT_[:, t:t + 1], op0=OP.mult, op1=OP.mult),
                      [Wc, cc_, gT_], [zt])
                for half in range(2):
                    fw.op("pe", lambda e: e.matmul(bank(2 + half), lhsT=zt[:, :], rhs=gb_[:, D + half * 512:D + (half + 1) * 512], start=(t == 0), stop=(t == 127)),
                          [zt, gb_], [PR[2 + half]])

            AH = NGB - 2
            for t in range(min(AH, 128)):
                s_gather(t)
            s_bcast(0)
            for t in range(128):
                if t + AH < 128:
                    s_gather(t + AH)
                if t + 1 < 128:
                    s_bcast(t + 1)
                s_dot(t)
                if t >= 1:
                    s_up(t - 1)
            s_up(127)
            fw.op("dve", lambda e: e.tensor_tensor(out=tmpfL[:, :], in0=pair(1), in1=mod_bc[:, 5 * D:6 * D], op=OP.mult), [PR[2], PR[3], mod_bc], [tmpfL])
            fw.op("dve", lambda e: e.tensor_tensor(out=xg[bl][:, :], in0=xg[bl][:, :], in1=tmpfL[:, :], op=OP.add), [xg[bl], tmpfL], [xg[bl]])
            ssb = ss_r[ss_i[0] % 4]
            ss_i[0] += 1
            fw.op("act", lambda e: e.activation(out=junkL[:, :], in_=xg[bl][:, :], func=AF.Square, accum_out=ssb[:, 0:1]), [xg[bl]], [junkL, ssb])
            rstd_of(ssb, 0, D, 1)
            fw.op("dve", lambda e: e.scalar_tensor_tensor(out=tmpfL[:, :], in0=xg[bl][:, :], scalar=ssb[:, 1:2], in1=fg_bc[:, :], op0=OP.mult, op1=OP.mult), [xg[bl], ssb, fg_bc], [tmpfL])
            fw.dma("sp", lambda e: e.dma_start(out=out_d[j * 128:(j + 1) * 128, :], in_=tmpfL[:, :]), [tmpfL], [r_none])

    cmain(0)
    if debug is not None and debug[0] == "x2":
        fw.barrier()
        return nc
    for g in range(NGC):
        if g + 1 < NGC:
            interleave(fw, [lambda: loops(g), lambda: cmain(g + 1)], [2, 1])
        else:
            loops(g)
    fw.barrier()
    return nc


def _prep_inputs(inputs):
    g = {k: np.asarray(v) for k, v in inputs.items()}
    x = np.ascontiguousarray(g["x"], dtype=np.float32)
    shared = {
        "w_ada": g["w_ada"][0], "b_ada": g["b_ada"], "norm1_g": g["norm1_g"], "w_in": g["w_in"][0],
        "da_lambda_q1": g["da_lambda_q1"], "da_lambda_k1": g["da_lambda_k1"],
        "da_lambda_q2": g["da_lambda_q2"], "da_lambda_k2": g["da_lambda_k2"],
        "da_head_g": g["da_head_g"], "sg_ln_g": g["sg_ln_g"], "sg_ln_b": g["sg_ln_b"],
        "sg_w": g["sg_w"][0], "sg_b": g["sg_b"][0], "w_branch_a": g["w_branch_a"][0],
        "w_branch_b": g["w_branch_b"][0], "w_out": g["w_out"][0], "norm2_g": g["norm2_g"],
        "peer_w_query": g["peer_w_query"][0], "peer_sub_keys": g["peer_sub_keys"][0].reshape(16, 128, 128),
        "peer_down": g["peer_down"][0], "peer_up": g["peer_up"][0], "final_g": g["final_g"].reshape(1, D),
    }
    shared = {k: np.ascontiguousarray(v, dtype=np.float32) for k, v in shared.items()}
    kk = np.arange(128)[:, None] // 64
    qq = np.arange(128)[None, :] // 64
    diag = (kk <= qq).astype(np.float32)
    in_maps = []
    for core in range(8):
        b, par = core // 2, core % 2
        xb = x[b]
        xo = np.ascontiguousarray(xb.reshape(NB, 128, D)[par::2].reshape(NOWN * 128, D))
        if par == 0:
            am = np.concatenate([diag, np.zeros((128, 128), np.float32)], axis=1)
        else:
            am = np.concatenate([np.ones((128, 128), np.float32), diag], axis=1)
        m = dict(shared)
        m["x_b"] = xb
        m["x_own"] = xo
        m["c_b"] = np.ascontiguousarray(g["c"][b:b + 1], dtype=np.float32)
        m["amask"] = np.ascontiguousarray(am)
        in_maps.append(m)
    return in_maps


def kernel(**inputs):
    in_maps = _prep_inputs(inputs)
    nc = build()
    res = run_bass_kernel_spmd(nc, in_maps, core_ids=list(range(8)))
    out = np.zeros((4, S, D), np.float32)
    for core in range(8):
        b, par = core // 2, core % 2
        o = np.asarray(res.results[core]["out"]).reshape(NOWN, 128, D)
        out[b].reshape(NB, 128, D)[par::2] = o
    return out
```
